# Optimizing a Trainium2 kernel written in Bass

```python
import jax, jax.numpy as jnp
from jax import lax
import numpy as np

D_MODEL = 2048
BATCH = 4
SEQ = 8192
DEPTH = 2

N_EVEN = (DEPTH + 1) // 2
N_ODD = DEPTH // 2
HEAD_DIM = 64
EPS = 1e-6
LN_EPS = 1e-5

ATT_HEADS = 16
ATT_KV_HEADS = 4
ATT_GROUP = ATT_HEADS // ATT_KV_HEADS
ATT_Q = ATT_HEADS * HEAD_DIM
ATT_KV = ATT_KV_HEADS * HEAD_DIM
WINDOW = 128
ATT_BLOCK = 128
ROPE_THETA = 10000.0
CONV_CH = 1024
CONV_WIDTH = 31
AB_IN = ATT_Q + 2 * ATT_KV + 2 * CONV_CH
AB_OUT = ATT_Q + CONV_CH

RWKV_HEADS = 16
RWKV_DIM = RWKV_HEADS * HEAD_DIM
DECAY_LORA = 96
AAA_LORA = 96
GATE_LORA = 256
RWKV_LN_EPS = 64e-5
CD_SHIFT = 3 * RWKV_DIM + DECAY_LORA + AAA_LORA + GATE_LORA
LRU_WIDTH = 1024
LRU_BLOCKS = 16
LRU_BLOCK_DIM = LRU_WIDTH // LRU_BLOCKS
LRU_CONV_WIDTH = 4
LRU_C = 8.0
CD_IN = CD_SHIFT + 2 * LRU_WIDTH
CD_OUT = RWKV_DIM + LRU_WIDTH

N_EXPERTS = 16
N_GROUPS = 4
EXPERTS_PER_GROUP = N_EXPERTS // N_GROUPS
TOP_K = 2
D_EXPERT = 1024
MOE_BLOCK = 256

kernel_name = 'hybrid_swa_conformer_rwkv7_rglru_moe'


def rms_norm(x, g):
    xf = x.astype(jnp.float32)
    y = xf * lax.rsqrt(jnp.mean(xf * xf, axis=-1, keepdims=True) + EPS)
    return (y * g.astype(jnp.float32)).astype(x.dtype)


def layer_norm(x, g, b):
    xf = x.astype(jnp.float32)
    mu = jnp.mean(xf, axis=-1, keepdims=True)
    var = jnp.mean(jnp.square(xf - mu), axis=-1, keepdims=True)
    y = (xf - mu) * lax.rsqrt(var + LN_EPS)
    return (y * g.astype(jnp.float32) + b.astype(jnp.float32)).astype(x.dtype)


def modulate(h, shift, scale):
    return h * (1.0 + scale[:, None, :]) + shift[:, None, :]


def rope(x, positions):
    half = HEAD_DIM // 2
    inv_freq = ROPE_THETA ** (-jnp.arange(half, dtype=jnp.float32) / half)
    ang = positions.astype(jnp.float32)[..., None] * inv_freq
    cos = jnp.cos(ang)[:, :, None, :]
    sin = jnp.sin(ang)[:, :, None, :]
    xf = x.astype(jnp.float32)
    x1, x2 = xf[..., :half], xf[..., half:]
    return jnp.concatenate([x1 * cos - x2 * sin, x2 * cos + x1 * sin], axis=-1).astype(x.dtype)


def token_shift(z):
    return jnp.pad(z[:, :-1], ((0, 0), (1, 0), (0, 0)))


def causal_depthwise_conv(x, w, b):
    width = w.shape[0]
    y = lax.conv_general_dilated(x, w.astype(x.dtype), window_strides=(1,), padding=[(width - 1, 0)],
                                 dimension_numbers=('NWC', 'WIO', 'NWC'), feature_group_count=x.shape[-1])
    return y + b


def sliding_window_attention(q, k, v, sinks):
    B, S = q.shape[0], q.shape[1]
    nb = S // ATT_BLOCK
    qb = q.reshape(B, nb, ATT_BLOCK, ATT_KV_HEADS, ATT_GROUP, HEAD_DIM)
    kb = k.reshape(B, nb, ATT_BLOCK, ATT_KV_HEADS, HEAD_DIM)
    vb = v.reshape(B, nb, ATT_BLOCK, ATT_KV_HEADS, HEAD_DIM)

    def with_prev(t):
        prev = jnp.pad(t[:, :-1], ((0, 0), (1, 0), (0, 0), (0, 0), (0, 0)))
        return jnp.concatenate([prev, t], axis=2)

    kk, vv = with_prev(kb), with_prev(vb)
    s = jnp.einsum('bnqhgd,bnkhd->bnhgqk', qb, kk).astype(jnp.float32) * (HEAD_DIM ** -0.5)
    qi = jnp.arange(ATT_BLOCK)[:, None]
    kj = jnp.arange(2 * ATT_BLOCK)[None, :]
    dist = ATT_BLOCK + qi - kj
    kpos = (jnp.arange(nb)[:, None, None] - 1) * ATT_BLOCK + kj[None]
    valid = (dist >= 0) & (dist < WINDOW) & (kpos >= 0)
    s = jnp.where(valid[None, :, None, None], s, -1e30)
    sink = sinks.astype(jnp.float32).reshape(ATT_KV_HEADS, ATT_GROUP)[None, None, :, :, None, None]
    m = jnp.maximum(jnp.max(s, axis=-1, keepdims=True), sink)
    p = jnp.exp(s - m)
    p = p / (jnp.sum(p, axis=-1, keepdims=True) + jnp.exp(sink - m))
    o = jnp.einsum('bnhgqk,bnkhd->bnqhgd', p.astype(v.dtype), vv)
    return o.reshape(B, S, ATT_Q)


def conformer_conv(u, conv_w, conv_b, ln_g, ln_b):
    val, gate = u[..., :CONV_CH], u[..., CONV_CH:]
    y = val * jax.nn.sigmoid(gate)
    y = causal_depthwise_conv(y, conv_w, conv_b)
    return jax.nn.silu(layer_norm(y, ln_g, ln_b))


def mixer_ab(h, positions, w_in, sinks, conv_w, conv_b, ln_g, ln_b, w_out):
    B, S, _ = h.shape
    z = h @ w_in
    q, k, v, u = jnp.split(z, [ATT_Q, ATT_Q + ATT_KV, ATT_Q + 2 * ATT_KV], axis=-1)
    q = rope(q.reshape(B, S, ATT_HEADS, HEAD_DIM), positions)
    k = rope(k.reshape(B, S, ATT_KV_HEADS, HEAD_DIM), positions)
    v = v.reshape(B, S, ATT_KV_HEADS, HEAD_DIM)
    att = sliding_window_attention(q, k, v, sinks)
    conv = conformer_conv(u, conv_w, conv_b, ln_g, ln_b)
    return jnp.concatenate([att, conv], axis=-1) @ w_out


def _rwkv7_step(state, inp):
    r_t, w_t, k_t, v_t, kk_t, b_t = inp
    sa = jnp.einsum('bhvk,bhk->bhv', state, -kk_t)
    state = (state * w_t[:, :, None, :]
             + sa[..., None] * b_t[:, :, None, :]
             + v_t[..., None] * k_t[:, :, None, :])
    return state, jnp.einsum('bhvk,bhk->bhv', state, r_t)


def rwkv7_time_mix(r, k, v, zw, za, zg, w0, w2, a0, a2, g2, k_k, k_a, r_k, ln_g, ln_b):
    B, S, _ = r.shape
    f32 = jnp.float32
    heads = lambda t: t.reshape(B, S, RWKV_HEADS, HEAD_DIM)
    w = -jax.nn.softplus(-(w0 + jnp.tanh(zw) @ w2).astype(f32)) - 0.5
    decay = jnp.exp(-jnp.exp(w))
    a = jax.nn.sigmoid((a0 + za @ a2).astype(f32))
    g = jax.nn.sigmoid(zg) @ g2
    kk = heads((k * k_k).astype(f32))
    kk = kk * lax.rsqrt(jnp.maximum(jnp.sum(kk * kk, axis=-1, keepdims=True), 1e-24))
    k = k.astype(f32) * (1.0 + (a - 1.0) * k_a)
    r_h, k_h, v_h, w_h, a_h = heads(r.astype(f32)), heads(k), heads(v.astype(f32)), heads(decay), heads(a)
    xs = tuple(jnp.moveaxis(t, 1, 0) for t in (r_h, w_h, k_h, v_h, kk, kk * a_h))
    state0 = jnp.zeros((B, RWKV_HEADS, HEAD_DIM, HEAD_DIM), f32)
    _, y = lax.scan(_rwkv7_step, state0, xs)
    y = jnp.moveaxis(y, 0, 1)
    mu = jnp.mean(y, axis=-1, keepdims=True)
    var = jnp.mean(jnp.square(y - mu), axis=-1, keepdims=True)
    y = ((y - mu) * lax.rsqrt(var + RWKV_LN_EPS)).reshape(B, S, RWKV_DIM) * ln_g + ln_b
    bonus = jnp.sum(r_h * k_h * r_k, axis=-1, keepdims=True) * v_h
    y = (y + bonus.reshape(B, S, RWKV_DIM)) * g
    return y.astype(r.dtype)


def _linear_recurrence_combine(left, right):
    a_l, b_l = left
    a_r, b_r = right
    return a_l * a_r, a_r * b_l + b_r


def rglru_branch(zd, conv_w, conv_b, wa, ba, wx, bx, lam):
    B, S, _ = zd.shape
    f32 = jnp.float32
    xb, gb = zd[..., :LRU_WIDTH], zd[..., LRU_WIDTH:]
    xb = causal_depthwise_conv(xb, conv_w, conv_b)
    xh = xb.reshape(B, S, LRU_BLOCKS, LRU_BLOCK_DIM)
    gate_r = jnp.einsum('bsgi,gij->bsgj', xh, wa).reshape(B, S, LRU_WIDTH) + ba
    gate_i = jnp.einsum('bsgi,gij->bsgj', xh, wx).reshape(B, S, LRU_WIDTH) + bx
    r = jax.nn.sigmoid(gate_r.astype(f32))
    i = jax.nn.sigmoid(gate_i.astype(f32))
    log_a = -LRU_C * r * jax.nn.softplus(-lam.astype(f32))
    a = jnp.exp(log_a)
    u = jnp.sqrt(-jnp.expm1(2.0 * log_a)) * (i * xb.astype(f32))
    _, hseq = lax.associative_scan(_linear_recurrence_combine, (a, u), axis=1)
    return (hseq * jax.nn.gelu(gb.astype(f32))).astype(zd.dtype)


def mixer_cd(h, w_in, shift_mu, w0, w2, a0, a2, g2, k_k, k_a, r_k, ln_g, ln_b,
             conv_w, conv_b, wa, ba, wx, bx, lam, w_out):
    z = h @ w_in
    zc, zd = z[..., :CD_SHIFT], z[..., CD_SHIFT:]
    zc = zc + shift_mu * (token_shift(zc) - zc)
    r, k, v, zw, za, zg = jnp.split(zc, [RWKV_DIM, 2 * RWKV_DIM, 3 * RWKV_DIM,
                                         3 * RWKV_DIM + DECAY_LORA,
                                         3 * RWKV_DIM + DECAY_LORA + AAA_LORA], axis=-1)
    y_c = rwkv7_time_mix(r, k, v, zw, za, zg, w0, w2, a0, a2, g2, k_k, k_a, r_k, ln_g, ln_b)
    y_d = rglru_branch(zd, conv_w, conv_b, wa, ba, wx, bx, lam)
    return jnp.concatenate([y_c, y_d], axis=-1) @ w_out


def route(xt, router_w, router_bias):
    probs = jax.nn.softmax((xt @ router_w).astype(jnp.float32), axis=-1)
    sel = (probs + router_bias.astype(jnp.float32)).reshape(-1, N_GROUPS, EXPERTS_PER_GROUP)
    group_score = jnp.sum(lax.top_k(sel, TOP_K)[0], axis=-1)
    g_idx = jnp.argmax(group_score, axis=-1)
    in_group = jnp.take_along_axis(sel, g_idx[:, None, None], axis=1)[:, 0]
    _, local = lax.top_k(in_group, TOP_K)
    expert_idx = (g_idx[:, None] * EXPERTS_PER_GROUP + local).astype(jnp.int32)
    gate = jnp.take_along_axis(probs, expert_idx, axis=1)
    return expert_idx, gate / jnp.sum(gate, axis=-1, keepdims=True)


def moe_ffn(h, router_w, router_bias, w1, w3, w2):
    B, S, D = h.shape
    xt = h.reshape(-1, D)
    n = xt.shape[0]
    nk = n * TOP_K
    expert_idx, gate = route(xt, router_w, router_bias)
    flat_e = expert_idx.reshape(-1)
    flat_tok = jnp.repeat(jnp.arange(n, dtype=jnp.int32), TOP_K)
    flat_w = gate.reshape(-1)
    order = jnp.argsort(flat_e)
    se = flat_e[order]
    counts = jnp.zeros((N_EXPERTS,), jnp.int32).at[flat_e].add(1)
    padded = (counts + MOE_BLOCK - 1) // MOE_BLOCK * MOE_BLOCK
    start = jnp.cumsum(counts) - counts
    pstart = jnp.cumsum(padded) - padded
    pend = pstart + padded
    dest = pstart[se] + (jnp.arange(nk, dtype=jnp.int32) - start[se])
    n_blocks = -(-nk // MOE_BLOCK) + N_EXPERTS
    rows = n_blocks * MOE_BLOCK
    row_tok = jnp.full((rows,), n, jnp.int32).at[dest].set(flat_tok[order])
    row_w = jnp.zeros((rows,), h.dtype).at[dest].set(flat_w[order].astype(h.dtype))
    block_start = jnp.arange(n_blocks, dtype=jnp.int32) * MOE_BLOCK
    block_e = jnp.minimum(jnp.sum(block_start[:, None] >= pend[None, :], axis=1), N_EXPERTS - 1)
    x_pad = jnp.concatenate([xt, jnp.zeros((1, D), xt.dtype)], axis=0)
    xb = x_pad[row_tok].reshape(n_blocks, MOE_BLOCK, D)

    def expert_block(args):
        xblk, e = args
        hid = jax.nn.silu(xblk @ w1[e]) * (xblk @ w3[e])
        return hid @ w2[e]

    yb = lax.map(expert_block, (xb, block_e))
    y = jnp.zeros((n + 1, D), h.dtype).at[row_tok].add(yb.reshape(rows, D) * row_w[:, None])
    return y[:n].reshape(B, S, D)


def setup_inputs(seed: int = 0) -> dict:
    key = jax.random.key(seed)
    ks = iter(jax.random.split(key, 48))
    f32 = jnp.float32

    def nrm(shape, scale):
        return scale * jax.random.normal(next(ks), shape, f32)

    def gain(shape):
        return 1.0 + nrm(shape, 0.02)

    def unif(shape, lo, hi):
        return jax.random.uniform(next(ks), shape, f32, lo, hi)

    D = D_MODEL
    x = nrm((BATCH, SEQ, D), 1.0)
    c = nrm((BATCH, D), 1.0)
    positions = (jax.random.randint(next(ks), (BATCH, 1), 0, 1024, jnp.int32)
                 + jnp.arange(SEQ, dtype=jnp.int32)[None, :])
    router_w = nrm((D, N_EXPERTS), D ** -0.5)
    router_bias = nrm((N_EXPERTS,), 0.01)
    ada_w = nrm((DEPTH, D, 6 * D), 0.5 * D ** -0.5)
    ada_b = nrm((DEPTH, 6 * D), 0.02)
    norm_mix = gain((DEPTH, D))
    norm_ffn = gain((DEPTH, D))
    moe_w1 = nrm((DEPTH, N_EXPERTS, D, D_EXPERT), D ** -0.5)
    moe_w3 = nrm((DEPTH, N_EXPERTS, D, D_EXPERT), D ** -0.5)
    moe_w2 = nrm((DEPTH, N_EXPERTS, D_EXPERT, D), D_EXPERT ** -0.5)
    ab_w_in = nrm((N_EVEN, D, AB_IN), D ** -0.5)
    ab_sinks = nrm((N_EVEN, ATT_HEADS), 0.5)
    ab_conv_w = nrm((N_EVEN, CONV_WIDTH, 1, CONV_CH), CONV_WIDTH ** -0.5)
    ab_conv_b = nrm((N_EVEN, CONV_CH), 0.02)
    ab_conv_ln_g = gain((N_EVEN, CONV_CH))
    ab_conv_ln_b = nrm((N_EVEN, CONV_CH), 0.02)
    ab_w_out = nrm((N_EVEN, AB_OUT, D), AB_OUT ** -0.5)
    cd_w_in = nrm((N_ODD, D, CD_IN), D ** -0.5)
    cd_shift_mu = unif((N_ODD, CD_SHIFT), 0.0, 1.0)
    cd_w0 = unif((N_ODD, RWKV_DIM), -3.0, 1.0)
    cd_w2 = nrm((N_ODD, DECAY_LORA, RWKV_DIM), 0.1 * DECAY_LORA ** -0.5)
    cd_a0 = nrm((N_ODD, RWKV_DIM), 0.5)
    cd_a2 = nrm((N_ODD, AAA_LORA, RWKV_DIM), 0.1 * AAA_LORA ** -0.5)
    cd_g2 = nrm((N_ODD, GATE_LORA, RWKV_DIM), GATE_LORA ** -0.5)
    cd_k_k = 0.85 + nrm((N_ODD, RWKV_DIM), 0.02)
    cd_k_a = gain((N_ODD, RWKV_DIM))
    cd_r_k = nrm((N_ODD, RWKV_HEADS, HEAD_DIM), 0.1)
    cd_ln_x_g = gain((N_ODD, RWKV_DIM))
    cd_ln_x_b = nrm((N_ODD, RWKV_DIM), 0.02)
    cd_lru_conv_w = nrm((N_ODD, LRU_CONV_WIDTH, 1, LRU_WIDTH), 0.5)
    cd_lru_conv_b = nrm((N_ODD, LRU_WIDTH), 0.02)
    cd_lru_wa = nrm((N_ODD, LRU_BLOCKS, LRU_BLOCK_DIM, LRU_BLOCK_DIM), LRU_BLOCK_DIM ** -0.5)
    cd_lru_ba = nrm((N_ODD, LRU_WIDTH), 0.02)
    cd_lru_wx = nrm((N_ODD, LRU_BLOCKS, LRU_BLOCK_DIM, LRU_BLOCK_DIM), LRU_BLOCK_DIM ** -0.5)
    cd_lru_bx = nrm((N_ODD, LRU_WIDTH), 0.02)
    a_c = unif((N_ODD, LRU_WIDTH), 0.9, 0.999)
    sig = a_c ** (1.0 / LRU_C)
    cd_lru_lambda = jnp.log(sig) - jnp.log1p(-sig)
    cd_w_out = nrm((N_ODD, CD_OUT, D), CD_OUT ** -0.5)
    final_norm = gain((D,))
    return {'x': x, 'c': c, 'positions': positions, 'router_w': router_w, 'router_bias': router_bias,
            'ada_w': ada_w, 'ada_b': ada_b, 'norm_mix': norm_mix, 'norm_ffn': norm_ffn,
            'moe_w1': moe_w1, 'moe_w3': moe_w3, 'moe_w2': moe_w2,
            'ab_w_in': ab_w_in, 'ab_sinks': ab_sinks, 'ab_conv_w': ab_conv_w, 'ab_conv_b': ab_conv_b,
            'ab_conv_ln_g': ab_conv_ln_g, 'ab_conv_ln_b': ab_conv_ln_b, 'ab_w_out': ab_w_out,
            'cd_w_in': cd_w_in, 'cd_shift_mu': cd_shift_mu, 'cd_w0': cd_w0, 'cd_w2': cd_w2,
            'cd_a0': cd_a0, 'cd_a2': cd_a2, 'cd_g2': cd_g2, 'cd_k_k': cd_k_k, 'cd_k_a': cd_k_a,
            'cd_r_k': cd_r_k, 'cd_ln_x_g': cd_ln_x_g, 'cd_ln_x_b': cd_ln_x_b,
            'cd_lru_conv_w': cd_lru_conv_w, 'cd_lru_conv_b': cd_lru_conv_b,
            'cd_lru_wa': cd_lru_wa, 'cd_lru_ba': cd_lru_ba, 'cd_lru_wx': cd_lru_wx, 'cd_lru_bx': cd_lru_bx,
            'cd_lru_lambda': cd_lru_lambda, 'cd_w_out': cd_w_out, 'final_norm': final_norm}


def reference(x, c, positions, router_w, router_bias, ada_w, ada_b, norm_mix, norm_ffn,
              moe_w1, moe_w3, moe_w2, ab_w_in, ab_sinks, ab_conv_w, ab_conv_b, ab_conv_ln_g,
              ab_conv_ln_b, ab_w_out, cd_w_in, cd_shift_mu, cd_w0, cd_w2, cd_a0, cd_a2, cd_g2,
              cd_k_k, cd_k_a, cd_r_k, cd_ln_x_g, cd_ln_x_b, cd_lru_conv_w, cd_lru_conv_b,
              cd_lru_wa, cd_lru_ba, cd_lru_wx, cd_lru_bx, cd_lru_lambda, cd_w_out, final_norm):
    c_act = jax.nn.silu(c)
    for layer in range(DEPTH):
        j = layer // 2
        mod = c_act @ ada_w[layer] + ada_b[layer]
        shift_m, scale_m, gate_m, shift_f, scale_f, gate_f = jnp.split(mod, 6, axis=-1)
        h = modulate(rms_norm(x, norm_mix[layer]), shift_m, scale_m)
        if layer % 2 == 0:
            y = mixer_ab(h, positions, ab_w_in[j], ab_sinks[j], ab_conv_w[j], ab_conv_b[j],
                         ab_conv_ln_g[j], ab_conv_ln_b[j], ab_w_out[j])
        else:
            y = mixer_cd(h, cd_w_in[j], cd_shift_mu[j], cd_w0[j], cd_w2[j], cd_a0[j], cd_a2[j],
                         cd_g2[j], cd_k_k[j], cd_k_a[j], cd_r_k[j], cd_ln_x_g[j], cd_ln_x_b[j],
                         cd_lru_conv_w[j], cd_lru_conv_b[j], cd_lru_wa[j], cd_lru_ba[j],
                         cd_lru_wx[j], cd_lru_bx[j], cd_lru_lambda[j], cd_w_out[j])
        x = x + gate_m[:, None, :] * y
        h = modulate(rms_norm(x, norm_ffn[layer]), shift_f, scale_f)
        x = x + gate_f[:, None, :] * moe_ffn(h, router_w, router_bias, moe_w1[layer], moe_w3[layer], moe_w2[layer])
    return rms_norm(x, final_norm)
```

```python
import numpy as np
from contextlib import ExitStack
import concourse.bass as bass
import concourse.mybir as mybir
from concourse.bass_utils import run_bass_kernel_spmd

F32 = mybir.dt.float32
BF16 = mybir.dt.bfloat16
I32 = mybir.dt.int32
AF = mybir.ActivationFunctionType
ALU = mybir.AluOpType
AX = mybir.AxisListType
NCORES = 8


class V:
    def __init__(self, ap, key):
        self.ap = ap
        self.key = key

    def __getitem__(self, idx):
        return V(self.ap[idx], self.key)


def _u(x):
    return x.ap if isinstance(x, V) else x


def _key(x):
    if isinstance(x, str):
        return x
    if isinstance(x, V):
        return x.key
    t = getattr(x, "tensor", x)
    return t.name


class Prog:
    ENG = ("tensor", "vector", "scalar", "gpsimd", "sync")
    NLANES = 8

    def __init__(self, name="k"):
        self.nc = bass.Bass("TRN2", target_bir_lowering=False)
        self.es = ExitStack()
        self.ges = ExitStack()
        self.stage_no = 0
        self._reset()

    def _reset(self):
        self.streams = {e: [] for e in self.ENG}
        self.count = {e: 0 for e in self.ENG}
        self.known = {e: {} for e in self.ENG}
        self.last_w = {}
        self.readers = {}
        self.lane_cnt = [0] * self.NLANES
        self.lane_next = 0
        self.out_tokens = []
        self.ntiles = 0

    def dram_in(self, name, shape, dtype=F32):
        return self.nc.dram_tensor(name, list(shape), dtype, kind="ExternalInput").ap()

    def dram_out(self, name, shape, dtype=F32):
        return self.nc.dram_tensor(name, list(shape), dtype, kind="ExternalOutput").ap()

    def dram_tmp(self, name, shape, dtype=F32):
        return self.nc.dram_tensor(name, list(shape), dtype, kind="Internal").ap()

    def sb(self, name, shape, dtype=F32):
        return self.es.enter_context(self.nc.sbuf_tensor("g%d_%s" % (self.stage_no, name), list(shape), dtype))

    def ps(self, name, shape, dtype=F32):
        return self.es.enter_context(self.nc.psum_tensor("g%d_%s" % (self.stage_no, name), list(shape), dtype))

    def _deps(self, eng, r, w):
        deps = set()
        for k in r:
            k = _key(k)
            if k in self.last_w:
                deps.add(self.last_w[k])
        for k in w:
            k = _key(k)
            if k in self.last_w:
                deps.add(self.last_w[k])
            for t in self.readers.get(k, ()):
                deps.add(t)
        need = {}
        for (s, v) in deps:
            if s == "tensor" and eng == "tensor":
                continue
            if self.known[eng].get(s, 0) < v:
                need[s] = max(need.get(s, 0), v)
        for s, v in need.items():
            self.known[eng][s] = v
        return list(need.items())

    def _commit(self, tok, r, w):
        for k in w:
            k = _key(k)
            self.last_w[k] = tok
            self.readers[k] = []
        for k in r:
            k = _key(k)
            self.readers.setdefault(k, []).append(tok)

    def op(self, eng, fn, r=(), w=()):
        waits = self._deps(eng, r, w)
        self.count[eng] += 1
        tok = (eng, self.count[eng])
        self.known[eng][eng] = max(self.known[eng].get(eng, 0), 0)
        self.streams[eng].append((waits, fn, (eng, 1)))
        self._commit(tok, r, w)
        return tok

    def dma(self, out, in_, r=(), w=(), q="sync", is_out=False, **kw):
        lane = self.lane_next
        self.lane_next = (self.lane_next + 1) % self.NLANES
        s = "lane%d" % lane
        waits = self._deps(q, r, w)
        prev = self.lane_cnt[lane]
        if prev > 0 and self.known[q].get(s, 0) < prev:
            waits.append((s, prev))
            self.known[q][s] = prev
        self.lane_cnt[lane] += 16
        tok = (s, self.lane_cnt[lane])
        self.streams[q].append((waits, (lambda e, o=out, i=in_, kw=kw: e.dma_start(out=o, in_=i, **kw)), (s, 16)))
        self._commit(tok, r, w)
        if is_out:
            self.out_tokens.append(tok)
        return tok

    def mm(self, out, lhsT, rhs, start=True, stop=True, r=None, w=None, **kw):
        r = [lhsT, rhs] if r is None else r
        w = [out] if w is None else w
        return self.op("tensor", lambda e: e.matmul(_u(out), _u(lhsT), _u(rhs), start=start, stop=stop, **kw), r=r, w=w)

    def tr(self, out, in_, ident):
        return self.op("tensor", lambda e: e.transpose(_u(out), _u(in_), _u(ident)), r=[in_, ident], w=[out])

    def act(self, out, in_, func, bias=None, scale=None, accum_out=None, eng="scalar", extra_r=()):
        kw = {}
        r = [in_] + list(extra_r)
        w = [out]
        if bias is not None:
            kw["bias"] = _u(bias)
            if not isinstance(bias, (int, float)):
                r.append(bias)
        if scale is not None:
            kw["scale"] = _u(scale)
            if not isinstance(scale, (int, float)):
                r.append(scale)
        if accum_out is not None:
            kw["accum_out"] = _u(accum_out)
            w.append(accum_out)
        return self.op("scalar", lambda e: e.activation(_u(out), _u(in_), func, **kw), r=r, w=w)

    def tt(self, out, in0, in1, op, eng="vector"):
        return self.op(eng, lambda e: e.tensor_tensor(_u(out), _u(in0), _u(in1), op), r=[in0, in1], w=[out])

    def ts(self, out, in0, s1, s2, op0, op1=None, eng="vector", accum_out=None):
        r = [in0] + [s for s in (s1, s2) if s is not None and not isinstance(s, (int, float))]
        w = [out] + ([accum_out] if accum_out is not None else [])
        kw = {}
        if op1 is not None:
            kw["op1"] = op1
        if accum_out is not None:
            kw["accum_out"] = _u(accum_out)
        return self.op(eng, lambda e: e.tensor_scalar(_u(out), _u(in0), _u(s1), _u(s2), op0, **kw), r=r, w=w)

    def stt(self, out, in0, scalar, in1, op0, op1, eng="vector"):
        r = [in0, in1] + ([] if isinstance(scalar, (int, float)) else [scalar])
        return self.op(eng, lambda e: e.scalar_tensor_tensor(_u(out), _u(in0), _u(scalar), _u(in1), op0, op1), r=r, w=[out])

    def copy(self, out, in_, eng="vector"):
        if eng == "scalar":
            return self.op("scalar", lambda e: e.copy(_u(out), _u(in_)), r=[in_], w=[out])
        return self.op(eng, lambda e: e.tensor_copy(_u(out), _u(in_)), r=[in_], w=[out])

    def memset(self, out, val, eng="vector"):
        return self.op(eng, lambda e: e.memset(out, val), r=[], w=[out])

    def recip(self, out, in_):
        return self.op("vector", lambda e: e.reciprocal(out, in_), r=[in_], w=[out])

    def reduce(self, out, in_, op, axis=AX.X, eng="vector"):
        return self.op(eng, lambda e: e.tensor_reduce(out, in_, axis, op), r=[in_], w=[out])

    def end_stage(self):
        self.build(final=False)
        self.stage_no += 1
        self.es = ExitStack()
        self._reset()

    def build(self, final=True):
        nc = self.nc
        fin = {}
        for (s, v) in self.out_tokens:
            fin[s] = max(fin.get(s, 0), v)
        sem_names = set(self.ENG)
        for i in range(self.NLANES):
            sem_names.add("lane%d" % i)
        sems = {}
        for s in sorted(sem_names):
            sems[s] = nc.alloc_semaphore(name="s%d_%s" % (self.stage_no, s))
        streams = self.streams
        block = self.es.enter_context(nc.Block())

        def emit(eng_name):
            def body(e):
                for waits, fn, (s, n) in streams[eng_name]:
                    for (ws, wv) in waits:
                        e.wait_ge(sems[ws], wv)
                    ins = fn(e)
                    ins.then_inc(sems[s], n)
                if eng_name == "sync":
                    for s, v in fin.items():
                        e.wait_ge(sems[s], v)
            return body

        block.tensor(emit("tensor"))
        block.vector(emit("vector"))
        block.scalar(emit("scalar"))
        block.gpsimd(emit("gpsimd"))
        block.sync(emit("sync"))
        self.es.close()
        nc.clear_and_free_semaphores(list(sems.values()))
        nc.all_engine_barrier()
        return nc


def run(P, in_maps, trace=False):
    P.ges.close()
    nc = P.nc
    res = run_bass_kernel_spmd(nc, in_maps, core_ids=list(range(len(in_maps))), trace=trace)
    return res


def stage_lin(P, xT, w, yT, K, M, T, mode="plain", fp32=False, in_silu=False, TS=2048,
              bias_d=None, gate_d=None, rT=None):
    KC, MC = K // 128, M // 128
    TS = min(TS, T)
    xv = xT.rearrange("(kc p) t -> p kc t", p=128)
    wv = w.rearrange("(kc p) m -> p kc m", p=128)
    DT = F32 if fp32 else BF16
    if mode == "bias":
        bias = P.sb("bias_s", [128, MC])
        P.dma(bias[:], bias_d, w=[bias])
    if mode == "resid":
        gate = P.sb("gate_s", [128, MC])
        P.dma(gate[:], gate_d, w=[gate], allow_slow_non_contiguous=True)
    TT = min(512, TS)
    xb = P.sb("xb", [128, KC, TS], DT)
    xst = [P.sb("xst%d" % i, [128, TS]) for i in range(2)]
    wst = [P.sb("wst%d" % i, [128, KC, 128]) for i in range(2)]
    wb = [P.sb("wb%d" % i, [128, KC, 128], DT) for i in range(2)] if not fp32 else wst
    acc = [P.ps("acc%d" % i, [128, TT]) for i in range(4)]
    ot = [P.sb("ot%d" % i, [128, TT]) for i in range(3)]
    rt = [P.sb("rt%d" % i, [128, TT]) for i in range(2)]
    cnt = 0
    wcnt = 0
    for t0 in range(0, T, TS):
        tl = min(TS, T - t0)
        tt_ = min(TT, tl)
        for kc in range(KC):
            s = xst[kc % 2]
            P.dma(s[:, :tl], xv[:, kc, t0:t0 + tl], w=[s])
            if in_silu:
                P.act(xb[:, kc, :tl], s[:, :tl], AF.Silu)
            else:
                P.copy(xb[:, kc, :tl], s[:, :tl], eng="vector")
        for mc in range(MC):
            wi = wcnt % 2
            wcnt += 1
            P.dma(wst[wi][:], wv[:, :, mc * 128:(mc + 1) * 128], w=[wst[wi]])
            if not fp32:
                P.copy(wb[wi][:], wst[wi][:], eng="gpsimd")
            for tt in range(0, tl, TT):
                tt_ = min(TT, tl - tt)
                a = acc[cnt % 4]
                o = ot[cnt % 3]
                for kc in range(KC):
                    P.mm(a[:, :tt_], wb[wi][:, kc, :], xb[:, kc, tt:tt + tt_], start=(kc == 0), stop=(kc == KC - 1))
                if mode == "plain":
                    P.act(o[:, :tt_], a[:, :tt_], AF.Copy)
                elif mode == "bias":
                    P.act(o[:, :tt_], a[:, :tt_], AF.Identity, bias=bias[:, mc:mc + 1])
                else:
                    rr = rt[cnt % 2]
                    P.dma(rr[:, :tt_], rT[mc * 128:(mc + 1) * 128, t0 + tt:t0 + tt + tt_], w=[rr])
                    P.stt(o[:, :tt_], a[:, :tt_], gate[:, mc:mc + 1], rr[:, :tt_], ALU.mult, ALU.add)
                P.dma(yT[mc * 128:(mc + 1) * 128, t0 + tt:t0 + tt + tt_], o[:, :tt_], r=[o], q="gpsimd", is_out=True)
                cnt += 1
    P.end_stage()


def stage_normfm(P, xT, modT, g_d, shift_row, scale_row, hT, T, D=2048, eps=1e-6, TT=512):
    KC = D // 128
    g = P.sb("g_s", [128, KC]); P.dma(g[:], g_d, w=[g])
    A = P.sb("A_s", [128, KC]); B = P.sb("B_s", [128, KC])
    if modT is None:
        P.copy(A[:], g[:])
        P.memset(B[:], 0.0)
    else:
        mv = modT.rearrange("(c p) o -> p (c o)", p=128)
        P.dma(A[:], mv[:, scale_row * KC:(scale_row + 1) * KC], w=[A], allow_slow_non_contiguous=True)
        P.dma(B[:], mv[:, shift_row * KC:(shift_row + 1) * KC], w=[B], allow_slow_non_contiguous=True)
        P.stt(A[:], A[:], 1.0, g[:], ALU.add, ALU.mult)
    ones = P.sb("ones_s", [128, 128]); P.memset(ones[:], 1.0 / D)
    xs = [P.sb("xs%d" % i, [128, KC, TT]) for i in range(2)]
    sq = [P.sb("sq%d" % i, [128, TT]) for i in range(2)]
    ps = [P.ps("ps%d" % i, [128, TT]) for i in range(2)]
    rstd = [P.sb("rstd%d" % i, [128, TT]) for i in range(2)]
    tm = [P.sb("tm%d" % i, [128, TT]) for i in range(2)]
    ho = [P.sb("ho%d" % i, [128, TT]) for i in range(3)]
    xv = xT.rearrange("(kc p) t -> p kc t", p=128)
    n = 0
    k = 0
    for t0 in range(0, T, TT):
        tl = min(TT, T - t0)
        i = n % 2; n += 1
        x = xs[i]
        for kc in range(KC):
            P.dma(x[:, kc, :tl], xv[:, kc, t0:t0 + tl], w=[x])
        for kc in range(KC):
            q = sq[kc % 2]
            P.act(q[:, :tl], x[:, kc, :tl], AF.Square)
            P.mm(ps[i][:, :tl], ones[:], q[:, :tl], start=(kc == 0), stop=(kc == KC - 1))
        P.ts(rstd[i][:, :tl], ps[i][:, :tl], eps, None, ALU.add)
        P.act(rstd[i][:, :tl], rstd[i][:, :tl], AF.Sqrt)
        P.recip(rstd[i][:, :tl], rstd[i][:, :tl])
        for kc in range(KC):
            t_ = tm[kc % 2]
            o = ho[k % 3]; k += 1
            P.tt(t_[:, :tl], x[:, kc, :tl], rstd[i][:, :tl], ALU.mult)
            P.act(o[:, :tl], t_[:, :tl], AF.Identity, scale=A[:, kc:kc + 1], bias=B[:, kc:kc + 1])
            P.dma(hT[kc * 128:(kc + 1) * 128, t0:t0 + tl], o[:, :tl], r=[o], q="gpsimd", is_out=True)
    P.end_stage()


def stage_att(P, zT, pos_bc, invf_d, sgn_d, maskg_d, mask0_d, sink_d, ident_d, attT, TQ, NH=16, NKV=4):
    import math
    TK = TQ + 128
    NB = TQ // 128
    QC = NH // 2
    QOFF, KOFF, VOFF = 0, NH * 64, NH * 64 + NKV * 64
    invf = P.sb("invf_s", [128, 1]); P.dma(invf[:], invf_d, w=[invf])
    sgn = P.sb("sgn_s", [128, 1]); P.dma(sgn[:], sgn_d, w=[sgn])
    maskg = P.sb("maskg_s", [128, 256]); P.dma(maskg[:], maskg_d, w=[maskg])
    mask0 = P.sb("mask0_s", [128, 256]); P.dma(mask0[:], mask0_d, w=[mask0])
    sink = P.sb("sink_s", [128, NH]); P.dma(sink[:], sink_d, w=[sink])
    identf = P.sb("identf", [128, 128]); P.dma(identf[:], ident_d, w=[identf])
    ident = P.sb("identb", [128, 128], BF16); P.copy(ident[:], identf[:])

    bufA = P.sb("bufA", [128, TK])
    bufB = P.sb("bufB", [128, TK])
    S = P.sb("S", [128, TK])
    C = P.sb("C", [128, TK])
    posi = bufB[:].bitcast(I32)
    P.dma(posi, pos_bc, w=[bufB])
    ang = bufA
    P.copy(ang[:], posi)
    P.ts(ang[:], ang[:], invf[:, 0:1], None, ALU.mult)
    TWO_PI = 2.0 * math.pi
    C1 = 6.28125
    C2 = TWO_PI - C1
    Wt = TK // 4
    for q4 in range(0, TK, Wt):
        wl = min(Wt, TK - q4)
        kf = bufB[:, 0:wl]
        ki = bufB[:, Wt:Wt + wl].bitcast(I32)
        yy = bufB[:, 2 * Wt:2 * Wt + wl]
        mw = bufB[:, 3 * Wt:3 * Wt + wl]
        for which, dst in ((0, S), (1, C)):
            src = ang[:, q4:q4 + wl]
            if which == 1:
                P.ts(yy, src, math.pi / 2.0, None, ALU.add)
                src = yy
            P.ts(kf, src, 1.0 / TWO_PI, None, ALU.mult)
            P.copy(ki, kf)
            P.copy(kf, ki)
            P.stt(yy, kf, -C1, src, ALU.mult, ALU.add)
            P.stt(yy, kf, -C2, yy, ALU.mult, ALU.add)
            P.ts(mw, yy, math.pi, -TWO_PI, ALU.is_gt, ALU.mult)
            P.tt(yy, yy, mw, ALU.add)
            P.ts(mw, yy, -math.pi, TWO_PI, ALU.is_lt, ALU.mult)
            P.tt(yy, yy, mw, ALU.add)
            P.ts(yy, yy, math.pi, -math.pi, ALU.min, ALU.max)
            if which == 0:
                P.act(dst[:, q4:q4 + wl], yy, AF.Sin, scale=sgn[:, 0:1])
            else:
                P.act(dst[:, q4:q4 + wl], yy, AF.Sin)

    kr = P.sb("kr", [128, NKV, TK], BF16)
    vb = P.sb("vb", [128, NB + 1, NKV * 64], BF16)
    for g in range(NKV):
        r0 = KOFF + g * 64
        for dup in range(2):
            P.dma(bufA[dup * 64:(dup + 1) * 64, :], zT[r0:r0 + 64, :], w=[bufA])
            for half in range(2):
                P.dma(bufB[dup * 64 + half * 32:dup * 64 + half * 32 + 32, :],
                      zT[r0 + (1 - half) * 32:r0 + (1 - half) * 32 + 32, :], w=[bufB])
        P.tt(bufA[:], bufA[:], C[:], ALU.mult)
        P.tt(bufB[:], bufB[:], S[:], ALU.mult, eng="gpsimd")
        P.tt(kr[:, g, :], bufA[:], bufB[:], ALU.add)
    ps_s = [P.ps("ps_s%d" % i, [128, 256]) for i in range(2)]
    VC = NKV * 64 // 128
    vt = [P.sb("vt%d" % i, [128, VC, 128]) for i in range(2)]
    vv = zT[VOFF:VOFF + NKV * 64, :].rearrange("(c p) t -> p c t", p=128)
    for blk in range(NB + 1):
        v_ = vt[blk % 2]
        P.dma(v_[:], vv[:, :, blk * 128:(blk + 1) * 128], w=[v_])
        for c in range(VC):
            P.tr(ps_s[blk % 2][:, c * 128:(c + 1) * 128], v_[:, c, :], identf[:])
        P.copy(vb[:, blk, :], ps_s[blk % 2][:, 0:VC * 128], eng="scalar")

    GS = min(512, TQ)
    qa = [P.sb("qa%d" % i, [128, GS]) for i in range(2)]
    qb = [P.sb("qb%d" % i, [128, GS]) for i in range(2)]
    qr = [P.sb("qr%d" % i, [128, QC, GS], BF16) for i in range(2)]
    ps_t = [P.ps("ps_t%d" % i, [128, 2, 128], BF16) for i in range(2)]
    ps_o = [P.ps("ps_o%d" % i, [128, 128]) for i in range(2)]
    sm = [P.sb("sm%d" % i, [128, 256]) for i in range(2)]
    pe_ = [P.sb("pe%d" % i, [128, 256]) for i in range(2)]
    pb = [P.sb("pb%d" % i, [128, 256], BF16) for i in range(2)]
    pT = [P.sb("pT%d" % i, [128, 2, 128], BF16) for i in range(2)]
    st = [[P.sb("st%s%d" % (nm, i), [128, 1]) for i in range(2)] for nm in ("mx", "ng", "rs", "es")]
    ao = [P.sb("ao%d" % i, [128, QC, 128]) for i in range(2)]
    attv = attT.rearrange("(c p) t -> p c t", p=128)
    u = 0
    n = 0
    for g0 in range(0, TQ, GS):
        qrg = qr[(g0 // GS) % 2]
        for c in range(QC):
            a, b = qa[n % 2], qb[n % 2]; n += 1
            r0 = QOFF + c * 128
            P.dma(a[:], zT[r0:r0 + 128, 128 + g0:128 + g0 + GS], w=[a])
            for hh in range(2):
                for half in range(2):
                    P.dma(b[hh * 64 + half * 32:hh * 64 + half * 32 + 32, :],
                          zT[r0 + hh * 64 + (1 - half) * 32:r0 + hh * 64 + (1 - half) * 32 + 32, 128 + g0:128 + g0 + GS], w=[b])
            P.tt(a[:], a[:], C[:, 128 + g0:128 + g0 + GS], ALU.mult)
            P.tt(b[:], b[:], S[:, 128 + g0:128 + g0 + GS], ALU.mult, eng="gpsimd")
            P.tt(qrg[:, c, :], a[:], b[:], ALU.add)
        for jl in range(GS // 128):
            j = g0 // 128 + jl
            msk = mask0 if j == 0 else maskg
            aoj = ao[j % 2]
            for c in range(QC):
                po = ps_o[c % 2]
                for hh in range(2):
                    h = 2 * c + hh
                    g = h // (NH // NKV)
                    i2 = u % 2; u += 1
                    pb0 = hh * 64
                    mx, ng, rs, es = st[0][i2], st[1][i2], st[2][i2], st[3][i2]
                    P.mm(ps_s[i2][:], qrg[pb0:pb0 + 64, c, jl * 128:(jl + 1) * 128], kr[pb0:pb0 + 64, g, j * 128:j * 128 + 256])
                    P.stt(sm[i2][:], ps_s[i2][:], 0.125, msk[:], ALU.mult, ALU.add)
                    P.reduce(mx[:], sm[i2][:], ALU.max)
                    P.ts(ng[:], mx[:], sink[:, h:h + 1], -1.0, ALU.max, ALU.mult)
                    P.act(pe_[i2][:], sm[i2][:], AF.Exp, bias=ng[:, 0:1], accum_out=rs[:])
                    P.act(es[:], sink[:, h:h + 1], AF.Exp, bias=ng[:, 0:1])
                    P.tt(rs[:], rs[:], es[:], ALU.add)
                    P.recip(rs[:], rs[:])
                    P.ts(pb[i2][:], pe_[i2][:], rs[:, 0:1], None, ALU.mult)
                    for kc in range(2):
                        P.tr(ps_t[i2][:, kc, :], pb[i2][:, kc * 128:(kc + 1) * 128], ident[:])
                    P.copy(pT[i2][:], ps_t[i2][:], eng="scalar")
                    for kc in range(2):
                        P.mm(po[pb0:pb0 + 64, :], vb[:, j + kc, g * 64:(g + 1) * 64], pT[i2][:, kc, :],
                             start=(kc == 0), stop=(kc == 1))
                P.copy(aoj[:, c, :], po[:], eng="vector")
            P.dma(attv[:, :, j * 128:(j + 1) * 128], aoj[:], r=[aoj], q="gpsimd", is_out=True)
    P.end_stage()


def stage_conv(P, valT, gateT, cw_d, cb_d, lg_d, lb_d, flag_d, outT, TQ, CH=1024, W=31, ln_eps=1e-5, TT=512):
    CC = CH // 128
    HL = W - 1
    TT = min(TT, TQ)
    cw = P.sb("cw_s", [128, CC, W]); P.dma(cw[:], cw_d, w=[cw])
    cb = P.sb("cb_s", [128, CC]); P.dma(cb[:], cb_d, w=[cb])
    lg = P.sb("lg_s", [128, CC]); P.dma(lg[:], lg_d, w=[lg])
    lb = P.sb("lb_s", [128, CC]); P.dma(lb[:], lb_d, w=[lb])
    flag = P.sb("flag_s", [128, 1]); P.dma(flag[:], flag_d, w=[flag])
    ones = P.sb("ones_s", [128, 128]); P.memset(ones[:], 1.0 / CH)
    va = [P.sb("va%d" % i, [128, TT + HL]) for i in range(2)]
    ga = [P.sb("ga%d" % i, [128, TT + HL]) for i in range(2)]
    a1 = [P.sb("a1_%d" % i, [128, TT]) for i in range(2)]
    yc = [P.sb("yc%d" % c, [128, TT]) for c in range(CC)]
    sq = [P.sb("sq%d" % i, [128, TT]) for i in range(2)]
    ps_m = P.ps("ps_m", [128, TT])
    ps_q = P.ps("ps_q", [128, TT])
    mean = P.sb("mean", [128, TT])
    rstd = P.sb("rstd", [128, TT])
    ot = [P.sb("cot%d" % i, [128, TT]) for i in range(2)]
    n = 0
    for t0 in range(0, TQ, TT):
        for c in range(CC):
            i2 = n % 2; n += 1
            v, g = va[i2], ga[i2]
            P.dma(v[:], valT[c * 128:(c + 1) * 128, t0:t0 + TT + HL], w=[v])
            P.dma(g[:], gateT[c * 128:(c + 1) * 128, t0:t0 + TT + HL], w=[g])
            P.act(g[:], g[:], AF.Sigmoid)
            P.tt(v[:], v[:], g[:], ALU.mult)
            if t0 == 0:
                P.ts(v[:, 0:HL], v[:, 0:HL], flag[:, 0:1], None, ALU.mult)
            P.ts(a1[i2][:], v[:, 0:TT], cw[:, c, 0:1], None, ALU.mult)
            for j in range(1, W):
                P.stt(a1[i2][:], v[:, j:j + TT], cw[:, c, j:j + 1], a1[i2][:], ALU.mult, ALU.add)
            P.ts(yc[c][:], a1[i2][:], cb[:, c:c + 1], None, ALU.add)
            s = sq[c % 2]
            P.act(s[:], yc[c][:], AF.Square)
            P.mm(ps_m[:], ones[:], yc[c][:], start=(c == 0), stop=(c == CC - 1))
            P.mm(ps_q[:], ones[:], s[:], start=(c == 0), stop=(c == CC - 1))
        P.copy(mean[:], ps_m[:])
        P.tt(rstd[:], mean[:], mean[:], ALU.mult)
        P.tt(rstd[:], ps_q[:], rstd[:], ALU.subtract)
        P.ts(rstd[:], rstd[:], ln_eps, None, ALU.add)
        P.act(rstd[:], rstd[:], AF.Sqrt)
        P.recip(rstd[:], rstd[:])
        for c in range(CC):
            o = ot[c % 2]
            P.tt(yc[c][:], yc[c][:], mean[:], ALU.subtract)
            P.tt(yc[c][:], yc[c][:], rstd[:], ALU.mult, eng="gpsimd")
            P.act(o[:], yc[c][:], AF.Silu, scale=lg[:, c:c + 1], bias=lb[:, c:c + 1])
            P.dma(outT[c * 128:(c + 1) * 128, t0:t0 + TT], o[:], r=[o], q="gpsimd", is_out=True)
    P.end_stage()


def stage_route(P, hT, rw_d, rb_d, ident_d, gatesT, T, D=2048, E=16, G=4, TS=1024):
    KC = D // 128
    EG = E // G
    TS = min(TS, T)
    hv = hT.rearrange("(kc p) t -> p kc t", p=128)
    rw = P.sb("rw_s", [128, KC, E]); P.dma(rw[:], rw_d, w=[rw])
    rb = P.sb("rb_s", [128, E]); P.dma(rb[:], rb_d, w=[rb])
    identf = P.sb("identf", [128, 128]); P.dma(identf[:], ident_d, w=[identf])
    hs = [P.sb("hs%d" % i, [128, KC, TS]) for i in range(1)]
    ps = [P.ps("psl%d" % i, [128, E]) for i in range(2)]
    psT = [P.ps("psT%d" % i, [E, 128]) for i in range(2)]
    def t(name, shape):
        return [P.sb("%s%d" % (name, i), shape) for i in range(2)]
    lg, pr, sel, sel2, msk = t("lg", [128, E]), t("pr", [128, E]), t("sel", [128, E]), t("sel2", [128, E]), t("msk", [128, E])
    mx, sm, m1, m2, gs, gm, gsel = t("mx", [128, 1]), t("sm", [128, 1]), t("m1", [128, G]), t("m2", [128, G]), t("gs", [128, G]), t("gm", [128, 1]), t("gsel", [128, G])
    og = t("og", [128, E])
    ogT = t("ogT", [E, 128])
    n = 0
    for t0 in range(0, T, TS):
        h = hs[0]
        for kc in range(KC):
            P.dma(h[:, kc, :], hv[:, kc, t0:t0 + TS], w=[h])
        for tt in range(0, TS, 128):
            i = n % 2; n += 1
            for kc in range(KC):
                P.mm(ps[i][:], h[:, kc, tt:tt + 128], rw[:, kc, :], start=(kc == 0), stop=(kc == KC - 1))
            P.copy(lg[i][:], ps[i][:])
            P.reduce(mx[i][:], lg[i][:], ALU.max)
            P.ts(mx[i][:], mx[i][:], -1.0, None, ALU.mult)
            P.act(pr[i][:], lg[i][:], AF.Exp, bias=mx[i][:, 0:1], accum_out=sm[i][:])
            P.recip(sm[i][:], sm[i][:])
            P.ts(pr[i][:], pr[i][:], sm[i][:, 0:1], None, ALU.mult)
            P.tt(sel[i][:], pr[i][:], rb[:], ALU.add)
            s3 = sel[i][:].rearrange("p (g e) -> p g e", g=G)
            s23 = sel2[i][:].rearrange("p (g e) -> p g e", g=G)
            k3 = msk[i][:].rearrange("p (g e) -> p g e", g=G)
            P.reduce(m1[i][:], s3, ALU.max)
            m1b = m1[i][:].unsqueeze(2).broadcast_to([128, G, EG])
            P.tt(s23, s3, m1b, ALU.is_equal)
            P.stt(sel2[i][:], sel2[i][:], -1e9, sel[i][:], ALU.mult, ALU.add)
            P.reduce(m2[i][:], s23, ALU.max)
            P.tt(gs[i][:], m1[i][:], m2[i][:], ALU.add)
            P.reduce(gm[i][:], gs[i][:], ALU.max)
            P.ts(gsel[i][:], gs[i][:], gm[i][:, 0:1], None, ALU.is_equal)
            m2b = m2[i][:].unsqueeze(2).broadcast_to([128, G, EG])
            P.tt(k3, s3, m2b, ALU.is_ge)
            gselb = gsel[i][:].unsqueeze(2).broadcast_to([128, G, EG])
            P.tt(k3, k3, gselb, ALU.mult)
            P.tt(og[i][:], pr[i][:], msk[i][:], ALU.mult)
            P.reduce(sm[i][:], og[i][:], ALU.add)
            P.recip(sm[i][:], sm[i][:])
            P.ts(og[i][:], og[i][:], sm[i][:, 0:1], None, ALU.mult)
            P.tr(psT[i][:], og[i][:], identf[:])
            P.copy(ogT[i][:], psT[i][:], eng="scalar")
            P.dma(gatesT[:, t0 + tt:t0 + tt + 128], ogT[i][:], r=[ogT[i]], q="gpsimd", is_out=True)
    P.end_stage()


def _swap_half(a):
    sh = a.shape
    a4 = a.reshape(sh[:-1] + (sh[-1] // 64, 2, 32))
    return np.ascontiguousarray(a4[..., ::-1, :]).reshape(sh)


def att_inmaps(q, k, v, pos, sinks, TQ):
    import math
    B, S, _ = q.shape
    per = S // TQ
    half = 32
    invf = (10000.0 ** (-np.arange(half, dtype=np.float32) / half)).astype(np.float32)
    invf128 = np.tile(invf, 4).reshape(128, 1).astype(np.float32)
    sgn = np.tile(np.concatenate([-np.ones(32), np.ones(32)]), 2).reshape(128, 1).astype(np.float32)
    qi = np.arange(128)[:, None]
    kj = np.arange(256)[None, :]
    dist = 128 + qi - kj
    valid = (dist >= 0) & (dist < 128)
    maskg = np.where(valid, 0.0, -1e30).astype(np.float32)
    mask_first = np.where(valid & (kj >= 128), 0.0, -1e30).astype(np.float32)
    ident = np.eye(128, dtype=np.float32)
    sink_bc = np.ascontiguousarray(np.broadcast_to(sinks[None, :], (128, sinks.shape[0]))).astype(np.float32)
    qs = _swap_half(q)
    ks = _swap_half(k)
    maps = []
    for b in range(B):
        for c in range(per):
            t0 = c * TQ
            def halo(a):
                if c == 0:
                    return np.concatenate([np.zeros((128,) + a.shape[2:], a.dtype), a[b, 0:TQ]], 0)
                return a[b, t0 - 128:t0 + TQ]
            kh, ksh, vh = halo(k), halo(ks), halo(v)
            if c == 0:
                ph = np.concatenate([np.zeros((128,), np.int32), pos[b, 0:TQ]])
            else:
                ph = pos[b, t0 - 128:t0 + TQ]
            def dup(a):
                aT = a.T.reshape(4, 64, -1)
                return np.ascontiguousarray(np.concatenate([aT, aT], 1).reshape(512, -1))
            maps.append({
                "qT": np.ascontiguousarray(q[b, t0:t0 + TQ].T), "qsT": np.ascontiguousarray(qs[b, t0:t0 + TQ].T),
                "kdT": dup(kh), "ksdT": dup(ksh), "vtm": np.ascontiguousarray(vh),
                "pos_bc": np.ascontiguousarray(np.broadcast_to(ph[None, :], (128, ph.shape[0]))).astype(np.int32),
                "invf": invf128, "sgn": sgn, "maskg": maskg, "mask0": mask_first if c == 0 else maskg,
                "sink_bc": sink_bc, "ident": ident})
    return maps


def pl(v):
    return np.ascontiguousarray(np.asarray(v, np.float32).reshape(-1, 128).T)


def conv_inmaps(u, conv_w, conv_b, ln_g, ln_b, TQ):
    B, S, _ = u.shape
    per = S // TQ
    CH = 1024
    W = conv_w.shape[0]
    cw = np.ascontiguousarray(conv_w.reshape(W, CH // 128, 128).transpose(2, 1, 0)).astype(np.float32)
    maps = []
    for b in range(B):
        for c in range(per):
            t0 = c * TQ
            if c == 0:
                seg = np.concatenate([np.zeros((W - 1, 2 * CH), np.float32), u[b, 0:TQ]], 0)
            else:
                seg = u[b, t0 - (W - 1):t0 + TQ]
            maps.append({"valT": np.ascontiguousarray(seg[:, :CH].T), "gateT": np.ascontiguousarray(seg[:, CH:].T),
                         "cw": cw, "cb": pl(conv_b), "lg": pl(ln_g), "lb": pl(ln_b)})
    return maps


def stage_moe(P, hT, xT, gatesT, w1, w3, w2, modT, gate_row, oT, T, D=2048, DE=1024, E=16, TS=512):
    KC, JC = D // 128, DE // 128
    TS = min(TS, T)
    hv = hT.rearrange("(kc p) t -> p kc t", p=128)
    mv = modT.rearrange("(c p) o -> p (c o)", p=128)
    gf = P.sb("gf_s", [128, KC]); P.dma(gf[:], mv[:, gate_row * KC:(gate_row + 1) * KC], w=[gf], allow_slow_non_contiguous=True)
    hb = P.sb("hb", [128, KC, TS], BF16)
    hst = [P.sb("hst%d" % i, [128, TS]) for i in range(2)]
    yacc = [P.sb("yacc%d" % f, [128, TS]) for f in range(KC)]
    hid = [P.sb("hid%d" % j, [128, TS], BF16) for j in range(JC)]
    ge = [P.sb("ge%d" % i, [128, TS]) for i in range(2)]
    wst = [P.sb("wst%d" % i, [128, KC, 128]) for i in range(4)]
    wbf = [P.sb("wbf%d" % i, [128, KC, 128], BF16) for i in range(4)]
    w2st = [P.sb("w2st%d" % i, [128, JC, 128]) for i in range(2)]
    w2bf = [P.sb("w2bf%d" % i, [128, JC, 128], BF16) for i in range(2)]
    sa = [P.sb("sa%d" % i, [128, TS]) for i in range(2)]
    ps_a = [P.ps("ps_a%d" % i, [128, TS]) for i in range(2)]
    ps_b = [P.ps("ps_b%d" % i, [128, TS]) for i in range(2)]
    ps_y = [P.ps("ps_y%d" % i, [128, TS]) for i in range(2)]
    xt = [P.sb("xt%d" % i, [128, TS]) for i in range(2)]
    n1 = n2 = 0
    for t0 in range(0, T, TS):
        for kc in range(KC):
            s = hst[kc % 2]
            P.dma(s[:], hv[:, kc, t0:t0 + TS], w=[s])
            P.copy(hb[:, kc, :], s[:], eng="vector")
        for e in range(E):
            g = ge[e % 2]
            P.dma(g[:], gatesT[e:e + 1, t0:t0 + TS].partition_broadcast(128), w=[g])
            for jc in range(JC):
                i = n1 % 2; n1 += 1
                P.dma(wst[2 * i][:], w1[e, jc], w=[wst[2 * i]])
                P.dma(wst[2 * i + 1][:], w3[e, jc], w=[wst[2 * i + 1]])
                P.copy(wbf[2 * i][:], wst[2 * i][:], eng="gpsimd")
                P.copy(wbf[2 * i + 1][:], wst[2 * i + 1][:], eng="scalar")
                for kc in range(KC):
                    P.mm(ps_a[i][:], wbf[2 * i][:, kc, :], hb[:, kc, :], start=(kc == 0), stop=(kc == KC - 1))
                for kc in range(KC):
                    P.mm(ps_b[i][:], wbf[2 * i + 1][:, kc, :], hb[:, kc, :], start=(kc == 0), stop=(kc == KC - 1))
                P.act(sa[i][:], ps_a[i][:], AF.Silu)
                P.tt(sa[i][:], sa[i][:], ps_b[i][:], ALU.mult)
                P.tt(hid[jc][:], sa[i][:], g[:], ALU.mult)
            for fc in range(KC):
                i = n2 % 2; n2 += 1
                P.dma(w2st[i][:], w2[e, fc], w=[w2st[i]])
                P.copy(w2bf[i][:], w2st[i][:], eng="gpsimd")
                for jc in range(JC):
                    P.mm(ps_y[i][:], w2bf[i][:, jc, :], hid[jc][:], start=(jc == 0), stop=(jc == JC - 1))
                if e == 0:
                    P.copy(yacc[fc][:], ps_y[i][:])
                else:
                    P.tt(yacc[fc][:], yacc[fc][:], ps_y[i][:], ALU.add)
        for fc in range(KC):
            x_ = xt[fc % 2]
            P.dma(x_[:], xT[fc * 128:(fc + 1) * 128, t0:t0 + TS], w=[x_])
            P.stt(x_[:], yacc[fc][:], gf[:, fc:fc + 1], x_[:], ALU.mult, ALU.add)
            P.dma(oT[fc * 128:(fc + 1) * 128, t0:t0 + TS], x_[:], r=[x_], q="gpsimd", is_out=True)
    P.end_stage()


def moe_weights_layout(w1, w3, w2):
    E, D, DE = w1.shape
    KC, JC = D // 128, DE // 128
    f = lambda w: np.ascontiguousarray(w.reshape(E, KC, 128, JC, 128).transpose(0, 3, 2, 1, 4))
    w2r = np.ascontiguousarray(w2.reshape(E, JC, 128, KC, 128).transpose(0, 3, 2, 1, 4))
    return f(w1), f(w3), w2r


def build_block0(TQ, D=2048):
    P = Prog()
    TK = TQ + 128
    AB_IN = 3584
    xhT = P.dram_in("xhT", [D, TK])
    cT = P.dram_in("cT", [D, 1])
    ada_w = P.dram_in("ada_w", [D, 6 * D])
    ada_b = P.dram_in("ada_b", [128, 6 * D // 128])
    g_mix = P.dram_in("g_mix", [128, D // 128])
    g_ffn = P.dram_in("g_ffn", [128, D // 128])
    w_in = P.dram_in("w_in", [D, AB_IN])
    w_out = P.dram_in("w_out", [D, D])
    pos_bc = P.dram_in("pos_bc", [128, TK], I32)
    invf = P.dram_in("invf", [128, 1])
    sgn = P.dram_in("sgn", [128, 1])
    maskg = P.dram_in("maskg", [128, 256])
    mask0 = P.dram_in("mask0", [128, 256])
    sink_bc = P.dram_in("sink_bc", [128, 16])
    ident = P.dram_in("ident", [128, 128])
    cw = P.dram_in("cw", [128, 8, 31])
    cb = P.dram_in("cb", [128, 8])
    lg = P.dram_in("lg", [128, 8])
    lb = P.dram_in("lb", [128, 8])
    flag = P.dram_in("flag", [128, 1])
    rw = P.dram_in("rw", [128, 16, 16])
    rb = P.dram_in("rb_bc", [128, 16])
    w1 = P.dram_in("w1r", [16, 8, 128, 16, 128])
    w3 = P.dram_in("w3r", [16, 8, 128, 16, 128])
    w2 = P.dram_in("w2r", [16, 16, 128, 8, 128])
    x2T = P.dram_out("x2T", [D, TQ])
    modT = P.dram_tmp("modT", [6 * D, 1])
    hT = P.dram_tmp("hT", [D, TK])
    zT = P.dram_tmp("zT", [AB_IN, TK])
    mixT = P.dram_tmp("mixT", [D, TQ])
    x1T = P.dram_tmp("x1T", [D, TQ])
    hfT = P.dram_tmp("hfT", [D, TQ])
    gatesT = P.dram_tmp("gatesT", [16, TQ])
    mv = modT.rearrange("(c p) o -> p (c o)", p=128)
    stage_lin(P, cT, ada_w, modT, K=D, M=6 * D, T=1, mode="bias", fp32=True, in_silu=True, bias_d=ada_b)
    stage_normfm(P, xhT, modT, g_mix, 0, 1, hT, TK)
    stage_lin(P, hT, w_in, zT, K=D, M=AB_IN, T=TK)
    stage_att(P, zT, pos_bc, invf, sgn, maskg, mask0, sink_bc, ident, mixT[0:1024, :], TQ)
    stage_conv(P, zT[1536:2560, 98:TK], zT[2560:3584, 98:TK], cw, cb, lg, lb, flag, mixT[1024:2048, :], TQ)
    stage_lin(P, mixT, w_out, x1T, K=D, M=D, T=TQ, mode="resid", gate_d=mv[:, 32:48], rT=xhT[:, 128:TK])
    stage_normfm(P, x1T, modT, g_ffn, 3, 4, hfT, TQ)
    stage_route(P, hfT, rw, rb, ident, gatesT, TQ)
    stage_moe(P, hfT, x1T, gatesT, w1, w3, w2, modT, 5, x2T, TQ)
    return P


def block0_inmaps(inp, TQ, layer=0):
    x = np.asarray(inp["x"], np.float32)
    B, S, D = x.shape
    per = S // TQ
    pos = np.asarray(inp["positions"], np.int32)
    j = layer // 2
    invf = (10000.0 ** (-np.arange(32, dtype=np.float32) / 32)).astype(np.float32)
    invf128 = np.tile(invf, 4).reshape(128, 1).astype(np.float32)
    sgn = np.tile(np.concatenate([-np.ones(32), np.ones(32)]), 2).reshape(128, 1).astype(np.float32)
    qi = np.arange(128)[:, None]
    kj = np.arange(256)[None, :]
    dist = 128 + qi - kj
    valid = (dist >= 0) & (dist < 128)
    maskg = np.where(valid, 0.0, -1e30).astype(np.float32)
    mask_first = np.where(valid & (kj >= 128), 0.0, -1e30).astype(np.float32)
    ident = np.eye(128, dtype=np.float32)
    sinks = np.asarray(inp["ab_sinks"][j], np.float32)
    sink_bc = np.ascontiguousarray(np.broadcast_to(sinks[None, :], (128, 16))).astype(np.float32)
    conv_w = np.asarray(inp["ab_conv_w"][j], np.float32).reshape(31, 1024)
    cw = np.ascontiguousarray(conv_w.reshape(31, 8, 128).transpose(2, 1, 0))
    w1r, w3r, w2r = moe_weights_layout(np.asarray(inp["moe_w1"][layer]), np.asarray(inp["moe_w3"][layer]), np.asarray(inp["moe_w2"][layer]))
    rwl = np.ascontiguousarray(np.asarray(inp["router_w"], np.float32).reshape(16, 128, 16).transpose(1, 0, 2))
    rbb = np.ascontiguousarray(np.broadcast_to(np.asarray(inp["router_bias"], np.float32)[None], (128, 16)))
    common = {
        "ada_w": np.ascontiguousarray(inp["ada_w"][layer]), "ada_b": pl(inp["ada_b"][layer]),
        "g_mix": pl(inp["norm_mix"][layer]), "g_ffn": pl(inp["norm_ffn"][layer]),
        "w_in": np.ascontiguousarray(inp["ab_w_in"][j]), "w_out": np.ascontiguousarray(inp["ab_w_out"][j]),
        "invf": invf128, "sgn": sgn, "maskg": maskg, "sink_bc": sink_bc, "ident": ident,
        "cw": cw, "cb": pl(inp["ab_conv_b"][j]), "lg": pl(inp["ab_conv_ln_g"][j]), "lb": pl(inp["ab_conv_ln_b"][j]),
        "rw": rwl, "rb_bc": rbb, "w1r": w1r, "w3r": w3r, "w2r": w2r,
    }
    maps = []
    for b in range(B):
        for c in range(per):
            t0 = c * TQ
            if c == 0:
                xh = np.concatenate([np.zeros((128, D), np.float32), x[b, 0:TQ]], 0)
                ph = np.concatenate([np.zeros((128,), np.int32), pos[b, 0:TQ]])
            else:
                xh = x[b, t0 - 128:t0 + TQ]
                ph = pos[b, t0 - 128:t0 + TQ]
            m = dict(common)
            m["xhT"] = np.ascontiguousarray(xh.T)
            m["cT"] = np.ascontiguousarray(np.asarray(inp["c"], np.float32)[b].reshape(D, 1))
            m["pos_bc"] = np.ascontiguousarray(np.broadcast_to(ph[None, :], (128, ph.shape[0]))).astype(np.int32)
            m["mask0"] = mask_first if c == 0 else maskg
            m["flag"] = np.full((128, 1), 0.0 if c == 0 else 1.0, np.float32)
            maps.append(m)
    return maps


CH = 64


def _lerp_load(P, dst, tmp, src_rows, t0, tl, mu_col, np_):
    if t0 == 0:
        P.memset(tmp[:np_, 0:1], 0.0)
        P.dma(tmp[:np_, 1:tl + 1], src_rows[:, 0:tl], w=[tmp])
    else:
        P.dma(tmp[:np_, 0:tl + 1], src_rows[:, t0 - 1:t0 + tl], w=[tmp])
    P.tt(dst[:np_, :tl], tmp[:np_, 0:tl], tmp[:np_, 1:tl + 1], ALU.subtract)
    P.stt(dst[:np_, :tl], dst[:np_, :tl], mu_col, tmp[:np_, 1:tl + 1], ALU.mult, ALU.add)


def stage_rwkv_prep(P, z1T, prm, scr, T, TT=512):
    import math
    NC = 8
    mu_rkv = P.sb("mu_rkv", [128, 24]); P.dma(mu_rkv[:], prm["mu_rkv"], w=[mu_rkv])
    mu_w = P.sb("mu_w", [96, 1]); P.dma(mu_w[:], prm["mu_w"], w=[mu_w])
    mu_a = P.sb("mu_a", [96, 1]); P.dma(mu_a[:], prm["mu_a"], w=[mu_a])
    mu_g = P.sb("mu_g", [128, 2]); P.dma(mu_g[:], prm["mu_g"], w=[mu_g])
    w0 = P.sb("w0", [128, NC]); P.dma(w0[:], prm["w0"], w=[w0])
    a0 = P.sb("a0", [128, NC]); P.dma(a0[:], prm["a0"], w=[a0])
    k_k = P.sb("k_k", [128, NC]); P.dma(k_k[:], prm["k_k"], w=[k_k])
    k_a = P.sb("k_a", [128, NC]); P.dma(k_a[:], prm["k_a"], w=[k_a])
    r_k = P.sb("r_k", [128, NC]); P.dma(r_k[:], prm["r_k"], w=[r_k])
    w2 = P.sb("w2", [96, 1024]); P.dma(w2[:], prm["w2"], w=[w2])
    a2 = P.sb("a2", [96, 1024]); P.dma(a2[:], prm["a2"], w=[a2])
    g2 = P.sb("g2", [128, 2, 1024]); P.dma(g2[:], prm["g2"].rearrange("(c p) m -> p c m", p=128), w=[g2])
    bones = P.sb("bones", [128, 128]); P.dma(bones[:], prm["bones"], w=[bones])
    cmask = P.sb("cmask", [128, TT]); P.dma(cmask[:], prm["cmask"], w=[cmask])
    tmp = [P.sb("tmp%d" % i, [128, TT + 1]) for i in range(2)]
    twT = P.sb("twT", [96, TT]); zaT = P.sb("zaT", [96, TT]); sgT = P.sb("sgT", [128, 2, TT])
    rl = P.sb("rl", [128, TT]); kl = P.sb("kl", [128, TT]); vl = P.sb("vl", [128, TT])
    ps_w = P.ps("ps_w", [128, TT]); ps_a = P.ps("ps_a", [128, TT]); ps_g = P.ps("ps_g", [128, TT])
    ps_s = P.ps("ps_s", [128, TT]); ps_r = P.ps("ps_r", [128, TT])
    def t(n):
        return P.sb(n, [128, TT])
    lw, av, gv, kk, kp, bb, L, e1, e2, t1, t2, o1, o2, o3, o4 = [t(n) for n in
        ("lw", "av", "gv", "kk", "kp", "bb", "L", "e1", "e2", "t1", "t2", "o1", "o2", "o3", "o4")]
    elc = P.sb("elc", [128, TT // CH])
    CW = -math.exp(-0.5)
    nch = TT // CH
    for t0 in range(0, T, TT):
        _lerp_load(P, twT, tmp[0], z1T[3072:3168, :], t0, TT, mu_w[:, 0:1], 96)
        P.act(twT[:], twT[:], AF.Tanh)
        _lerp_load(P, zaT, tmp[1], z1T[3168:3264, :], t0, TT, mu_a[:, 0:1], 96)
        for c2 in range(2):
            _lerp_load(P, sgT[:, c2, :], tmp[c2], z1T[3264 + c2 * 128:3264 + (c2 + 1) * 128, :], t0, TT, mu_g[:, c2:c2 + 1], 128)
        P.act(sgT[:], sgT[:], AF.Sigmoid)
        for c in range(NC):
            rows = slice(c * 128, (c + 1) * 128)
            _lerp_load(P, rl, tmp[0], z1T[0 + c * 128:0 + (c + 1) * 128, :], t0, TT, mu_rkv[:, c:c + 1], 128)
            _lerp_load(P, kl, tmp[1], z1T[1024 + c * 128:1024 + (c + 1) * 128, :], t0, TT, mu_rkv[:, 8 + c:9 + c], 128)
            _lerp_load(P, vl, tmp[0], z1T[2048 + c * 128:2048 + (c + 1) * 128, :], t0, TT, mu_rkv[:, 16 + c:17 + c], 128)
            P.dma(scr["vT"][rows, t0:t0 + TT], vl[:], r=[vl], q="gpsimd", is_out=True)
            P.mm(ps_w[:], w2[:, rows], twT[:])
            P.act(lw[:], ps_w[:], AF.Sigmoid, bias=w0[:, c:c + 1])
            P.ts(lw[:], lw[:], CW, None, ALU.mult)
            P.mm(ps_a[:], a2[:, rows], zaT[:])
            P.act(av[:], ps_a[:], AF.Sigmoid, bias=a0[:, c:c + 1])
            for c2 in range(2):
                P.mm(ps_g[:], g2[:, c2, rows], sgT[:, c2, :], start=(c2 == 0), stop=(c2 == 1))
            P.copy(gv[:], ps_g[:], eng="scalar")
            P.dma(scr["gT"][rows, t0:t0 + TT], gv[:], r=[gv], q="gpsimd", is_out=True)
            P.ts(kk[:], kl[:], k_k[:, c:c + 1], None, ALU.mult)
            P.act(t1[:], kk[:], AF.Square)
            P.mm(ps_s[:], bones[:], t1[:])
            P.ts(t1[:], ps_s[:], 1e-24, None, ALU.max)
            P.act(t1[:], t1[:], AF.Sqrt)
            P.recip(t1[:], t1[:])
            P.tt(kk[:], kk[:], t1[:], ALU.mult)
            P.ts(t2[:], av[:], -1.0, k_a[:, c:c + 1], ALU.add, ALU.mult)
            P.stt(kp[:], t2[:], 1.0, kl[:], ALU.add, ALU.mult)
            P.tt(bb[:], kk[:], av[:], ALU.mult)
            P.op("vector", lambda e, L=L, lw=lw: e.tensor_tensor_scan(L[:], cmask[:], lw[:], 0.0, ALU.mult, ALU.add),
                 r=[cmask, lw], w=[L])
            P.act(e1[:], L[:], AF.Exp)
            P.tt(o1[:], rl[:], e1[:], ALU.mult)
            P.dma(scr["rtT"][rows, t0:t0 + TT], o1[:], r=[o1], q="gpsimd", is_out=True)
            P.copy(elc[:], e1[:].rearrange("p (c s) -> p c s", s=CH)[:, :, CH - 1])
            P.dma(scr["eLC"][rows, t0 // CH:t0 // CH + nch], elc[:], r=[elc], q="gpsimd", is_out=True)
            P.tt(t1[:], L[:], lw[:], ALU.subtract)
            P.act(t1[:], t1[:], AF.Exp)
            P.stt(o2[:], kk[:], -1.0, t1[:], ALU.mult, ALU.mult)
            P.dma(scr["atT"][rows, t0:t0 + TT], o2[:], r=[o2], q="gpsimd", is_out=True)
            P.act(e2[:], L[:], AF.Exp, scale=-1.0)
            P.tt(o3[:], bb[:], e2[:], ALU.mult)
            P.dma(scr["btT"][rows, t0:t0 + TT], o3[:], r=[o3], q="gpsimd", is_out=True)
            P.tt(o4[:], kp[:], e2[:], ALU.mult)
            P.dma(scr["ktT"][rows, t0:t0 + TT], o4[:], r=[o4], q="gpsimd", is_out=True)
            L3 = L[:].rearrange("p (c s) -> p c s", s=CH)
            P.tt(t2[:].rearrange("p (c s) -> p c s", s=CH), L3[:, :, CH - 1:CH].broadcast_to([128, nch, CH]), L3, ALU.subtract)
            P.act(t2[:], t2[:], AF.Exp)
            P.tt(o1[:], bb[:], t2[:], ALU.mult)
            P.dma(scr["BhT"][rows, t0:t0 + TT], o1[:], r=[o1], q="gpsimd", is_out=True)
            P.tt(o2[:], kp[:], t2[:], ALU.mult)
            P.dma(scr["KhT"][rows, t0:t0 + TT], o2[:], r=[o2], q="gpsimd", is_out=True)
            P.stt(t1[:], rl[:], r_k[:, c:c + 1], kp[:], ALU.mult, ALU.mult)
            P.mm(ps_r[:], bones[:], t1[:])
            P.tt(o3[:], ps_r[:], vl[:], ALU.mult)
            P.dma(scr["bonT"][rows, t0:t0 + TT], o3[:], r=[o3], q="gpsimd", is_out=True)
    P.end_stage()


def stage_rwkv_chunk(P, scr, masks_d, ident_d, yT, T, NH=16, SUP=512):
    NCH = T // CH
    ident = P.sb("ident", [128, 128]); P.dma(ident[:], ident_d, w=[ident])
    msk = P.sb("msk", [64, 320]); P.dma(msk[:], masks_d, w=[msk])
    ST = [P.sb("ST%d" % h, [64, 64]) for h in range(NH)]
    for h in range(NH):
        P.memset(ST[h][:], 0.0, eng="gpsimd")
    names = ("atT", "rtT", "btT", "ktT", "BhT", "KhT", "vT")
    nsc = SUP // CH
    bufs = {}
    for par in range(2):
        for nm in names:
            bufs[(nm, par)] = P.sb("in_%s%d" % (nm, par), [64, SUP])
        bufs[("elc", par)] = P.sb("in_elc%d" % par, [64, nsc])
        bufs[("y", par)] = P.sb("out_y%d" % par, [64, SUP])
    def dbl(name, shape, kind="sb"):
        f = P.sb if kind == "sb" else P.ps
        return [f("%s%d" % (name, i), shape) for i in range(2)]
    bG = P.ps("bG", [64, 512]); bP = P.ps("bP", [64, 64]); bQ = P.ps("bQ", [64, 64]); bX = P.ps("bX", [64, 64])
    bR0 = P.ps("bR0", [64, 64]); bU = P.ps("bU", [64, 64]); bY = P.ps("bY", [64, 64]); bSN = P.ps("bSN", [64, 64])
    tm = dbl("tm", [64, 192]); gm = dbl("gm", [64, 320])
    Pm = [dbl("Pm%d_" % i, [64, 64]) for i in range(2)]
    Qm = [dbl("Qm%d_" % i, [64, 64]) for i in range(2)]
    Xm = [dbl("Xm%d_" % i, [64, 64]) for i in range(2)]
    r0s = dbl("r0s", [64, 64]); us = dbl("us", [64, 64])
    I64 = ident[0:64, 0:64]
    u = 0
    for s0 in range(0, T, SUP):
        for h in range(NH):
            par = h % 2
            rows = slice(h * 64, (h + 1) * 64)
            for nm in names:
                P.dma(bufs[(nm, par)][:], scr[nm][rows, s0:s0 + SUP], w=[bufs[(nm, par)]])
            P.dma(bufs[("elc", par)][:], scr["eLC"][rows, s0 // CH:s0 // CH + nsc], w=[bufs[("elc", par)]])
            at, rt, bt, kt, Bh, Kh, vT_ = [bufs[(nm, par)] for nm in names]
            elc = bufs[("elc", par)]
            yb = bufs[("y", par)]
            for ci in range(nsc):
                i2 = u % 2; u += 1
                cs = slice(ci * CH, (ci + 1) * CH)
                for k3, src in enumerate((vT_, Bh, Kh)):
                    P.tr(bG[:, 320 + k3 * 64:320 + (k3 + 1) * 64], src[:, cs], I64)
                P.mm(bG[:, 0:64], bt[:, cs], at[:, cs])
                P.mm(bG[:, 64:128], bt[:, cs], rt[:, cs])
                P.mm(bG[:, 128:192], kt[:, cs], at[:, cs])
                P.mm(bG[:, 192:256], kt[:, cs], rt[:, cs])
                P.mm(bG[:, 256:320], at[:, cs], bt[:, cs])
                P.copy(tm[i2][:], bG[:, 320:512], eng="vector")
                P.tt(gm[i2][:], bG[:, 0:320], msk[:], ALU.mult)
                Vt, Bt, Kt = tm[i2][:, 0:64], tm[i2][:, 64:128], tm[i2][:, 128:192]
                P0, NrbT, MakT, NrkT, Q0 = (gm[i2][:, 0:64], gm[i2][:, 64:128], gm[i2][:, 128:192],
                                            gm[i2][:, 192:256], gm[i2][:, 256:320])
                Pc, Qc = P0, Q0
                X = Xm[0][i2]
                P.tt(X[:], P0, I64, ALU.add, eng="gpsimd")
                for it in range(5):
                    Qn = Qm[it % 2][i2]
                    P.mm(bQ[:], Pc, Qc)
                    P.copy(Qn[:], bQ[:], eng="scalar")
                    if it < 4:
                        Pn = Pm[it % 2][i2]
                        P.mm(bP[:], Qc, Pc)
                        P.copy(Pn[:], bP[:], eng="vector")
                    Xn = Xm[(it + 1) % 2][i2]
                    P.mm(bX[:], Qn[:], X[:], start=True, stop=False)
                    P.mm(bX[:], I64, X[:], start=False, stop=True)
                    P.copy(Xn[:], bX[:], eng=("scalar" if it % 2 else "vector"))
                    X = Xn
                    Qc = Qn[:]
                    if it < 4:
                        Pc = Pn[:]
                S0 = ST[h]
                P.mm(bR0[:], at[:, cs], S0[:], start=True, stop=False)
                P.mm(bR0[:], MakT, Vt, start=False, stop=True)
                P.copy(r0s[i2][:], bR0[:], eng="scalar")
                P.mm(bU[:], X[:], r0s[i2][:])
                P.copy(us[i2][:], bU[:], eng="vector")
                P.mm(bY[:], S0[:], rt[:, cs], start=True, stop=False)
                P.mm(bY[:], us[i2][:], NrbT, start=False, stop=False)
                P.mm(bY[:], Vt, NrkT, start=False, stop=True)
                P.copy(yb[:, cs], bY[:], eng="scalar")
                P.mm(bSN[:], Bt, us[i2][:], start=True, stop=False)
                P.mm(bSN[:], Kt, Vt, start=False, stop=True)
                P.stt(S0[:], S0[:], elc[:, ci:ci + 1], bSN[:], ALU.mult, ALU.add)
            P.dma(yT[rows, s0:s0 + SUP], yb[:], r=[yb], q="gpsimd", is_out=True)
    P.end_stage()


def stage_rwkv_post(P, yT, scr, prm, outT, T, TT=512, gn_eps=64e-5):
    NC = 8
    lg = P.sb("lg", [128, NC]); P.dma(lg[:], prm["ln_g"], w=[lg])
    lb = P.sb("lb", [128, NC]); P.dma(lb[:], prm["ln_b"], w=[lb])
    bo = P.sb("bo64", [128, 128]); P.dma(bo[:], prm["bones64"], w=[bo])
    def d(n):
        return [P.sb("%s%d" % (n, i), [128, TT]) for i in range(2)]
    y, sq, mu, rs, gv, bn, o = d("y"), d("sq"), d("mu"), d("rs"), d("gv"), d("bn"), d("o")
    ps_m = [P.ps("ps_m%d" % i, [128, TT]) for i in range(2)]
    ps_q = [P.ps("ps_q%d" % i, [128, TT]) for i in range(2)]
    n = 0
    for t0 in range(0, T, TT):
        for c in range(NC):
            i = n % 2; n += 1
            rows = slice(c * 128, (c + 1) * 128)
            P.dma(y[i][:], yT[rows, t0:t0 + TT], w=[y[i]])
            P.dma(gv[i][:], scr["gT"][rows, t0:t0 + TT], w=[gv[i]])
            P.dma(bn[i][:], scr["bonT"][rows, t0:t0 + TT], w=[bn[i]])
            P.act(sq[i][:], y[i][:], AF.Square)
            P.mm(ps_m[i][:], bo[:], y[i][:])
            P.mm(ps_q[i][:], bo[:], sq[i][:])
            P.copy(mu[i][:], ps_m[i][:])
            P.tt(rs[i][:], mu[i][:], mu[i][:], ALU.mult)
            P.tt(rs[i][:], ps_q[i][:], rs[i][:], ALU.subtract)
            P.ts(rs[i][:], rs[i][:], gn_eps, None, ALU.add)
            P.act(rs[i][:], rs[i][:], AF.Sqrt)
            P.recip(rs[i][:], rs[i][:])
            P.tt(y[i][:], y[i][:], mu[i][:], ALU.subtract)
            P.tt(y[i][:], y[i][:], rs[i][:], ALU.mult, eng="gpsimd")
            P.act(o[i][:], y[i][:], AF.Identity, scale=lg[:, c:c + 1], bias=lb[:, c:c + 1])
            P.tt(o[i][:], o[i][:], bn[i][:], ALU.add)
            P.tt(o[i][:], o[i][:], gv[i][:], ALU.mult, eng="gpsimd")
            P.dma(outT[rows, t0:t0 + TT], o[i][:], r=[o[i]], q="gpsimd", is_out=True)
    P.end_stage()


def stage_lru(P, z1T, prm, outT, T, TT=512):
    NC = 8
    XO, GO = 3520, 4544
    cw = P.sb("cw", [128, NC, 4]); P.dma(cw[:], prm["lru_cw"], w=[cw])
    cb = P.sb("cb", [128, NC]); P.dma(cb[:], prm["lru_cb"], w=[cb])
    ba = P.sb("ba", [128, NC]); P.dma(ba[:], prm["lru_ba"], w=[ba])
    bx = P.sb("bx", [128, NC]); P.dma(bx[:], prm["lru_bx"], w=[bx])
    lam = P.sb("lam", [128, NC]); P.dma(lam[:], prm["lru_lam"], w=[lam])
    wa = P.sb("wa", [128, NC, 128]); P.dma(wa[:], prm["lru_wa_bd"], w=[wa])
    wx = P.sb("wx", [128, NC, 128]); P.dma(wx[:], prm["lru_wx_bd"], w=[wx])
    cl = P.sb("cl", [128, NC])
    P.act(cl[:], lam[:], AF.Exp, scale=-1.0)
    P.act(cl[:], cl[:], AF.Ln, bias=1.0)
    P.ts(cl[:], cl[:], -8.0, None, ALU.mult)
    hst = [P.sb("hst%d" % c, [128, 1]) for c in range(NC)]
    for c in range(NC):
        P.memset(hst[c][:], 0.0)
    def d(n, w=TT):
        return [P.sb("%s%d" % (n, i), [128, w]) for i in range(2)]
    xin, xc, gb, r_, i_, a_, u_, h_, o_ = d("xin", TT + 3), d("xc"), d("gb"), d("r_"), d("i_"), d("a_"), d("u_"), d("h_"), d("o_")
    ps_a = [P.ps("ps_a%d" % i, [128, TT]) for i in range(2)]
    ps_x = [P.ps("ps_x%d" % i, [128, TT]) for i in range(2)]
    n = 0
    for t0 in range(0, T, TT):
        for c in range(NC):
            i = n % 2; n += 1
            rows = slice(c * 128, (c + 1) * 128)
            if t0 == 0:
                P.memset(xin[i][:, 0:3], 0.0)
                P.dma(xin[i][:, 3:TT + 3], z1T[XO + c * 128:XO + (c + 1) * 128, 0:TT], w=[xin[i]])
            else:
                P.dma(xin[i][:], z1T[XO + c * 128:XO + (c + 1) * 128, t0 - 3:t0 + TT], w=[xin[i]])
            P.dma(gb[i][:], z1T[GO + c * 128:GO + (c + 1) * 128, t0:t0 + TT], w=[gb[i]])
            P.ts(xc[i][:], xin[i][:, 0:TT], cw[:, c, 0:1], cb[:, c:c + 1], ALU.mult, ALU.add)
            for j in range(1, 4):
                P.stt(xc[i][:], xin[i][:, j:j + TT], cw[:, c, j:j + 1], xc[i][:], ALU.mult, ALU.add)
            P.mm(ps_a[i][:], wa[:, c, :], xc[i][:])
            P.mm(ps_x[i][:], wx[:, c, :], xc[i][:])
            P.act(r_[i][:], ps_a[i][:], AF.Sigmoid, bias=ba[:, c:c + 1])
            P.act(i_[i][:], ps_x[i][:], AF.Sigmoid, bias=bx[:, c:c + 1])
            P.act(a_[i][:], r_[i][:], AF.Exp, scale=cl[:, c:c + 1])
            P.tt(u_[i][:], a_[i][:], a_[i][:], ALU.mult)
            P.ts(u_[i][:], u_[i][:], -1.0, 1.0, ALU.mult, ALU.add)
            P.act(u_[i][:], u_[i][:], AF.Sqrt)
            P.tt(i_[i][:], i_[i][:], xc[i][:], ALU.mult, eng="gpsimd")
            P.tt(u_[i][:], u_[i][:], i_[i][:], ALU.mult)
            P.op("vector", lambda e, h=h_[i], a=a_[i], uu=u_[i], st=hst[c]: e.tensor_tensor_scan(h[:], a[:], uu[:], st[:, 0:1], ALU.mult, ALU.add),
                 r=[a_[i], u_[i], hst[c]], w=[h_[i]])
            P.copy(hst[c][:], h_[i][:, TT - 1:TT])
            P.act(r_[i][:], gb[i][:], AF.Square)
            P.ts(r_[i][:], r_[i][:], 0.044715, 1.0, ALU.mult, ALU.add)
            P.tt(r_[i][:], r_[i][:], gb[i][:], ALU.mult, eng="gpsimd")
            P.act(r_[i][:], r_[i][:], AF.Tanh, scale=0.7978845608028654)
            P.ts(r_[i][:], r_[i][:], 1.0, 0.5, ALU.add, ALU.mult)
            P.tt(gb[i][:], gb[i][:], r_[i][:], ALU.mult, eng="gpsimd")
            P.tt(o_[i][:], h_[i][:], gb[i][:], ALU.mult, eng="gpsimd")
            P.dma(outT[rows, t0:t0 + TT], o_[i][:], r=[o_[i]], q="gpsimd", is_out=True)
    P.end_stage()


def cd_params(inp, j=0):
    f = lambda k: np.asarray(inp[k][j], np.float32)
    mu = f("cd_shift_mu")
    mu_rkv = np.ascontiguousarray(mu[:3072].reshape(24, 128).T)
    bones = np.kron(np.eye(2, dtype=np.float32), np.ones((64, 64), np.float32))
    su = np.triu(np.ones((64, 64), np.float32), 1)
    iu = np.triu(np.ones((64, 64), np.float32), 0)
    sl = np.tril(np.ones((64, 64), np.float32), -1)
    masks = np.ascontiguousarray(np.concatenate([su, iu, su, iu, sl], 1))
    cm = np.ones((128, 512), np.float32); cm[:, ::CH] = 0.0
    def bd(w):
        out = np.zeros((8, 128, 128), np.float32)
        for c in range(8):
            out[c, :64, :64] = w[2 * c]
            out[c, 64:, 64:] = w[2 * c + 1]
        return np.ascontiguousarray(out.transpose(1, 0, 2))
    return {
        "mu_rkv": mu_rkv, "mu_w": mu[3072:3168].reshape(96, 1).copy(), "mu_a": mu[3168:3264].reshape(96, 1).copy(),
        "mu_g": np.ascontiguousarray(mu[3264:3520].reshape(2, 128).T),
        "w0": pl(f("cd_w0")), "a0": pl(f("cd_a0")), "k_k": pl(f("cd_k_k")), "k_a": pl(f("cd_k_a")),
        "r_k": pl(f("cd_r_k").reshape(-1)), "w2": f("cd_w2"), "a2": f("cd_a2"), "g2": f("cd_g2"),
        "bones": bones, "bones64": bones / 64.0 * 1.0, "cmask": cm, "masks": masks,
        "ln_g": pl(f("cd_ln_x_g")), "ln_b": pl(f("cd_ln_x_b")),
        "lru_cw": np.ascontiguousarray(f("cd_lru_conv_w").reshape(4, 8, 128).transpose(2, 1, 0)),
        "lru_cb": pl(f("cd_lru_conv_b")), "lru_ba": pl(f("cd_lru_ba")), "lru_bx": pl(f("cd_lru_bx")),
        "lru_lam": pl(f("cd_lru_lambda")), "lru_wa_bd": bd(f("cd_lru_wa")), "lru_wx_bd": bd(f("cd_lru_wx")),
        "ident": np.eye(128, dtype=np.float32),
    }


CD_PRM_SHAPES = {
    "mu_rkv": [128, 24], "mu_w": [96, 1], "mu_a": [96, 1], "mu_g": [128, 2], "w0": [128, 8], "a0": [128, 8],
    "k_k": [128, 8], "k_a": [128, 8], "r_k": [128, 8], "w2": [96, 1024], "a2": [96, 1024], "g2": [256, 1024],
    "bones": [128, 128], "bones64": [128, 128], "cmask": [128, 512], "masks": [64, 320],
    "ln_g": [128, 8], "ln_b": [128, 8], "lru_cw": [128, 8, 4], "lru_cb": [128, 8], "lru_ba": [128, 8],
    "lru_bx": [128, 8], "lru_lam": [128, 8], "lru_wa_bd": [128, 8, 128], "lru_wx_bd": [128, 8, 128],
    "ident": [128, 128],
}


def emit_mixer_cd(P, z1T, prm, mixT, T):
    scr = {nm: P.dram_tmp("cd_" + nm, [1024, T]) for nm in ("atT", "rtT", "btT", "ktT", "BhT", "KhT", "vT", "gT", "bonT")}
    scr["eLC"] = P.dram_tmp("cd_eLC", [1024, T // CH])
    yT = P.dram_tmp("cd_yT", [1024, T])
    stage_rwkv_prep(P, z1T, prm, scr, T)
    stage_rwkv_chunk(P, scr, prm["masks"], prm["ident"], yT, T)
    stage_rwkv_post(P, yT, scr, prm, mixT[0:1024, :], T)
    stage_lru(P, z1T, prm, mixT[1024:2048, :], T)


def stage_select(P, srcT, flag_d, dstT, R, TH, TT=2048):
    flag = P.sb("flag_s", [128, 1]); P.dma(flag[:], flag_d, w=[flag])
    a = [P.sb("sa%d" % i, [128, TT]) for i in range(2)]
    b = [P.sb("sb%d" % i, [128, TT]) for i in range(2)]
    TT = min(TT, TH)
    n = 0
    for r0 in range(0, R, 128):
        for t0 in range(0, TH, TT):
            i = n % 2; n += 1
            P.dma(a[i][:, :TT], srcT[r0:r0 + 128, t0:t0 + TT], w=[a[i]])
            P.dma(b[i][:, :TT], srcT[r0:r0 + 128, TH + t0:TH + t0 + TT], w=[b[i]])
            P.tt(b[i][:, :TT], b[i][:, :TT], a[i][:, :TT], ALU.subtract, eng="gpsimd")
            P.stt(a[i][:, :TT], b[i][:, :TT], flag[:, 0:1], a[i][:, :TT], ALU.mult, ALU.add)
            P.dma(dstT[r0:r0 + 128, t0:t0 + TT], a[i][:, :TT], r=[a[i]], q="gpsimd", is_out=True)
    P.end_stage()


CD_INP = 5632


def build_full(S, D=2048, DE=1024):
    P = Prog()
    TH = S // 2
    SK = S + 128
    AB_IN = 3584
    JC = DE // 128
    di = P.dram_in
    xhT = di("xhT", [D, SK]); cT = di("cT", [D, 1])
    ada_w = [di("ada_w%d" % l, [D, 6 * D]) for l in range(2)]
    ada_b = [di("ada_b%d" % l, [128, 6 * D // 128]) for l in range(2)]
    g_mix = [di("g_mix%d" % l, [128, 16]) for l in range(2)]
    g_ffn = [di("g_ffn%d" % l, [128, 16]) for l in range(2)]
    g_fin = di("g_fin", [128, 16])
    w_in0 = di("w_in0", [D, AB_IN]); w_out0 = di("w_out0", [D, D])
    w_in1 = di("w_in1", [D, CD_INP]); w_out1 = di("w_out1", [D, D])
    pos_bc = di("pos_bc", [128, SK], I32)
    invf = di("invf", [128, 1]); sgn = di("sgn", [128, 1])
    maskg = di("maskg", [128, 256]); mask0 = di("mask0", [128, 256])
    sink_bc = di("sink_bc", [128, 16]); ident = di("ident", [128, 128])
    cw = di("cw", [128, 8, 31]); cb = di("cb", [128, 8]); lg = di("lg", [128, 8]); lb = di("lb", [128, 8])
    zflag = di("zflag", [128, 1]); hflag = di("hflag", [128, 1])
    rw = di("rw", [128, 16, 16]); rb = di("rb_bc", [128, 16])
    w1 = [di("w1r%d" % l, [16, JC, 128, 16, 128]) for l in range(2)]
    w3 = [di("w3r%d" % l, [16, JC, 128, 16, 128]) for l in range(2)]
    w2 = [di("w2r%d" % l, [16, 16, 128, JC, 128]) for l in range(2)]
    prm = {k: di("p_" + k, sh) for k, sh in CD_PRM_SHAPES.items()}
    outT = P.dram_out("outT", [D, TH])
    tmp = P.dram_tmp
    modT = [tmp("modT%d" % l, [6 * D, 1]) for l in range(2)]
    hT = tmp("hT", [D, SK]); zT = tmp("zT", [AB_IN, SK]); mixT = tmp("mixT", [D, S])
    x1T = tmp("x1T", [D, S]); hfT = tmp("hfT", [D, S]); gatesT = tmp("gatesT", [16, S]); x2T = tmp("x2T", [D, S])
    z1T = tmp("z1T", [CD_INP, S]); mix1T = tmp("mix1T", [D, S])
    x2hT = tmp("x2hT", [D, TH]); mix1hT = tmp("mix1hT", [D, TH]); x3T = tmp("x3T", [D, TH]); x4T = tmp("x4T", [D, TH])
    mv = [m.rearrange("(c p) o -> p (c o)", p=128) for m in modT]
    for l in range(2):
        stage_lin(P, cT, ada_w[l], modT[l], K=D, M=6 * D, T=1, mode="bias", fp32=True, in_silu=True, bias_d=ada_b[l])
    stage_normfm(P, xhT, modT[0], g_mix[0], 0, 1, hT, SK)
    stage_lin(P, hT, w_in0, zT, K=D, M=AB_IN, T=SK)
    TQ = min(4096, S)
    for sg in range(S // TQ):
        c0 = sg * TQ
        stage_att(P, zT[:, c0:c0 + TQ + 128], pos_bc[:, c0:c0 + TQ + 128], invf, sgn, maskg,
                  mask0 if sg == 0 else maskg, sink_bc, ident, mixT[0:1024, c0:c0 + TQ], TQ)
    stage_conv(P, zT[1536:2560, 98:SK], zT[2560:3584, 98:SK], cw, cb, lg, lb, zflag, mixT[1024:2048, :], S)
    stage_lin(P, mixT, w_out0, x1T, K=D, M=D, T=S, mode="resid", gate_d=mv[0][:, 32:48], rT=xhT[:, 128:SK])
    stage_normfm(P, x1T, modT[0], g_ffn[0], 3, 4, hfT, S)
    stage_route(P, hfT, rw, rb, ident, gatesT, S)
    stage_moe(P, hfT, x1T, gatesT, w1[0], w3[0], w2[0], modT[0], 5, x2T, S, DE=DE)
    stage_normfm(P, x2T, modT[1], g_mix[1], 0, 1, hT[:, 0:S], S)
    stage_lin(P, hT[:, 0:S], w_in1, z1T, K=D, M=CD_INP, T=S)
    emit_mixer_cd(P, z1T, prm, mix1T, S)
    stage_select(P, x2T, hflag, x2hT, D, TH)
    stage_select(P, mix1T, hflag, mix1hT, D, TH)
    stage_lin(P, mix1hT, w_out1, x3T, K=D, M=D, T=TH, mode="resid", gate_d=mv[1][:, 32:48], rT=x2hT)
    stage_normfm(P, x3T, modT[1], g_ffn[1], 3, 4, hfT[:, 0:TH], TH)
    stage_route(P, hfT[:, 0:TH], rw, rb, ident, gatesT[:, 0:TH], TH)
    stage_moe(P, hfT[:, 0:TH], x3T, gatesT[:, 0:TH], w1[1], w3[1], w2[1], modT[1], 5, x4T, TH, DE=DE)
    stage_normfm(P, x4T, None, g_fin, 0, 0, outT, TH)
    return P


def full_inmaps(inp):
    x = np.asarray(inp["x"], np.float32)
    B, S, D = x.shape
    pos = np.asarray(inp["positions"], np.int32)
    invf = (10000.0 ** (-np.arange(32, dtype=np.float32) / 32)).astype(np.float32)
    invf128 = np.tile(invf, 4).reshape(128, 1).astype(np.float32)
    sgn = np.tile(np.concatenate([-np.ones(32), np.ones(32)]), 2).reshape(128, 1).astype(np.float32)
    qi = np.arange(128)[:, None]
    kj = np.arange(256)[None, :]
    dist = 128 + qi - kj
    valid = (dist >= 0) & (dist < 128)
    maskg = np.where(valid, 0.0, -1e30).astype(np.float32)
    mask_first = np.where(valid & (kj >= 128), 0.0, -1e30).astype(np.float32)
    sinks = np.asarray(inp["ab_sinks"][0], np.float32)
    conv_w = np.asarray(inp["ab_conv_w"][0], np.float32).reshape(31, 1024)
    w_in1 = np.asarray(inp["cd_w_in"][0], np.float32)
    w_in1p = np.concatenate([w_in1, np.zeros((D, CD_INP - w_in1.shape[1]), np.float32)], 1)
    common = {
        "g_fin": pl(inp["final_norm"]),
        "w_in0": np.ascontiguousarray(inp["ab_w_in"][0]), "w_out0": np.ascontiguousarray(inp["ab_w_out"][0]),
        "w_in1": np.ascontiguousarray(w_in1p), "w_out1": np.ascontiguousarray(inp["cd_w_out"][0]),
        "invf": invf128, "sgn": sgn, "maskg": maskg, "mask0": mask_first,
        "sink_bc": np.ascontiguousarray(np.broadcast_to(sinks[None, :], (128, 16))).astype(np.float32),
        "ident": np.eye(128, dtype=np.float32),
        "cw": np.ascontiguousarray(conv_w.reshape(31, 8, 128).transpose(2, 1, 0)),
        "cb": pl(inp["ab_conv_b"][0]), "lg": pl(inp["ab_conv_ln_g"][0]), "lb": pl(inp["ab_conv_ln_b"][0]),
        "zflag": np.zeros((128, 1), np.float32),
        "rw": np.ascontiguousarray(np.asarray(inp["router_w"], np.float32).reshape(16, 128, 16).transpose(1, 0, 2)),
        "rb_bc": np.ascontiguousarray(np.broadcast_to(np.asarray(inp["router_bias"], np.float32)[None], (128, 16))),
    }
    for l in range(2):
        common["ada_w%d" % l] = np.ascontiguousarray(inp["ada_w"][l])
        common["ada_b%d" % l] = pl(inp["ada_b"][l])
        common["g_mix%d" % l] = pl(inp["norm_mix"][l])
        common["g_ffn%d" % l] = pl(inp["norm_ffn"][l])
        a, b_, c_ = moe_weights_layout(np.asarray(inp["moe_w1"][l]), np.asarray(inp["moe_w3"][l]), np.asarray(inp["moe_w2"][l]))
        common["w1r%d" % l], common["w3r%d" % l], common["w2r%d" % l] = a, b_, c_
    for k, v in cd_params(inp).items():
        common["p_" + k] = np.ascontiguousarray(v, dtype=np.float32)
    maps = []
    for b in range(B):
        xh = np.concatenate([np.zeros((128, D), np.float32), x[b]], 0)
        ph = np.concatenate([np.zeros((128,), np.int32), pos[b]])
        xhT = np.ascontiguousarray(xh.T)
        cT = np.ascontiguousarray(np.asarray(inp["c"], np.float32)[b].reshape(D, 1))
        pbc = np.ascontiguousarray(np.broadcast_to(ph[None, :], (128, ph.shape[0]))).astype(np.int32)
        for f in range(2):
            m = dict(common)
            m["xhT"] = xhT
            m["cT"] = cT
            m["pos_bc"] = pbc
            m["hflag"] = np.full((128, 1), float(f), np.float32)
            maps.append(m)
    return maps


def kernel(**inputs):
    inp = {k: np.asarray(v) for k, v in inputs.items()}
    B, S, D = inp["x"].shape
    DE = inp["moe_w1"].shape[-1]
    P = build_full(S, D, DE)
    maps = full_inmaps(inp)
    res = run(P, maps)
    TH = S // 2
    out = np.empty((B, S, D), np.float32)
    for b in range(B):
        for f in range(2):
            out[b, f * TH:(f + 1) * TH, :] = res.results[2 * b + f]["outT"].T
    return out
```

```python
import numpy as np
from contextlib import ExitStack
import concourse.bass as bass
import concourse.mybir as mybir
from concourse.bass_utils import run_bass_kernel_spmd

F32 = mybir.dt.float32
BF16 = mybir.dt.bfloat16
I32 = mybir.dt.int32
AF = mybir.ActivationFunctionType
ALU = mybir.AluOpType
AX = mybir.AxisListType
NCORES = 8


class V:
    def __init__(self, ap, key):
        self.ap = ap
        self.key = key

    def __getitem__(self, idx):
        return V(self.ap[idx], self.key)


def _u(x):
    return x.ap if isinstance(x, V) else x


def _key(x):
    if isinstance(x, str):
        return x
    if isinstance(x, V):
        return x.key
    t = getattr(x, "tensor", x)
    return t.name


class Prog:
    ENG = ("tensor", "vector", "scalar", "gpsimd", "sync")
    NLANES = 8

    def __init__(self, name="k"):
        self.nc = bass.Bass("TRN2", target_bir_lowering=False)
        self.es = ExitStack()
        self.ges = ExitStack()
        self.stage_no = 0
        self._reset()

    def _reset(self):
        self.streams = {e: [] for e in self.ENG}
        self.count = {e: 0 for e in self.ENG}
        self.known = {e: {} for e in self.ENG}
        self.last_w = {}
        self.readers = {}
        self.lane_cnt = [0] * self.NLANES
        self.lane_next = 0
        self.out_tokens = []
        self.ntiles = 0

    def dram_in(self, name, shape, dtype=F32):
        return self.nc.dram_tensor(name, list(shape), dtype, kind="ExternalInput").ap()

    def dram_out(self, name, shape, dtype=F32):
        return self.nc.dram_tensor(name, list(shape), dtype, kind="ExternalOutput").ap()

    def dram_tmp(self, name, shape, dtype=F32):
        return self.nc.dram_tensor(name, list(shape), dtype, kind="Internal").ap()

    def sb(self, name, shape, dtype=F32):
        return self.es.enter_context(self.nc.sbuf_tensor("g%d_%s" % (self.stage_no, name), list(shape), dtype))

    def ps(self, name, shape, dtype=F32):
        return self.es.enter_context(self.nc.psum_tensor("g%d_%s" % (self.stage_no, name), list(shape), dtype))

    def _deps(self, eng, r, w):
        deps = set()
        for k in r:
            k = _key(k)
            if k in self.last_w:
                deps.add(self.last_w[k])
        for k in w:
            k = _key(k)
            if k in self.last_w:
                deps.add(self.last_w[k])
            for t in self.readers.get(k, ()):
                deps.add(t)
        need = {}
        for (s, v) in deps:
            if s == "tensor" and eng == "tensor":
                continue
            if self.known[eng].get(s, 0) < v:
                need[s] = max(need.get(s, 0), v)
        for s, v in need.items():
            self.known[eng][s] = v
        return list(need.items())

    def _commit(self, tok, r, w):
        for k in w:
            k = _key(k)
            self.last_w[k] = tok
            self.readers[k] = []
        for k in r:
            k = _key(k)
            self.readers.setdefault(k, []).append(tok)

    def op(self, eng, fn, r=(), w=()):
        waits = self._deps(eng, r, w)
        self.count[eng] += 1
        tok = (eng, self.count[eng])
        self.known[eng][eng] = max(self.known[eng].get(eng, 0), 0)
        self.streams[eng].append((waits, fn, (eng, 1)))
        self._commit(tok, r, w)
        return tok

    def dma(self, out, in_, r=(), w=(), q="sync", is_out=False, **kw):
        lane = self.lane_next
        self.lane_next = (self.lane_next + 1) % self.NLANES
        s = "lane%d" % lane
        waits = self._deps(q, r, w)
        prev = self.lane_cnt[lane]
        if prev > 0 and self.known[q].get(s, 0) < prev:
            waits.append((s, prev))
            self.known[q][s] = prev
        self.lane_cnt[lane] += 16
        tok = (s, self.lane_cnt[lane])
        self.streams[q].append((waits, (lambda e, o=out, i=in_, kw=kw: e.dma_start(out=o, in_=i, **kw)), (s, 16)))
        self._commit(tok, r, w)
        if is_out:
            self.out_tokens.append(tok)
        return tok

    def allgather(self, out, in_, groups):
        lane = self.lane_next
        self.lane_next = (self.lane_next + 1) % self.NLANES
        s = "lane%d" % lane
        waits = []
        prev = self.lane_cnt[lane]
        if prev > 0 and self.known["gpsimd"].get(s, 0) < prev:
            waits.append((s, prev))
            self.known["gpsimd"][s] = prev
        self.lane_cnt[lane] += 16
        tok = (s, self.lane_cnt[lane])
        self.streams["gpsimd"].append((waits, (lambda e, o=out, i=in_: e.collective_compute(
            "AllGather", ALU.bypass, replica_groups=groups, ins=[i], outs=[o])), (s, 16)))
        self.out_tokens.append(tok)
        return tok

    def mm(self, out, lhsT, rhs, start=True, stop=True, r=None, w=None, **kw):
        r = [lhsT, rhs] if r is None else r
        w = [out] if w is None else w
        return self.op("tensor", lambda e: e.matmul(_u(out), _u(lhsT), _u(rhs), start=start, stop=stop, **kw), r=r, w=w)

    def tr(self, out, in_, ident):
        return self.op("tensor", lambda e: e.transpose(_u(out), _u(in_), _u(ident)), r=[in_, ident], w=[out])

    def act(self, out, in_, func, bias=None, scale=None, accum_out=None, eng="scalar", extra_r=()):
        kw = {}
        r = [in_] + list(extra_r)
        w = [out]
        if bias is not None:
            kw["bias"] = _u(bias)
            if not isinstance(bias, (int, float)):
                r.append(bias)
        if scale is not None:
            kw["scale"] = _u(scale)
            if not isinstance(scale, (int, float)):
                r.append(scale)
        if accum_out is not None:
            kw["accum_out"] = _u(accum_out)
            w.append(accum_out)
        return self.op("scalar", lambda e: e.activation(_u(out), _u(in_), func, **kw), r=r, w=w)

    def tt(self, out, in0, in1, op, eng="vector"):
        return self.op(eng, lambda e: e.tensor_tensor(_u(out), _u(in0), _u(in1), op), r=[in0, in1], w=[out])

    def ts(self, out, in0, s1, s2, op0, op1=None, eng="vector", accum_out=None):
        r = [in0] + [s for s in (s1, s2) if s is not None and not isinstance(s, (int, float))]
        w = [out] + ([accum_out] if accum_out is not None else [])
        kw = {}
        if op1 is not None:
            kw["op1"] = op1
        if accum_out is not None:
            kw["accum_out"] = _u(accum_out)
        return self.op(eng, lambda e: e.tensor_scalar(_u(out), _u(in0), _u(s1), _u(s2), op0, **kw), r=r, w=w)

    def stt(self, out, in0, scalar, in1, op0, op1, eng="vector"):
        r = [in0, in1] + ([] if isinstance(scalar, (int, float)) else [scalar])
        return self.op(eng, lambda e: e.scalar_tensor_tensor(_u(out), _u(in0), _u(scalar), _u(in1), op0, op1), r=r, w=[out])

    def copy(self, out, in_, eng="vector"):
        if eng == "scalar":
            return self.op("scalar", lambda e: e.copy(_u(out), _u(in_)), r=[in_], w=[out])
        return self.op(eng, lambda e: e.tensor_copy(_u(out), _u(in_)), r=[in_], w=[out])

    def memset(self, out, val, eng="vector"):
        return self.op(eng, lambda e: e.memset(out, val), r=[], w=[out])

    def recip(self, out, in_):
        return self.op("vector", lambda e: e.reciprocal(out, in_), r=[in_], w=[out])

    def reduce(self, out, in_, op, axis=AX.X, eng="vector"):
        return self.op(eng, lambda e: e.tensor_reduce(out, in_, axis, op), r=[in_], w=[out])

    def end_stage(self):
        self.build(final=False)
        self.stage_no += 1
        self.es = ExitStack()
        self._reset()

    def build(self, final=True):
        nc = self.nc
        fin = {}
        for (s, v) in self.out_tokens:
            fin[s] = max(fin.get(s, 0), v)
        sem_names = set(self.ENG)
        for i in range(self.NLANES):
            sem_names.add("lane%d" % i)
        sems = {}
        for s in sorted(sem_names):
            sems[s] = nc.alloc_semaphore(name="s%d_%s" % (self.stage_no, s))
        streams = self.streams
        block = self.es.enter_context(nc.Block())

        def emit(eng_name):
            def body(e):
                for waits, fn, (s, n) in streams[eng_name]:
                    for (ws, wv) in waits:
                        e.wait_ge(sems[ws], wv)
                    ins = fn(e)
                    ins.then_inc(sems[s], n)
                if eng_name == "sync":
                    for s, v in fin.items():
                        e.wait_ge(sems[s], v)
            return body

        block.tensor(emit("tensor"))
        block.vector(emit("vector"))
        block.scalar(emit("scalar"))
        block.gpsimd(emit("gpsimd"))
        block.sync(emit("sync"))
        self.es.close()
        nc.clear_and_free_semaphores(list(sems.values()))
        nc.all_engine_barrier()
        return nc


def run(P, in_maps, trace=False):
    P.ges.close()
    nc = P.nc
    res = run_bass_kernel_spmd(nc, in_maps, core_ids=list(range(len(in_maps))), trace=trace)
    return res


def stage_lin(P, xT, w, yT, K, M, T, mode="plain", fp32=False, in_silu=False, TS=2048,
              bias_d=None, gate_d=None, rT=None):
    KC, MC = K // 128, M // 128
    TS = min(TS, T)
    xv = xT.rearrange("(kc p) t -> p kc t", p=128)
    wv = w.rearrange("(kc p) m -> p kc m", p=128)
    DT = F32 if fp32 else BF16
    if mode == "bias":
        bias = P.sb("bias_s", [128, MC])
        P.dma(bias[:], bias_d, w=[bias])
    if mode == "resid":
        gate = P.sb("gate_s", [128, MC])
        P.dma(gate[:], gate_d, w=[gate], allow_slow_non_contiguous=True)
    TT = min(512, TS)
    xb = P.sb("xb", [128, KC, TS], DT)
    xst = [P.sb("xst%d" % i, [128, TS]) for i in range(2)]
    wst = [P.sb("wst%d" % i, [128, KC, 128]) for i in range(2)]
    wb = [P.sb("wb%d" % i, [128, KC, 128], DT) for i in range(2)] if not fp32 else wst
    acc = [P.ps("acc%d" % i, [128, TT]) for i in range(4)]
    ot = [P.sb("ot%d" % i, [128, TT]) for i in range(3)]
    rt = [P.sb("rt%d" % i, [128, TT]) for i in range(2)]
    cnt = 0
    wcnt = 0
    for t0 in range(0, T, TS):
        tl = min(TS, T - t0)
        tt_ = min(TT, tl)
        for kc in range(KC):
            s = xst[kc % 2]
            P.dma(s[:, :tl], xv[:, kc, t0:t0 + tl], w=[s])
            if in_silu:
                P.act(xb[:, kc, :tl], s[:, :tl], AF.Silu)
            else:
                P.copy(xb[:, kc, :tl], s[:, :tl], eng="vector")
        for mc in range(MC):
            wi = wcnt % 2
            wcnt += 1
            P.dma(wst[wi][:], wv[:, :, mc * 128:(mc + 1) * 128], w=[wst[wi]])
            if not fp32:
                P.copy(wb[wi][:], wst[wi][:], eng="gpsimd")
            for tt in range(0, tl, TT):
                tt_ = min(TT, tl - tt)
                a = acc[cnt % 4]
                o = ot[cnt % 3]
                for kc in range(KC):
                    P.mm(a[:, :tt_], wb[wi][:, kc, :], xb[:, kc, tt:tt + tt_], start=(kc == 0), stop=(kc == KC - 1))
                if mode == "plain":
                    P.act(o[:, :tt_], a[:, :tt_], AF.Copy)
                elif mode == "bias":
                    P.act(o[:, :tt_], a[:, :tt_], AF.Identity, bias=bias[:, mc:mc + 1])
                else:
                    rr = rt[cnt % 2]
                    P.dma(rr[:, :tt_], rT[mc * 128:(mc + 1) * 128, t0 + tt:t0 + tt + tt_], w=[rr])
                    P.stt(o[:, :tt_], a[:, :tt_], gate[:, mc:mc + 1], rr[:, :tt_], ALU.mult, ALU.add)
                P.dma(yT[mc * 128:(mc + 1) * 128, t0 + tt:t0 + tt + tt_], o[:, :tt_], r=[o], q="gpsimd", is_out=True)
                cnt += 1
    P.end_stage()


def stage_normfm(P, xT, modT, g_d, shift_row, scale_row, hT, T, D=2048, eps=1e-6, TT=512):
    KC = D // 128
    g = P.sb("g_s", [128, KC]); P.dma(g[:], g_d, w=[g])
    A = P.sb("A_s", [128, KC]); B = P.sb("B_s", [128, KC])
    if modT is None:
        P.copy(A[:], g[:])
        P.memset(B[:], 0.0)
    else:
        mv = modT.rearrange("(c p) o -> p (c o)", p=128)
        P.dma(A[:], mv[:, scale_row * KC:(scale_row + 1) * KC], w=[A], allow_slow_non_contiguous=True)
        P.dma(B[:], mv[:, shift_row * KC:(shift_row + 1) * KC], w=[B], allow_slow_non_contiguous=True)
        P.stt(A[:], A[:], 1.0, g[:], ALU.add, ALU.mult)
    ones = P.sb("ones_s", [128, 128]); P.memset(ones[:], 1.0 / D)
    xs = [P.sb("xs%d" % i, [128, KC, TT]) for i in range(2)]
    sq = [P.sb("sq%d" % i, [128, TT]) for i in range(2)]
    ps = [P.ps("ps%d" % i, [128, TT]) for i in range(2)]
    rstd = [P.sb("rstd%d" % i, [128, TT]) for i in range(2)]
    tm = [P.sb("tm%d" % i, [128, TT]) for i in range(2)]
    ho = [P.sb("ho%d" % i, [128, TT]) for i in range(3)]
    xv = xT.rearrange("(kc p) t -> p kc t", p=128)
    n = 0
    k = 0
    for t0 in range(0, T, TT):
        tl = min(TT, T - t0)
        i = n % 2; n += 1
        x = xs[i]
        for kc in range(KC):
            P.dma(x[:, kc, :tl], xv[:, kc, t0:t0 + tl], w=[x])
        for kc in range(KC):
            q = sq[kc % 2]
            P.act(q[:, :tl], x[:, kc, :tl], AF.Square)
            P.mm(ps[i][:, :tl], ones[:], q[:, :tl], start=(kc == 0), stop=(kc == KC - 1))
        P.ts(rstd[i][:, :tl], ps[i][:, :tl], eps, None, ALU.add)
        P.act(rstd[i][:, :tl], rstd[i][:, :tl], AF.Sqrt)
        P.recip(rstd[i][:, :tl], rstd[i][:, :tl])
        for kc in range(KC):
            t_ = tm[kc % 2]
            o = ho[k % 3]; k += 1
            P.tt(t_[:, :tl], x[:, kc, :tl], rstd[i][:, :tl], ALU.mult)
            P.act(o[:, :tl], t_[:, :tl], AF.Identity, scale=A[:, kc:kc + 1], bias=B[:, kc:kc + 1])
            P.dma(hT[kc * 128:(kc + 1) * 128, t0:t0 + tl], o[:, :tl], r=[o], q="gpsimd", is_out=True)
    P.end_stage()


def stage_att(P, zT, pos_bc, invf_d, sgn_d, maskg_d, mask0_d, sink_d, ident_d, attT, TQ, NH=16, NKV=4):
    import math
    TK = TQ + 128
    NB = TQ // 128
    QC = NH // 2
    QOFF, KOFF, VOFF = 0, NH * 64, NH * 64 + NKV * 64
    invf = P.sb("invf_s", [128, 1]); P.dma(invf[:], invf_d, w=[invf])
    sgn = P.sb("sgn_s", [128, 1]); P.dma(sgn[:], sgn_d, w=[sgn])
    maskg = P.sb("maskg_s", [128, 256]); P.dma(maskg[:], maskg_d, w=[maskg])
    mask0 = P.sb("mask0_s", [128, 256]); P.dma(mask0[:], mask0_d, w=[mask0])
    sink = P.sb("sink_s", [128, NH]); P.dma(sink[:], sink_d, w=[sink])
    identf = P.sb("identf", [128, 128]); P.dma(identf[:], ident_d, w=[identf])
    ident = P.sb("identb", [128, 128], BF16); P.copy(ident[:], identf[:])

    bufA = P.sb("bufA", [128, TK])
    bufB = P.sb("bufB", [128, TK])
    S = P.sb("S", [128, TK])
    C = P.sb("C", [128, TK])
    posi = bufB[:].bitcast(I32)
    P.dma(posi, pos_bc, w=[bufB])
    ang = bufA
    P.copy(ang[:], posi)
    P.ts(ang[:], ang[:], invf[:, 0:1], None, ALU.mult)
    TWO_PI = 2.0 * math.pi
    C1 = 6.28125
    C2 = TWO_PI - C1
    Wt = TK // 4
    for q4 in range(0, TK, Wt):
        wl = min(Wt, TK - q4)
        kf = bufB[:, 0:wl]
        ki = bufB[:, Wt:Wt + wl].bitcast(I32)
        yy = bufB[:, 2 * Wt:2 * Wt + wl]
        mw = bufB[:, 3 * Wt:3 * Wt + wl]
        for which, dst in ((0, S), (1, C)):
            src = ang[:, q4:q4 + wl]
            if which == 1:
                P.ts(yy, src, math.pi / 2.0, None, ALU.add)
                src = yy
            P.ts(kf, src, 1.0 / TWO_PI, None, ALU.mult)
            P.copy(ki, kf)
            P.copy(kf, ki)
            P.stt(yy, kf, -C1, src, ALU.mult, ALU.add)
            P.stt(yy, kf, -C2, yy, ALU.mult, ALU.add)
            P.ts(mw, yy, math.pi, -TWO_PI, ALU.is_gt, ALU.mult)
            P.tt(yy, yy, mw, ALU.add)
            P.ts(mw, yy, -math.pi, TWO_PI, ALU.is_lt, ALU.mult)
            P.tt(yy, yy, mw, ALU.add)
            P.ts(yy, yy, math.pi, -math.pi, ALU.min, ALU.max)
            if which == 0:
                P.act(dst[:, q4:q4 + wl], yy, AF.Sin, scale=sgn[:, 0:1])
            else:
                P.act(dst[:, q4:q4 + wl], yy, AF.Sin)

    kr = P.sb("kr", [128, NKV, TK], BF16)
    vb = P.sb("vb", [128, NB + 1, NKV * 64], BF16)
    for g in range(NKV):
        r0 = KOFF + g * 64
        for dup in range(2):
            P.dma(bufA[dup * 64:(dup + 1) * 64, :], zT[r0:r0 + 64, :], w=[bufA])
            for half in range(2):
                P.dma(bufB[dup * 64 + half * 32:dup * 64 + half * 32 + 32, :],
                      zT[r0 + (1 - half) * 32:r0 + (1 - half) * 32 + 32, :], w=[bufB])
        P.tt(bufA[:], bufA[:], C[:], ALU.mult)
        P.tt(bufB[:], bufB[:], S[:], ALU.mult, eng="gpsimd")
        P.tt(kr[:, g, :], bufA[:], bufB[:], ALU.add)
    ps_s = [P.ps("ps_s%d" % i, [128, 256]) for i in range(2)]
    VC = NKV * 64 // 128
    vt = [P.sb("vt%d" % i, [128, VC, 128]) for i in range(2)]
    vv = zT[VOFF:VOFF + NKV * 64, :].rearrange("(c p) t -> p c t", p=128)
    for blk in range(NB + 1):
        v_ = vt[blk % 2]
        P.dma(v_[:], vv[:, :, blk * 128:(blk + 1) * 128], w=[v_])
        for c in range(VC):
            P.tr(ps_s[blk % 2][:, c * 128:(c + 1) * 128], v_[:, c, :], identf[:])
        P.copy(vb[:, blk, :], ps_s[blk % 2][:, 0:VC * 128], eng="scalar")

    GS = min(512, TQ)
    qa = [P.sb("qa%d" % i, [128, GS]) for i in range(2)]
    qb = [P.sb("qb%d" % i, [128, GS]) for i in range(2)]
    qr = [P.sb("qr%d" % i, [128, QC, GS], BF16) for i in range(2)]
    ps_t = [P.ps("ps_t%d" % i, [128, 2, 128], BF16) for i in range(2)]
    ps_o = [P.ps("ps_o%d" % i, [128, 128]) for i in range(2)]
    sm = [P.sb("sm%d" % i, [128, 256]) for i in range(2)]
    pe_ = [P.sb("pe%d" % i, [128, 256]) for i in range(2)]
    pb = [P.sb("pb%d" % i, [128, 256], BF16) for i in range(2)]
    pT = [P.sb("pT%d" % i, [128, 2, 128], BF16) for i in range(2)]
    st = [[P.sb("st%s%d" % (nm, i), [128, 1]) for i in range(2)] for nm in ("mx", "ng", "rs", "es")]
    ao = [P.sb("ao%d" % i, [128, QC, 128]) for i in range(2)]
    attv = attT.rearrange("(c p) t -> p c t", p=128)
    u = 0
    n = 0
    for g0 in range(0, TQ, GS):
        qrg = qr[(g0 // GS) % 2]
        for c in range(QC):
            a, b = qa[n % 2], qb[n % 2]; n += 1
            r0 = QOFF + c * 128
            P.dma(a[:], zT[r0:r0 + 128, 128 + g0:128 + g0 + GS], w=[a])
            for hh in range(2):
                for half in range(2):
                    P.dma(b[hh * 64 + half * 32:hh * 64 + half * 32 + 32, :],
                          zT[r0 + hh * 64 + (1 - half) * 32:r0 + hh * 64 + (1 - half) * 32 + 32, 128 + g0:128 + g0 + GS], w=[b])
            P.tt(a[:], a[:], C[:, 128 + g0:128 + g0 + GS], ALU.mult)
            P.tt(b[:], b[:], S[:, 128 + g0:128 + g0 + GS], ALU.mult, eng="gpsimd")
            P.tt(qrg[:, c, :], a[:], b[:], ALU.add)
        for jl in range(GS // 128):
            j = g0 // 128 + jl
            msk = mask0 if j == 0 else maskg
            aoj = ao[j % 2]
            for c in range(QC):
                po = ps_o[c % 2]
                for hh in range(2):
                    h = 2 * c + hh
                    g = h // (NH // NKV)
                    i2 = u % 2; u += 1
                    pb0 = hh * 64
                    mx, ng, rs, es = st[0][i2], st[1][i2], st[2][i2], st[3][i2]
                    P.mm(ps_s[i2][:], qrg[pb0:pb0 + 64, c, jl * 128:(jl + 1) * 128], kr[pb0:pb0 + 64, g, j * 128:j * 128 + 256])
                    P.stt(sm[i2][:], ps_s[i2][:], 0.125, msk[:], ALU.mult, ALU.add)
                    P.reduce(mx[:], sm[i2][:], ALU.max)
                    P.ts(ng[:], mx[:], sink[:, h:h + 1], -1.0, ALU.max, ALU.mult)
                    P.act(pe_[i2][:], sm[i2][:], AF.Exp, bias=ng[:, 0:1], accum_out=rs[:])
                    P.act(es[:], sink[:, h:h + 1], AF.Exp, bias=ng[:, 0:1])
                    P.tt(rs[:], rs[:], es[:], ALU.add)
                    P.recip(rs[:], rs[:])
                    P.ts(pb[i2][:], pe_[i2][:], rs[:, 0:1], None, ALU.mult)
                    for kc in range(2):
                        P.tr(ps_t[i2][:, kc, :], pb[i2][:, kc * 128:(kc + 1) * 128], ident[:])
                    P.copy(pT[i2][:], ps_t[i2][:], eng="scalar")
                    for kc in range(2):
                        P.mm(po[pb0:pb0 + 64, :], vb[:, j + kc, g * 64:(g + 1) * 64], pT[i2][:, kc, :],
                             start=(kc == 0), stop=(kc == 1))
                P.copy(aoj[:, c, :], po[:], eng="vector")
            P.dma(attv[:, :, j * 128:(j + 1) * 128], aoj[:], r=[aoj], q="gpsimd", is_out=True)
    P.end_stage()


def stage_conv(P, valT, gateT, cw_d, cb_d, lg_d, lb_d, flag_d, outT, TQ, CH=1024, W=31, ln_eps=1e-5, TT=512):
    CC = CH // 128
    HL = W - 1
    TT = min(TT, TQ)
    cw = P.sb("cw_s", [128, CC, W]); P.dma(cw[:], cw_d, w=[cw])
    cb = P.sb("cb_s", [128, CC]); P.dma(cb[:], cb_d, w=[cb])
    lg = P.sb("lg_s", [128, CC]); P.dma(lg[:], lg_d, w=[lg])
    lb = P.sb("lb_s", [128, CC]); P.dma(lb[:], lb_d, w=[lb])
    flag = P.sb("flag_s", [128, 1]); P.dma(flag[:], flag_d, w=[flag])
    ones = P.sb("ones_s", [128, 128]); P.memset(ones[:], 1.0 / CH)
    va = [P.sb("va%d" % i, [128, TT + HL]) for i in range(2)]
    ga = [P.sb("ga%d" % i, [128, TT + HL]) for i in range(2)]
    a1 = [P.sb("a1_%d" % i, [128, TT]) for i in range(2)]
    yc = [P.sb("yc%d" % c, [128, TT]) for c in range(CC)]
    sq = [P.sb("sq%d" % i, [128, TT]) for i in range(2)]
    ps_m = P.ps("ps_m", [128, TT])
    ps_q = P.ps("ps_q", [128, TT])
    mean = P.sb("mean", [128, TT])
    rstd = P.sb("rstd", [128, TT])
    ot = [P.sb("cot%d" % i, [128, TT]) for i in range(2)]
    n = 0
    for t0 in range(0, TQ, TT):
        for c in range(CC):
            i2 = n % 2; n += 1
            v, g = va[i2], ga[i2]
            P.dma(v[:], valT[c * 128:(c + 1) * 128, t0:t0 + TT + HL], w=[v])
            P.dma(g[:], gateT[c * 128:(c + 1) * 128, t0:t0 + TT + HL], w=[g])
            P.act(g[:], g[:], AF.Sigmoid)
            P.tt(v[:], v[:], g[:], ALU.mult)
            if t0 == 0:
                P.ts(v[:, 0:HL], v[:, 0:HL], flag[:, 0:1], None, ALU.mult)
            P.ts(a1[i2][:], v[:, 0:TT], cw[:, c, 0:1], None, ALU.mult)
            for j in range(1, W):
                P.stt(a1[i2][:], v[:, j:j + TT], cw[:, c, j:j + 1], a1[i2][:], ALU.mult, ALU.add)
            P.ts(yc[c][:], a1[i2][:], cb[:, c:c + 1], None, ALU.add)
            s = sq[c % 2]
            P.act(s[:], yc[c][:], AF.Square)
            P.mm(ps_m[:], ones[:], yc[c][:], start=(c == 0), stop=(c == CC - 1))
            P.mm(ps_q[:], ones[:], s[:], start=(c == 0), stop=(c == CC - 1))
        P.copy(mean[:], ps_m[:])
        P.tt(rstd[:], mean[:], mean[:], ALU.mult)
        P.tt(rstd[:], ps_q[:], rstd[:], ALU.subtract)
        P.ts(rstd[:], rstd[:], ln_eps, None, ALU.add)
        P.act(rstd[:], rstd[:], AF.Sqrt)
        P.recip(rstd[:], rstd[:])
        for c in range(CC):
            o = ot[c % 2]
            P.tt(yc[c][:], yc[c][:], mean[:], ALU.subtract)
            P.tt(yc[c][:], yc[c][:], rstd[:], ALU.mult, eng="gpsimd")
            P.act(o[:], yc[c][:], AF.Silu, scale=lg[:, c:c + 1], bias=lb[:, c:c + 1])
            P.dma(outT[c * 128:(c + 1) * 128, t0:t0 + TT], o[:], r=[o], q="gpsimd", is_out=True)
    P.end_stage()


def stage_route(P, hT, rw_d, rb_d, ident_d, gatesT, T, D=2048, E=16, G=4, TS=1024):
    KC = D // 128
    EG = E // G
    TS = min(TS, T)
    hv = hT.rearrange("(kc p) t -> p kc t", p=128)
    rw = P.sb("rw_s", [128, KC, E]); P.dma(rw[:], rw_d, w=[rw])
    rb = P.sb("rb_s", [128, E]); P.dma(rb[:], rb_d, w=[rb])
    identf = P.sb("identf", [128, 128]); P.dma(identf[:], ident_d, w=[identf])
    hs = [P.sb("hs%d" % i, [128, KC, TS]) for i in range(1)]
    ps = [P.ps("psl%d" % i, [128, E]) for i in range(2)]
    psT = [P.ps("psT%d" % i, [E, 128]) for i in range(2)]
    def t(name, shape):
        return [P.sb("%s%d" % (name, i), shape) for i in range(2)]
    lg, pr, sel, sel2, msk = t("lg", [128, E]), t("pr", [128, E]), t("sel", [128, E]), t("sel2", [128, E]), t("msk", [128, E])
    mx, sm, m1, m2, gs, gm, gsel = t("mx", [128, 1]), t("sm", [128, 1]), t("m1", [128, G]), t("m2", [128, G]), t("gs", [128, G]), t("gm", [128, 1]), t("gsel", [128, G])
    og = t("og", [128, E])
    ogT = t("ogT", [E, 128])
    n = 0
    for t0 in range(0, T, TS):
        h = hs[0]
        for kc in range(KC):
            P.dma(h[:, kc, :], hv[:, kc, t0:t0 + TS], w=[h])
        for tt in range(0, TS, 128):
            i = n % 2; n += 1
            for kc in range(KC):
                P.mm(ps[i][:], h[:, kc, tt:tt + 128], rw[:, kc, :], start=(kc == 0), stop=(kc == KC - 1))
            P.copy(lg[i][:], ps[i][:])
            P.reduce(mx[i][:], lg[i][:], ALU.max)
            P.ts(mx[i][:], mx[i][:], -1.0, None, ALU.mult)
            P.act(pr[i][:], lg[i][:], AF.Exp, bias=mx[i][:, 0:1], accum_out=sm[i][:])
            P.recip(sm[i][:], sm[i][:])
            P.ts(pr[i][:], pr[i][:], sm[i][:, 0:1], None, ALU.mult)
            P.tt(sel[i][:], pr[i][:], rb[:], ALU.add)
            s3 = sel[i][:].rearrange("p (g e) -> p g e", g=G)
            s23 = sel2[i][:].rearrange("p (g e) -> p g e", g=G)
            k3 = msk[i][:].rearrange("p (g e) -> p g e", g=G)
            P.reduce(m1[i][:], s3, ALU.max)
            m1b = m1[i][:].unsqueeze(2).broadcast_to([128, G, EG])
            P.tt(s23, s3, m1b, ALU.is_equal)
            P.stt(sel2[i][:], sel2[i][:], -1e9, sel[i][:], ALU.mult, ALU.add)
            P.reduce(m2[i][:], s23, ALU.max)
            P.tt(gs[i][:], m1[i][:], m2[i][:], ALU.add)
            P.reduce(gm[i][:], gs[i][:], ALU.max)
            P.ts(gsel[i][:], gs[i][:], gm[i][:, 0:1], None, ALU.is_equal)
            m2b = m2[i][:].unsqueeze(2).broadcast_to([128, G, EG])
            P.tt(k3, s3, m2b, ALU.is_ge)
            gselb = gsel[i][:].unsqueeze(2).broadcast_to([128, G, EG])
            P.tt(k3, k3, gselb, ALU.mult)
            P.tt(og[i][:], pr[i][:], msk[i][:], ALU.mult)
            P.reduce(sm[i][:], og[i][:], ALU.add)
            P.recip(sm[i][:], sm[i][:])
            P.ts(og[i][:], og[i][:], sm[i][:, 0:1], None, ALU.mult)
            P.tr(psT[i][:], og[i][:], identf[:])
            P.copy(ogT[i][:], psT[i][:], eng="scalar")
            P.dma(gatesT[:, t0 + tt:t0 + tt + 128], ogT[i][:], r=[ogT[i]], q="gpsimd", is_out=True)
    P.end_stage()


def _swap_half(a):
    sh = a.shape
    a4 = a.reshape(sh[:-1] + (sh[-1] // 64, 2, 32))
    return np.ascontiguousarray(a4[..., ::-1, :]).reshape(sh)


def att_inmaps(q, k, v, pos, sinks, TQ):
    import math
    B, S, _ = q.shape
    per = S // TQ
    half = 32
    invf = (10000.0 ** (-np.arange(half, dtype=np.float32) / half)).astype(np.float32)
    invf128 = np.tile(invf, 4).reshape(128, 1).astype(np.float32)
    sgn = np.tile(np.concatenate([-np.ones(32), np.ones(32)]), 2).reshape(128, 1).astype(np.float32)
    qi = np.arange(128)[:, None]
    kj = np.arange(256)[None, :]
    dist = 128 + qi - kj
    valid = (dist >= 0) & (dist < 128)
    maskg = np.where(valid, 0.0, -1e30).astype(np.float32)
    mask_first = np.where(valid & (kj >= 128), 0.0, -1e30).astype(np.float32)
    ident = np.eye(128, dtype=np.float32)
    sink_bc = np.ascontiguousarray(np.broadcast_to(sinks[None, :], (128, sinks.shape[0]))).astype(np.float32)
    qs = _swap_half(q)
    ks = _swap_half(k)
    maps = []
    for b in range(B):
        for c in range(per):
            t0 = c * TQ
            def halo(a):
                if c == 0:
                    return np.concatenate([np.zeros((128,) + a.shape[2:], a.dtype), a[b, 0:TQ]], 0)
                return a[b, t0 - 128:t0 + TQ]
            kh, ksh, vh = halo(k), halo(ks), halo(v)
            if c == 0:
                ph = np.concatenate([np.zeros((128,), np.int32), pos[b, 0:TQ]])
            else:
                ph = pos[b, t0 - 128:t0 + TQ]
            def dup(a):
                aT = a.T.reshape(4, 64, -1)
                return np.ascontiguousarray(np.concatenate([aT, aT], 1).reshape(512, -1))
            maps.append({
                "qT": np.ascontiguousarray(q[b, t0:t0 + TQ].T), "qsT": np.ascontiguousarray(qs[b, t0:t0 + TQ].T),
                "kdT": dup(kh), "ksdT": dup(ksh), "vtm": np.ascontiguousarray(vh),
                "pos_bc": np.ascontiguousarray(np.broadcast_to(ph[None, :], (128, ph.shape[0]))).astype(np.int32),
                "invf": invf128, "sgn": sgn, "maskg": maskg, "mask0": mask_first if c == 0 else maskg,
                "sink_bc": sink_bc, "ident": ident})
    return maps


def pl(v):
    return np.ascontiguousarray(np.asarray(v, np.float32).reshape(-1, 128).T)


def conv_inmaps(u, conv_w, conv_b, ln_g, ln_b, TQ):
    B, S, _ = u.shape
    per = S // TQ
    CH = 1024
    W = conv_w.shape[0]
    cw = np.ascontiguousarray(conv_w.reshape(W, CH // 128, 128).transpose(2, 1, 0)).astype(np.float32)
    maps = []
    for b in range(B):
        for c in range(per):
            t0 = c * TQ
            if c == 0:
                seg = np.concatenate([np.zeros((W - 1, 2 * CH), np.float32), u[b, 0:TQ]], 0)
            else:
                seg = u[b, t0 - (W - 1):t0 + TQ]
            maps.append({"valT": np.ascontiguousarray(seg[:, :CH].T), "gateT": np.ascontiguousarray(seg[:, CH:].T),
                         "cw": cw, "cb": pl(conv_b), "lg": pl(ln_g), "lb": pl(ln_b)})
    return maps


def stage_moe(P, hT, xT, gatesT, w1, w3, w2, modT, gate_row, oT, T, D=2048, DE=1024, E=16, TS=512):
    KC, JC = D // 128, DE // 128
    TS = min(TS, T)
    hv = hT.rearrange("(kc p) t -> p kc t", p=128)
    mv = modT.rearrange("(c p) o -> p (c o)", p=128)
    gf = P.sb("gf_s", [128, KC]); P.dma(gf[:], mv[:, gate_row * KC:(gate_row + 1) * KC], w=[gf], allow_slow_non_contiguous=True)
    hb = P.sb("hb", [128, KC, TS], BF16)
    hst = [P.sb("hst%d" % i, [128, TS]) for i in range(2)]
    yacc = [P.sb("yacc%d" % f, [128, TS]) for f in range(KC)]
    hid = [P.sb("hid%d" % j, [128, TS], BF16) for j in range(JC)]
    ge = [P.sb("ge%d" % i, [128, TS]) for i in range(2)]
    wst = [P.sb("wst%d" % i, [128, KC, 128]) for i in range(4)]
    wbf = [P.sb("wbf%d" % i, [128, KC, 128], BF16) for i in range(4)]
    w2st = [P.sb("w2st%d" % i, [128, JC, 128]) for i in range(2)]
    w2bf = [P.sb("w2bf%d" % i, [128, JC, 128], BF16) for i in range(2)]
    sa = [P.sb("sa%d" % i, [128, TS]) for i in range(2)]
    ps_a = [P.ps("ps_a%d" % i, [128, TS]) for i in range(2)]
    ps_b = [P.ps("ps_b%d" % i, [128, TS]) for i in range(2)]
    ps_y = [P.ps("ps_y%d" % i, [128, TS]) for i in range(2)]
    xt = [P.sb("xt%d" % i, [128, TS]) for i in range(2)]
    n1 = n2 = 0
    for t0 in range(0, T, TS):
        for kc in range(KC):
            s = hst[kc % 2]
            P.dma(s[:], hv[:, kc, t0:t0 + TS], w=[s])
            P.copy(hb[:, kc, :], s[:], eng="vector")
        for e in range(E):
            g = ge[e % 2]
            P.dma(g[:], gatesT[e:e + 1, t0:t0 + TS].partition_broadcast(128), w=[g])
            for jc in range(JC):
                i = n1 % 2; n1 += 1
                P.dma(wst[2 * i][:], w1[e, jc], w=[wst[2 * i]])
                P.dma(wst[2 * i + 1][:], w3[e, jc], w=[wst[2 * i + 1]])
                P.copy(wbf[2 * i][:], wst[2 * i][:], eng="gpsimd")
                P.copy(wbf[2 * i + 1][:], wst[2 * i + 1][:], eng="scalar")
                for kc in range(KC):
                    P.mm(ps_a[i][:], wbf[2 * i][:, kc, :], hb[:, kc, :], start=(kc == 0), stop=(kc == KC - 1))
                for kc in range(KC):
                    P.mm(ps_b[i][:], wbf[2 * i + 1][:, kc, :], hb[:, kc, :], start=(kc == 0), stop=(kc == KC - 1))
                P.act(sa[i][:], ps_a[i][:], AF.Silu)
                P.tt(sa[i][:], sa[i][:], ps_b[i][:], ALU.mult)
                P.tt(hid[jc][:], sa[i][:], g[:], ALU.mult)
            for fc in range(KC):
                i = n2 % 2; n2 += 1
                P.dma(w2st[i][:], w2[e, fc], w=[w2st[i]])
                P.copy(w2bf[i][:], w2st[i][:], eng="gpsimd")
                for jc in range(JC):
                    P.mm(ps_y[i][:], w2bf[i][:, jc, :], hid[jc][:], start=(jc == 0), stop=(jc == JC - 1))
                if e == 0:
                    P.copy(yacc[fc][:], ps_y[i][:])
                else:
                    P.tt(yacc[fc][:], yacc[fc][:], ps_y[i][:], ALU.add)
        for fc in range(KC):
            x_ = xt[fc % 2]
            P.dma(x_[:], xT[fc * 128:(fc + 1) * 128, t0:t0 + TS], w=[x_])
            P.stt(x_[:], yacc[fc][:], gf[:, fc:fc + 1], x_[:], ALU.mult, ALU.add)
            P.dma(oT[fc * 128:(fc + 1) * 128, t0:t0 + TS], x_[:], r=[x_], q="gpsimd", is_out=True)
    P.end_stage()


def moe_weights_layout(w1, w3, w2):
    E, D, DE = w1.shape
    KC, JC = D // 128, DE // 128
    f = lambda w: np.ascontiguousarray(w.reshape(E, KC, 128, JC, 128).transpose(0, 3, 2, 1, 4))
    w2r = np.ascontiguousarray(w2.reshape(E, JC, 128, KC, 128).transpose(0, 3, 2, 1, 4))
    return f(w1), f(w3), w2r


def build_block0(TQ, D=2048):
    P = Prog()
    TK = TQ + 128
    AB_IN = 3584
    xhT = P.dram_in("xhT", [D, TK])
    cT = P.dram_in("cT", [D, 1])
    ada_w = P.dram_in("ada_w", [D, 6 * D])
    ada_b = P.dram_in("ada_b", [128, 6 * D // 128])
    g_mix = P.dram_in("g_mix", [128, D // 128])
    g_ffn = P.dram_in("g_ffn", [128, D // 128])
    w_in = P.dram_in("w_in", [D, AB_IN])
    w_out = P.dram_in("w_out", [D, D])
    pos_bc = P.dram_in("pos_bc", [128, TK], I32)
    invf = P.dram_in("invf", [128, 1])
    sgn = P.dram_in("sgn", [128, 1])
    maskg = P.dram_in("maskg", [128, 256])
    mask0 = P.dram_in("mask0", [128, 256])
    sink_bc = P.dram_in("sink_bc", [128, 16])
    ident = P.dram_in("ident", [128, 128])
    cw = P.dram_in("cw", [128, 8, 31])
    cb = P.dram_in("cb", [128, 8])
    lg = P.dram_in("lg", [128, 8])
    lb = P.dram_in("lb", [128, 8])
    flag = P.dram_in("flag", [128, 1])
    rw = P.dram_in("rw", [128, 16, 16])
    rb = P.dram_in("rb_bc", [128, 16])
    w1 = P.dram_in("w1r", [16, 8, 128, 16, 128])
    w3 = P.dram_in("w3r", [16, 8, 128, 16, 128])
    w2 = P.dram_in("w2r", [16, 16, 128, 8, 128])
    x2T = P.dram_out("x2T", [D, TQ])
    modT = P.dram_tmp("modT", [6 * D, 1])
    hT = P.dram_tmp("hT", [D, TK])
    zT = P.dram_tmp("zT", [AB_IN, TK])
    mixT = P.dram_tmp("mixT", [D, TQ])
    x1T = P.dram_tmp("x1T", [D, TQ])
    hfT = P.dram_tmp("hfT", [D, TQ])
    gatesT = P.dram_tmp("gatesT", [16, TQ])
    mv = modT.rearrange("(c p) o -> p (c o)", p=128)
    stage_lin(P, cT, ada_w, modT, K=D, M=6 * D, T=1, mode="bias", fp32=True, in_silu=True, bias_d=ada_b)
    stage_normfm(P, xhT, modT, g_mix, 0, 1, hT, TK)
    stage_lin(P, hT, w_in, zT, K=D, M=AB_IN, T=TK)
    stage_att(P, zT, pos_bc, invf, sgn, maskg, mask0, sink_bc, ident, mixT[0:1024, :], TQ)
    stage_conv(P, zT[1536:2560, 98:TK], zT[2560:3584, 98:TK], cw, cb, lg, lb, flag, mixT[1024:2048, :], TQ)
    stage_lin(P, mixT, w_out, x1T, K=D, M=D, T=TQ, mode="resid", gate_d=mv[:, 32:48], rT=xhT[:, 128:TK])
    stage_normfm(P, x1T, modT, g_ffn, 3, 4, hfT, TQ)
    stage_route(P, hfT, rw, rb, ident, gatesT, TQ)
    stage_moe(P, hfT, x1T, gatesT, w1, w3, w2, modT, 5, x2T, TQ)
    return P


def block0_inmaps(inp, TQ, layer=0):
    x = np.asarray(inp["x"], np.float32)
    B, S, D = x.shape
    per = S // TQ
    pos = np.asarray(inp["positions"], np.int32)
    j = layer // 2
    invf = (10000.0 ** (-np.arange(32, dtype=np.float32) / 32)).astype(np.float32)
    invf128 = np.tile(invf, 4).reshape(128, 1).astype(np.float32)
    sgn = np.tile(np.concatenate([-np.ones(32), np.ones(32)]), 2).reshape(128, 1).astype(np.float32)
    qi = np.arange(128)[:, None]
    kj = np.arange(256)[None, :]
    dist = 128 + qi - kj
    valid = (dist >= 0) & (dist < 128)
    maskg = np.where(valid, 0.0, -1e30).astype(np.float32)
    mask_first = np.where(valid & (kj >= 128), 0.0, -1e30).astype(np.float32)
    ident = np.eye(128, dtype=np.float32)
    sinks = np.asarray(inp["ab_sinks"][j], np.float32)
    sink_bc = np.ascontiguousarray(np.broadcast_to(sinks[None, :], (128, 16))).astype(np.float32)
    conv_w = np.asarray(inp["ab_conv_w"][j], np.float32).reshape(31, 1024)
    cw = np.ascontiguousarray(conv_w.reshape(31, 8, 128).transpose(2, 1, 0))
    w1r, w3r, w2r = moe_weights_layout(np.asarray(inp["moe_w1"][layer]), np.asarray(inp["moe_w3"][layer]), np.asarray(inp["moe_w2"][layer]))
    rwl = np.ascontiguousarray(np.asarray(inp["router_w"], np.float32).reshape(16, 128, 16).transpose(1, 0, 2))
    rbb = np.ascontiguousarray(np.broadcast_to(np.asarray(inp["router_bias"], np.float32)[None], (128, 16)))
    common = {
        "ada_w": np.ascontiguousarray(inp["ada_w"][layer]), "ada_b": pl(inp["ada_b"][layer]),
        "g_mix": pl(inp["norm_mix"][layer]), "g_ffn": pl(inp["norm_ffn"][layer]),
        "w_in": np.ascontiguousarray(inp["ab_w_in"][j]), "w_out": np.ascontiguousarray(inp["ab_w_out"][j]),
        "invf": invf128, "sgn": sgn, "maskg": maskg, "sink_bc": sink_bc, "ident": ident,
        "cw": cw, "cb": pl(inp["ab_conv_b"][j]), "lg": pl(inp["ab_conv_ln_g"][j]), "lb": pl(inp["ab_conv_ln_b"][j]),
        "rw": rwl, "rb_bc": rbb, "w1r": w1r, "w3r": w3r, "w2r": w2r,
    }
    maps = []
    for b in range(B):
        for c in range(per):
            t0 = c * TQ
            if c == 0:
                xh = np.concatenate([np.zeros((128, D), np.float32), x[b, 0:TQ]], 0)
                ph = np.concatenate([np.zeros((128,), np.int32), pos[b, 0:TQ]])
            else:
                xh = x[b, t0 - 128:t0 + TQ]
                ph = pos[b, t0 - 128:t0 + TQ]
            m = dict(common)
            m["xhT"] = np.ascontiguousarray(xh.T)
            m["cT"] = np.ascontiguousarray(np.asarray(inp["c"], np.float32)[b].reshape(D, 1))
            m["pos_bc"] = np.ascontiguousarray(np.broadcast_to(ph[None, :], (128, ph.shape[0]))).astype(np.int32)
            m["mask0"] = mask_first if c == 0 else maskg
            m["flag"] = np.full((128, 1), 0.0 if c == 0 else 1.0, np.float32)
            maps.append(m)
    return maps


CH = 64


def _lerp_load(P, dst, tmp, src_rows, t0, tl, mu_col, np_):
    if t0 == 0:
        P.memset(tmp[:np_, 0:1], 0.0)
        P.dma(tmp[:np_, 1:tl + 1], src_rows[:, 0:tl], w=[tmp])
    else:
        P.dma(tmp[:np_, 0:tl + 1], src_rows[:, t0 - 1:t0 + tl], w=[tmp])
    P.tt(dst[:np_, :tl], tmp[:np_, 0:tl], tmp[:np_, 1:tl + 1], ALU.subtract)
    P.stt(dst[:np_, :tl], dst[:np_, :tl], mu_col, tmp[:np_, 1:tl + 1], ALU.mult, ALU.add)


def stage_rwkv_prep(P, z1T, prm, scr, T, TT=512):
    import math
    NC = 8
    mu_rkv = P.sb("mu_rkv", [128, 24]); P.dma(mu_rkv[:], prm["mu_rkv"], w=[mu_rkv])
    mu_w = P.sb("mu_w", [96, 1]); P.dma(mu_w[:], prm["mu_w"], w=[mu_w])
    mu_a = P.sb("mu_a", [96, 1]); P.dma(mu_a[:], prm["mu_a"], w=[mu_a])
    mu_g = P.sb("mu_g", [128, 2]); P.dma(mu_g[:], prm["mu_g"], w=[mu_g])
    w0 = P.sb("w0", [128, NC]); P.dma(w0[:], prm["w0"], w=[w0])
    a0 = P.sb("a0", [128, NC]); P.dma(a0[:], prm["a0"], w=[a0])
    k_k = P.sb("k_k", [128, NC]); P.dma(k_k[:], prm["k_k"], w=[k_k])
    k_a = P.sb("k_a", [128, NC]); P.dma(k_a[:], prm["k_a"], w=[k_a])
    r_k = P.sb("r_k", [128, NC]); P.dma(r_k[:], prm["r_k"], w=[r_k])
    w2 = P.sb("w2", [96, 1024]); P.dma(w2[:], prm["w2"], w=[w2])
    a2 = P.sb("a2", [96, 1024]); P.dma(a2[:], prm["a2"], w=[a2])
    g2 = P.sb("g2", [128, 2, 1024]); P.dma(g2[:], prm["g2"].rearrange("(c p) m -> p c m", p=128), w=[g2])
    bones = P.sb("bones", [128, 128]); P.dma(bones[:], prm["bones"], w=[bones])
    cmask = P.sb("cmask", [128, TT]); P.dma(cmask[:], prm["cmask"], w=[cmask])
    tmp = [P.sb("tmp%d" % i, [128, TT + 1]) for i in range(2)]
    twT = P.sb("twT", [96, TT]); zaT = P.sb("zaT", [96, TT]); sgT = P.sb("sgT", [128, 2, TT])
    rl = P.sb("rl", [128, TT]); kl = P.sb("kl", [128, TT]); vl = P.sb("vl", [128, TT])
    ps_w = P.ps("ps_w", [128, TT]); ps_a = P.ps("ps_a", [128, TT]); ps_g = P.ps("ps_g", [128, TT])
    ps_s = P.ps("ps_s", [128, TT]); ps_r = P.ps("ps_r", [128, TT])
    def t(n):
        return P.sb(n, [128, TT])
    lw, av, gv, kk, kp, bb, L, e1, e2, t1, t2, o3f = [t(n) for n in
        ("lw", "av", "gv", "kk", "kp", "bb", "L", "e1", "e2", "t1", "t2", "o3f")]
    o1, o2, o3, o4, o5, o6, vb16 = [P.sb(n, [128, TT], BF16) for n in ("o1", "o2", "o3", "o4", "o5", "o6", "vb16")]
    elc = P.sb("elc", [128, TT // CH])
    CW = -math.exp(-0.5)
    nch = TT // CH
    for t0 in range(0, T, TT):
        _lerp_load(P, twT, tmp[0], z1T[3072:3168, :], t0, TT, mu_w[:, 0:1], 96)
        P.act(twT[:], twT[:], AF.Tanh)
        _lerp_load(P, zaT, tmp[1], z1T[3168:3264, :], t0, TT, mu_a[:, 0:1], 96)
        for c2 in range(2):
            _lerp_load(P, sgT[:, c2, :], tmp[c2], z1T[3264 + c2 * 128:3264 + (c2 + 1) * 128, :], t0, TT, mu_g[:, c2:c2 + 1], 128)
        P.act(sgT[:], sgT[:], AF.Sigmoid)
        for c in range(NC):
            rows = slice(c * 128, (c + 1) * 128)
            _lerp_load(P, rl, tmp[0], z1T[0 + c * 128:0 + (c + 1) * 128, :], t0, TT, mu_rkv[:, c:c + 1], 128)
            _lerp_load(P, kl, tmp[1], z1T[1024 + c * 128:1024 + (c + 1) * 128, :], t0, TT, mu_rkv[:, 8 + c:9 + c], 128)
            _lerp_load(P, vl, tmp[0], z1T[2048 + c * 128:2048 + (c + 1) * 128, :], t0, TT, mu_rkv[:, 16 + c:17 + c], 128)
            P.copy(vb16[:], vl[:], eng="gpsimd")
            P.dma(scr["vT"][rows, t0:t0 + TT], vb16[:], r=[vb16], q="gpsimd", is_out=True)
            P.mm(ps_w[:], w2[:, rows], twT[:])
            P.act(lw[:], ps_w[:], AF.Sigmoid, bias=w0[:, c:c + 1])
            P.ts(lw[:], lw[:], CW, None, ALU.mult)
            P.mm(ps_a[:], a2[:, rows], zaT[:])
            P.act(av[:], ps_a[:], AF.Sigmoid, bias=a0[:, c:c + 1])
            for c2 in range(2):
                P.mm(ps_g[:], g2[:, c2, rows], sgT[:, c2, :], start=(c2 == 0), stop=(c2 == 1))
            P.copy(gv[:], ps_g[:], eng="scalar")
            P.dma(scr["gT"][rows, t0:t0 + TT], gv[:], r=[gv], q="gpsimd", is_out=True)
            P.ts(kk[:], kl[:], k_k[:, c:c + 1], None, ALU.mult)
            P.act(t1[:], kk[:], AF.Square)
            P.mm(ps_s[:], bones[:], t1[:])
            P.ts(t1[:], ps_s[:], 1e-24, None, ALU.max)
            P.act(t1[:], t1[:], AF.Sqrt)
            P.recip(t1[:], t1[:])
            P.tt(kk[:], kk[:], t1[:], ALU.mult)
            P.ts(t2[:], av[:], -1.0, k_a[:, c:c + 1], ALU.add, ALU.mult)
            P.stt(kp[:], t2[:], 1.0, kl[:], ALU.add, ALU.mult)
            P.tt(bb[:], kk[:], av[:], ALU.mult)
            P.op("vector", lambda e, L=L, lw=lw: e.tensor_tensor_scan(L[:], cmask[:], lw[:], 0.0, ALU.mult, ALU.add),
                 r=[cmask, lw], w=[L])
            P.act(e1[:], L[:], AF.Exp)
            P.tt(o1[:], rl[:], e1[:], ALU.mult)
            P.dma(scr["rtT"][rows, t0:t0 + TT], o1[:], r=[o1], q="gpsimd", is_out=True)
            P.copy(elc[:], e1[:].rearrange("p (c s) -> p c s", s=CH)[:, :, CH - 1])
            P.dma(scr["eLC"][rows, t0 // CH:t0 // CH + nch], elc[:], r=[elc], q="gpsimd", is_out=True)
            P.tt(t1[:], L[:], lw[:], ALU.subtract)
            P.act(t1[:], t1[:], AF.Exp)
            P.stt(o2[:], kk[:], -1.0, t1[:], ALU.mult, ALU.mult)
            P.dma(scr["atT"][rows, t0:t0 + TT], o2[:], r=[o2], q="gpsimd", is_out=True)
            P.act(e2[:], L[:], AF.Exp, scale=-1.0)
            P.tt(o3[:], bb[:], e2[:], ALU.mult)
            P.dma(scr["btT"][rows, t0:t0 + TT], o3[:], r=[o3], q="gpsimd", is_out=True)
            P.tt(o4[:], kp[:], e2[:], ALU.mult)
            P.dma(scr["ktT"][rows, t0:t0 + TT], o4[:], r=[o4], q="gpsimd", is_out=True)
            L3 = L[:].rearrange("p (c s) -> p c s", s=CH)
            P.tt(t2[:].rearrange("p (c s) -> p c s", s=CH), L3[:, :, CH - 1:CH].broadcast_to([128, nch, CH]), L3, ALU.subtract)
            P.act(t2[:], t2[:], AF.Exp)
            P.tt(o5[:], bb[:], t2[:], ALU.mult)
            P.dma(scr["BhT"][rows, t0:t0 + TT], o5[:], r=[o5], q="gpsimd", is_out=True)
            P.tt(o6[:], kp[:], t2[:], ALU.mult)
            P.dma(scr["KhT"][rows, t0:t0 + TT], o6[:], r=[o6], q="gpsimd", is_out=True)
            P.stt(t1[:], rl[:], r_k[:, c:c + 1], kp[:], ALU.mult, ALU.mult)
            P.mm(ps_r[:], bones[:], t1[:])
            P.tt(o3f[:], ps_r[:], vl[:], ALU.mult)
            P.dma(scr["bonT"][rows, t0:t0 + TT], o3f[:], r=[o3f], q="gpsimd", is_out=True)
    P.end_stage()


def stage_rwkv_chunk(P, scr, masks_d, ident_d, yT, T, NH=16, SUP=512):
    U = 2
    identf = P.sb("identf", [128, 128]); P.dma(identf[:], ident_d, w=[identf])
    ident = P.sb("identb", [128, 128], BF16); P.copy(ident[:], identf[:])
    msk = P.sb("msk", [64, 320]); P.dma(msk[:], masks_d, w=[msk])
    ST = [P.sb("ST%d" % h, [64, 64]) for h in range(NH)]
    STb = [P.sb("STb%d" % h, [64, 64], BF16) for h in range(NH)]
    for h in range(NH):
        P.memset(ST[h][:], 0.0, eng="gpsimd")
        P.memset(STb[h][:], 0.0, eng="gpsimd")
    names = ("atT", "rtT", "btT", "ktT", "BhT", "KhT", "vT")
    nsc = SUP // CH
    bufs = {}
    for par in range(2):
        for j in range(U):
            for nm in names:
                bufs[(nm, par, j)] = P.sb("in_%s%d_%d" % (nm, par, j), [64, SUP], BF16)
            bufs[("elc", par, j)] = P.sb("in_elc%d_%d" % (par, j), [64, nsc])
            bufs[("y", par, j)] = P.sb("out_y%d_%d" % (par, j), [64, SUP])
    def dbl(name, shape, dt=BF16):
        return [[P.sb("%s%d_%d" % (name, i, j), shape, dt) for j in range(U)] for i in range(2)]
    bG = [P.ps("bG%d" % j, [64, 320]) for j in range(U)]
    bTM = P.ps("bTM", [64, U * 192], BF16)
    bP = P.ps("bP", [64, U * 64]); bQ = P.ps("bQ", [64, U * 64]); bX = P.ps("bX", [64, U * 64])
    bRU = P.ps("bRU", [64, U * 128]); bYS = P.ps("bYS", [64, U * 128])
    tm = dbl("tm", [64, 192]); gm = dbl("gm", [64, 320])
    Pm = [dbl("Pm%d_" % i, [64, 64]) for i in range(2)]
    Qm = [dbl("Qm%d_" % i, [64, 64]) for i in range(2)]
    Xm = [dbl("Xm%d_" % i, [64, 64]) for i in range(2)]
    r0s = dbl("r0s", [64, 64]); us = dbl("us", [64, 64])
    I64 = ident[0:64, 0:64]
    u = 0
    R = range(U)
    for s0 in range(0, T, SUP):
        for g in range(NH // U):
            par = g % 2
            hs = [g * U + j for j in R]
            for j in R:
                rows = slice(hs[j] * 64, (hs[j] + 1) * 64)
                for nm in names:
                    P.dma(bufs[(nm, par, j)][:], scr[nm][rows, s0:s0 + SUP], w=[bufs[(nm, par, j)]])
                P.dma(bufs[("elc", par, j)][:], scr["eLC"][rows, s0 // CH:s0 // CH + nsc], w=[bufs[("elc", par, j)]])
            B = [[bufs[(nm, par, j)] for nm in names] for j in R]
            for ci in range(nsc):
                i2 = u % 2; u += 1
                cs = slice(ci * CH, (ci + 1) * CH)
                for j in R:
                    at, rt, bt, kt, Bh, Kh, vT_ = B[j]
                    for k3, src in enumerate((vT_, Bh, Kh)):
                        P.tr(bTM[:, j * 192 + k3 * 64:j * 192 + (k3 + 1) * 64], src[:, cs], I64)
                for j in R:
                    at, rt, bt, kt, Bh, Kh, vT_ = B[j]
                    P.mm(bG[j][:, 0:64], bt[:, cs], at[:, cs])
                    P.mm(bG[j][:, 64:128], bt[:, cs], rt[:, cs])
                    P.mm(bG[j][:, 128:192], kt[:, cs], at[:, cs])
                    P.mm(bG[j][:, 192:256], kt[:, cs], rt[:, cs])
                    P.mm(bG[j][:, 256:320], at[:, cs], bt[:, cs])
                for j in R:
                    P.copy(tm[i2][j][:], bTM[:, j * 192:(j + 1) * 192], eng="scalar")
                    P.tt(gm[i2][j][:], bG[j][:], msk[:], ALU.mult)
                Vt = [tm[i2][j][:, 0:64] for j in R]; Bt = [tm[i2][j][:, 64:128] for j in R]; Kt = [tm[i2][j][:, 128:192] for j in R]
                P0 = [gm[i2][j][:, 0:64] for j in R]; NrbT = [gm[i2][j][:, 64:128] for j in R]
                MakT = [gm[i2][j][:, 128:192] for j in R]; NrkT = [gm[i2][j][:, 192:256] for j in R]
                Q0 = [gm[i2][j][:, 256:320] for j in R]
                Pc, Qc = list(P0), list(Q0)
                X = [Xm[0][i2][j] for j in R]
                for j in R:
                    P.tt(X[j][:], P0[j], I64, ALU.add, eng="gpsimd")
                for it in range(5):
                    Qn = [Qm[it % 2][i2][j] for j in R]
                    for j in R:
                        P.mm(bQ[:, j * 64:(j + 1) * 64], Pc[j], Qc[j])
                    if it < 4:
                        Pn = [Pm[it % 2][i2][j] for j in R]
                        for j in R:
                            P.mm(bP[:, j * 64:(j + 1) * 64], Qc[j], Pc[j])
                    for j in R:
                        P.copy(Qn[j][:], bQ[:, j * 64:(j + 1) * 64], eng="scalar")
                    if it < 4:
                        for j in R:
                            P.copy(Pn[j][:], bP[:, j * 64:(j + 1) * 64], eng="vector")
                    Xn = [Xm[(it + 1) % 2][i2][j] for j in R]
                    for j in R:
                        P.mm(bX[:, j * 64:(j + 1) * 64], Qn[j][:], X[j][:], start=True, stop=False)
                        P.mm(bX[:, j * 64:(j + 1) * 64], I64, X[j][:], start=False, stop=True)
                    for j in R:
                        P.copy(Xn[j][:], bX[:, j * 64:(j + 1) * 64], eng=("scalar" if it % 2 else "vector"))
                    X = Xn
                    Qc = [Qn[j][:] for j in R]
                    if it < 4:
                        Pc = [Pn[j][:] for j in R]
                for j in R:
                    at = B[j][0]
                    P.mm(bRU[:, j * 128:j * 128 + 64], at[:, cs], STb[hs[j]][:], start=True, stop=False)
                    P.mm(bRU[:, j * 128:j * 128 + 64], MakT[j], Vt[j], start=False, stop=True)
                for j in R:
                    P.copy(r0s[i2][j][:], bRU[:, j * 128:j * 128 + 64], eng="scalar")
                for j in R:
                    P.mm(bRU[:, j * 128 + 64:j * 128 + 128], X[j][:], r0s[i2][j][:])
                for j in R:
                    P.copy(us[i2][j][:], bRU[:, j * 128 + 64:j * 128 + 128], eng="scalar")
                for j in R:
                    rt = B[j][1]
                    P.mm(bYS[:, j * 128:j * 128 + 64], STb[hs[j]][:], rt[:, cs], start=True, stop=False)
                    P.mm(bYS[:, j * 128:j * 128 + 64], us[i2][j][:], NrbT[j], start=False, stop=False)
                    P.mm(bYS[:, j * 128:j * 128 + 64], Vt[j], NrkT[j], start=False, stop=True)
                    P.mm(bYS[:, j * 128 + 64:j * 128 + 128], Bt[j], us[i2][j][:], start=True, stop=False)
                    P.mm(bYS[:, j * 128 + 64:j * 128 + 128], Kt[j], Vt[j], start=False, stop=True)
                for j in R:
                    elc = bufs[("elc", par, j)]
                    P.copy(bufs[("y", par, j)][:, cs], bYS[:, j * 128:j * 128 + 64], eng="vector")
                    P.stt(ST[hs[j]][:], ST[hs[j]][:], elc[:, ci:ci + 1], bYS[:, j * 128 + 64:j * 128 + 128], ALU.mult, ALU.add)
                for j in R:
                    P.copy(STb[hs[j]][:], ST[hs[j]][:], eng="gpsimd")
            for j in R:
                rows = slice(hs[j] * 64, (hs[j] + 1) * 64)
                P.dma(yT[rows, s0:s0 + SUP], bufs[("y", par, j)][:], r=[bufs[("y", par, j)]], q="gpsimd", is_out=True)
    P.end_stage()


def stage_rwkv_post(P, yT, scr, prm, outT, T, TT=512, gn_eps=64e-5):
    NC = 8
    lg = P.sb("lg", [128, NC]); P.dma(lg[:], prm["ln_g"], w=[lg])
    lb = P.sb("lb", [128, NC]); P.dma(lb[:], prm["ln_b"], w=[lb])
    bo = P.sb("bo64", [128, 128]); P.dma(bo[:], prm["bones64"], w=[bo])
    def d(n):
        return [P.sb("%s%d" % (n, i), [128, TT]) for i in range(2)]
    y, sq, mu, rs, gv, bn, o = d("y"), d("sq"), d("mu"), d("rs"), d("gv"), d("bn"), d("o")
    ps_m = [P.ps("ps_m%d" % i, [128, TT]) for i in range(2)]
    ps_q = [P.ps("ps_q%d" % i, [128, TT]) for i in range(2)]
    n = 0
    for t0 in range(0, T, TT):
        for c in range(NC):
            i = n % 2; n += 1
            rows = slice(c * 128, (c + 1) * 128)
            P.dma(y[i][:], yT[rows, t0:t0 + TT], w=[y[i]])
            P.dma(gv[i][:], scr["gT"][rows, t0:t0 + TT], w=[gv[i]])
            P.dma(bn[i][:], scr["bonT"][rows, t0:t0 + TT], w=[bn[i]])
            P.act(sq[i][:], y[i][:], AF.Square)
            P.mm(ps_m[i][:], bo[:], y[i][:])
            P.mm(ps_q[i][:], bo[:], sq[i][:])
            P.copy(mu[i][:], ps_m[i][:])
            P.tt(rs[i][:], mu[i][:], mu[i][:], ALU.mult)
            P.tt(rs[i][:], ps_q[i][:], rs[i][:], ALU.subtract)
            P.ts(rs[i][:], rs[i][:], gn_eps, None, ALU.add)
            P.act(rs[i][:], rs[i][:], AF.Sqrt)
            P.recip(rs[i][:], rs[i][:])
            P.tt(y[i][:], y[i][:], mu[i][:], ALU.subtract)
            P.tt(y[i][:], y[i][:], rs[i][:], ALU.mult, eng="gpsimd")
            P.act(o[i][:], y[i][:], AF.Identity, scale=lg[:, c:c + 1], bias=lb[:, c:c + 1])
            P.tt(o[i][:], o[i][:], bn[i][:], ALU.add)
            P.tt(o[i][:], o[i][:], gv[i][:], ALU.mult, eng="gpsimd")
            P.dma(outT[rows, t0:t0 + TT], o[i][:], r=[o[i]], q="gpsimd", is_out=True)
    P.end_stage()


def stage_lru(P, z1T, prm, outT, T, TT=512):
    NC = 8
    XO, GO = 3520, 4544
    cw = P.sb("cw", [128, NC, 4]); P.dma(cw[:], prm["lru_cw"], w=[cw])
    cb = P.sb("cb", [128, NC]); P.dma(cb[:], prm["lru_cb"], w=[cb])
    ba = P.sb("ba", [128, NC]); P.dma(ba[:], prm["lru_ba"], w=[ba])
    bx = P.sb("bx", [128, NC]); P.dma(bx[:], prm["lru_bx"], w=[bx])
    lam = P.sb("lam", [128, NC]); P.dma(lam[:], prm["lru_lam"], w=[lam])
    wa = P.sb("wa", [128, NC, 128]); P.dma(wa[:], prm["lru_wa_bd"], w=[wa])
    wx = P.sb("wx", [128, NC, 128]); P.dma(wx[:], prm["lru_wx_bd"], w=[wx])
    cl = P.sb("cl", [128, NC])
    P.act(cl[:], lam[:], AF.Exp, scale=-1.0)
    P.act(cl[:], cl[:], AF.Ln, bias=1.0)
    P.ts(cl[:], cl[:], -8.0, None, ALU.mult)
    hst = [P.sb("hst%d" % c, [128, 1]) for c in range(NC)]
    for c in range(NC):
        P.memset(hst[c][:], 0.0)
    def d(n, w=TT):
        return [P.sb("%s%d" % (n, i), [128, w]) for i in range(2)]
    xin, xc, gb, r_, i_, a_, u_, h_, o_ = d("xin", TT + 3), d("xc"), d("gb"), d("r_"), d("i_"), d("a_"), d("u_"), d("h_"), d("o_")
    ps_a = [P.ps("ps_a%d" % i, [128, TT]) for i in range(2)]
    ps_x = [P.ps("ps_x%d" % i, [128, TT]) for i in range(2)]
    n = 0
    for t0 in range(0, T, TT):
        for c in range(NC):
            i = n % 2; n += 1
            rows = slice(c * 128, (c + 1) * 128)
            if t0 == 0:
                P.memset(xin[i][:, 0:3], 0.0)
                P.dma(xin[i][:, 3:TT + 3], z1T[XO + c * 128:XO + (c + 1) * 128, 0:TT], w=[xin[i]])
            else:
                P.dma(xin[i][:], z1T[XO + c * 128:XO + (c + 1) * 128, t0 - 3:t0 + TT], w=[xin[i]])
            P.dma(gb[i][:], z1T[GO + c * 128:GO + (c + 1) * 128, t0:t0 + TT], w=[gb[i]])
            P.ts(xc[i][:], xin[i][:, 0:TT], cw[:, c, 0:1], cb[:, c:c + 1], ALU.mult, ALU.add)
            for j in range(1, 4):
                P.stt(xc[i][:], xin[i][:, j:j + TT], cw[:, c, j:j + 1], xc[i][:], ALU.mult, ALU.add)
            P.mm(ps_a[i][:], wa[:, c, :], xc[i][:])
            P.mm(ps_x[i][:], wx[:, c, :], xc[i][:])
            P.act(r_[i][:], ps_a[i][:], AF.Sigmoid, bias=ba[:, c:c + 1])
            P.act(i_[i][:], ps_x[i][:], AF.Sigmoid, bias=bx[:, c:c + 1])
            P.act(a_[i][:], r_[i][:], AF.Exp, scale=cl[:, c:c + 1])
            P.tt(u_[i][:], a_[i][:], a_[i][:], ALU.mult)
            P.ts(u_[i][:], u_[i][:], -1.0, 1.0, ALU.mult, ALU.add)
            P.act(u_[i][:], u_[i][:], AF.Sqrt)
            P.tt(i_[i][:], i_[i][:], xc[i][:], ALU.mult, eng="gpsimd")
            P.tt(u_[i][:], u_[i][:], i_[i][:], ALU.mult)
            P.op("vector", lambda e, h=h_[i], a=a_[i], uu=u_[i], st=hst[c]: e.tensor_tensor_scan(h[:], a[:], uu[:], st[:, 0:1], ALU.mult, ALU.add),
                 r=[a_[i], u_[i], hst[c]], w=[h_[i]])
            P.copy(hst[c][:], h_[i][:, TT - 1:TT])
            P.act(r_[i][:], gb[i][:], AF.Square)
            P.ts(r_[i][:], r_[i][:], 0.044715, 1.0, ALU.mult, ALU.add)
            P.tt(r_[i][:], r_[i][:], gb[i][:], ALU.mult, eng="gpsimd")
            P.act(r_[i][:], r_[i][:], AF.Tanh, scale=0.7978845608028654)
            P.ts(r_[i][:], r_[i][:], 1.0, 0.5, ALU.add, ALU.mult)
            P.tt(gb[i][:], gb[i][:], r_[i][:], ALU.mult, eng="gpsimd")
            P.tt(o_[i][:], h_[i][:], gb[i][:], ALU.mult, eng="gpsimd")
            P.dma(outT[rows, t0:t0 + TT], o_[i][:], r=[o_[i]], q="gpsimd", is_out=True)
    P.end_stage()


def cd_params(inp, j=0):
    f = lambda k: np.asarray(inp[k][j], np.float32)
    mu = f("cd_shift_mu")
    mu_rkv = np.ascontiguousarray(mu[:3072].reshape(24, 128).T)
    bones = np.kron(np.eye(2, dtype=np.float32), np.ones((64, 64), np.float32))
    su = np.triu(np.ones((64, 64), np.float32), 1)
    iu = np.triu(np.ones((64, 64), np.float32), 0)
    sl = np.tril(np.ones((64, 64), np.float32), -1)
    masks = np.ascontiguousarray(np.concatenate([su, iu, su, iu, sl], 1))
    cm = np.ones((128, 512), np.float32); cm[:, ::CH] = 0.0
    def bd(w):
        out = np.zeros((8, 128, 128), np.float32)
        for c in range(8):
            out[c, :64, :64] = w[2 * c]
            out[c, 64:, 64:] = w[2 * c + 1]
        return np.ascontiguousarray(out.transpose(1, 0, 2))
    return {
        "mu_rkv": mu_rkv, "mu_w": mu[3072:3168].reshape(96, 1).copy(), "mu_a": mu[3168:3264].reshape(96, 1).copy(),
        "mu_g": np.ascontiguousarray(mu[3264:3520].reshape(2, 128).T),
        "w0": pl(f("cd_w0")), "a0": pl(f("cd_a0")), "k_k": pl(f("cd_k_k")), "k_a": pl(f("cd_k_a")),
        "r_k": pl(f("cd_r_k").reshape(-1)), "w2": f("cd_w2"), "a2": f("cd_a2"), "g2": f("cd_g2"),
        "bones": bones, "bones64": bones / 64.0 * 1.0, "cmask": cm, "masks": masks,
        "ln_g": pl(f("cd_ln_x_g")), "ln_b": pl(f("cd_ln_x_b")),
        "lru_cw": np.ascontiguousarray(f("cd_lru_conv_w").reshape(4, 8, 128).transpose(2, 1, 0)),
        "lru_cb": pl(f("cd_lru_conv_b")), "lru_ba": pl(f("cd_lru_ba")), "lru_bx": pl(f("cd_lru_bx")),
        "lru_lam": pl(f("cd_lru_lambda")), "lru_wa_bd": bd(f("cd_lru_wa")), "lru_wx_bd": bd(f("cd_lru_wx")),
        "ident": np.eye(128, dtype=np.float32),
    }


CD_PRM_SHAPES = {
    "mu_rkv": [128, 24], "mu_w": [96, 1], "mu_a": [96, 1], "mu_g": [128, 2], "w0": [128, 8], "a0": [128, 8],
    "k_k": [128, 8], "k_a": [128, 8], "r_k": [128, 8], "w2": [96, 1024], "a2": [96, 1024], "g2": [256, 1024],
    "bones": [128, 128], "bones64": [128, 128], "cmask": [128, 512], "masks": [64, 320],
    "ln_g": [128, 8], "ln_b": [128, 8], "lru_cw": [128, 8, 4], "lru_cb": [128, 8], "lru_ba": [128, 8],
    "lru_bx": [128, 8], "lru_lam": [128, 8], "lru_wa_bd": [128, 8, 128], "lru_wx_bd": [128, 8, 128],
    "ident": [128, 128],
}


def emit_mixer_cd(P, z1T, prm, mixT, T):
    scr = {nm: P.dram_tmp("cd_" + nm, [1024, T], BF16) for nm in ("atT", "rtT", "btT", "ktT", "BhT", "KhT", "vT")}
    for nm in ("gT", "bonT"):
        scr[nm] = P.dram_tmp("cd_" + nm, [1024, T])
    scr["eLC"] = P.dram_tmp("cd_eLC", [1024, T // CH])
    yT = P.dram_tmp("cd_yT", [1024, T])
    stage_rwkv_prep(P, z1T, prm, scr, T)
    stage_rwkv_chunk(P, scr, prm["masks"], prm["ident"], yT, T)
    stage_rwkv_post(P, yT, scr, prm, mixT[0:1024, :], T)
    stage_lru(P, z1T, prm, mixT[1024:2048, :], T)


def stage_select(P, srcT, flag_d, dstT, R, TH, TT=2048):
    flag = P.sb("flag_s", [128, 1]); P.dma(flag[:], flag_d, w=[flag])
    a = [P.sb("sa%d" % i, [128, TT]) for i in range(2)]
    b = [P.sb("sb%d" % i, [128, TT]) for i in range(2)]
    TT = min(TT, TH)
    n = 0
    for r0 in range(0, R, 128):
        for t0 in range(0, TH, TT):
            i = n % 2; n += 1
            P.dma(a[i][:, :TT], srcT[r0:r0 + 128, t0:t0 + TT], w=[a[i]])
            P.dma(b[i][:, :TT], srcT[r0:r0 + 128, TH + t0:TH + t0 + TT], w=[b[i]])
            P.tt(b[i][:, :TT], b[i][:, :TT], a[i][:, :TT], ALU.subtract, eng="gpsimd")
            P.stt(a[i][:, :TT], b[i][:, :TT], flag[:, 0:1], a[i][:, :TT], ALU.mult, ALU.add)
            P.dma(dstT[r0:r0 + 128, t0:t0 + TT], a[i][:, :TT], r=[a[i]], q="gpsimd", is_out=True)
    P.end_stage()


CD_INP = 5632


def build_full(S, D=2048, DE=1024):
    P = Prog()
    TH = S // 2
    SK = S + 128
    AB_IN = 3584
    JC = DE // 128
    di = P.dram_in
    xhT = di("xhT", [D, SK]); cT = di("cT", [D, 1])
    ada_w = [di("ada_w%d" % l, [D, 6 * D]) for l in range(2)]
    ada_b = [di("ada_b%d" % l, [128, 6 * D // 128]) for l in range(2)]
    g_mix = [di("g_mix%d" % l, [128, 16]) for l in range(2)]
    g_ffn = [di("g_ffn%d" % l, [128, 16]) for l in range(2)]
    g_fin = di("g_fin", [128, 16])
    w_in0 = di("w_in0", [D, AB_IN]); w_out0 = di("w_out0", [D, D])
    w_in1 = di("w_in1", [D, CD_INP]); w_out1 = di("w_out1", [D, D])
    pos_bc = di("pos_bc", [128, SK], I32)
    invf = di("invf", [128, 1]); sgn = di("sgn", [128, 1])
    maskg = di("maskg", [128, 256]); mask0 = di("mask0", [128, 256])
    sink_bc = di("sink_bc", [128, 16]); ident = di("ident", [128, 128])
    cw = di("cw", [128, 8, 31]); cb = di("cb", [128, 8]); lg = di("lg", [128, 8]); lb = di("lb", [128, 8])
    zflag = di("zflag", [128, 1]); hflag = di("hflag", [128, 1])
    rw = di("rw", [128, 16, 16]); rb = di("rb_bc", [128, 16])
    w1 = [di("w1r%d" % l, [16, JC, 128, 16, 128]) for l in range(2)]
    w3 = [di("w3r%d" % l, [16, JC, 128, 16, 128]) for l in range(2)]
    w2 = [di("w2r%d" % l, [16, 16, 128, JC, 128]) for l in range(2)]
    prm = {k: di("p_" + k, sh) for k, sh in CD_PRM_SHAPES.items()}
    outT = P.dram_out("outT", [D, TH])
    tmp = P.dram_tmp
    modT = [tmp("modT%d" % l, [6 * D, 1]) for l in range(2)]
    hT = tmp("hT", [D, SK]); zT = tmp("zT", [AB_IN, SK]); mixT = tmp("mixT", [D, S])
    x1T = tmp("x1T", [D, S]); hfT = tmp("hfT", [D, S]); gatesT = tmp("gatesT", [16, S]); x2T = tmp("x2T", [D, S])
    z1T = tmp("z1T", [CD_INP, S]); mix1T = tmp("mix1T", [D, S])
    x2hT = tmp("x2hT", [D, TH]); mix1hT = tmp("mix1hT", [D, TH]); x3T = tmp("x3T", [D, TH]); x4T = tmp("x4T", [D, TH])
    mv = [m.rearrange("(c p) o -> p (c o)", p=128) for m in modT]
    for l in range(2):
        stage_lin(P, cT, ada_w[l], modT[l], K=D, M=6 * D, T=1, mode="bias", fp32=True, in_silu=True, bias_d=ada_b[l])
    stage_normfm(P, xhT, modT[0], g_mix[0], 0, 1, hT, SK)
    stage_lin(P, hT, w_in0, zT, K=D, M=AB_IN, T=SK)
    TQ = min(4096, S)
    for sg in range(S // TQ):
        c0 = sg * TQ
        stage_att(P, zT[:, c0:c0 + TQ + 128], pos_bc[:, c0:c0 + TQ + 128], invf, sgn, maskg,
                  mask0 if sg == 0 else maskg, sink_bc, ident, mixT[0:1024, c0:c0 + TQ], TQ)
    stage_conv(P, zT[1536:2560, 98:SK], zT[2560:3584, 98:SK], cw, cb, lg, lb, zflag, mixT[1024:2048, :], S)
    stage_lin(P, mixT, w_out0, x1T, K=D, M=D, T=S, mode="resid", gate_d=mv[0][:, 32:48], rT=xhT[:, 128:SK])
    stage_normfm(P, x1T, modT[0], g_ffn[0], 3, 4, hfT, S)
    stage_route(P, hfT, rw, rb, ident, gatesT, S)
    stage_moe(P, hfT, x1T, gatesT, w1[0], w3[0], w2[0], modT[0], 5, x2T, S, DE=DE)
    stage_normfm(P, x2T, modT[1], g_mix[1], 0, 1, hT[:, 0:S], S)
    stage_lin(P, hT[:, 0:S], w_in1, z1T, K=D, M=CD_INP, T=S)
    emit_mixer_cd(P, z1T, prm, mix1T, S)
    stage_select(P, x2T, hflag, x2hT, D, TH)
    stage_select(P, mix1T, hflag, mix1hT, D, TH)
    stage_lin(P, mix1hT, w_out1, x3T, K=D, M=D, T=TH, mode="resid", gate_d=mv[1][:, 32:48], rT=x2hT)
    stage_normfm(P, x3T, modT[1], g_ffn[1], 3, 4, hfT[:, 0:TH], TH)
    stage_route(P, hfT[:, 0:TH], rw, rb, ident, gatesT[:, 0:TH], TH)
    stage_moe(P, hfT[:, 0:TH], x3T, gatesT[:, 0:TH], w1[1], w3[1], w2[1], modT[1], 5, x4T, TH, DE=DE)
    stage_normfm(P, x4T, None, g_fin, 0, 0, outT, TH)
    return P


def full_inmaps(inp):
    x = np.asarray(inp["x"], np.float32)
    B, S, D = x.shape
    pos = np.asarray(inp["positions"], np.int32)
    invf = (10000.0 ** (-np.arange(32, dtype=np.float32) / 32)).astype(np.float32)
    invf128 = np.tile(invf, 4).reshape(128, 1).astype(np.float32)
    sgn = np.tile(np.concatenate([-np.ones(32), np.ones(32)]), 2).reshape(128, 1).astype(np.float32)
    qi = np.arange(128)[:, None]
    kj = np.arange(256)[None, :]
    dist = 128 + qi - kj
    valid = (dist >= 0) & (dist < 128)
    maskg = np.where(valid, 0.0, -1e30).astype(np.float32)
    mask_first = np.where(valid & (kj >= 128), 0.0, -1e30).astype(np.float32)
    sinks = np.asarray(inp["ab_sinks"][0], np.float32)
    conv_w = np.asarray(inp["ab_conv_w"][0], np.float32).reshape(31, 1024)
    w_in1 = np.asarray(inp["cd_w_in"][0], np.float32)
    w_in1p = np.concatenate([w_in1, np.zeros((D, CD_INP - w_in1.shape[1]), np.float32)], 1)
    common = {
        "g_fin": pl(inp["final_norm"]),
        "w_in0": np.ascontiguousarray(inp["ab_w_in"][0]), "w_out0": np.ascontiguousarray(inp["ab_w_out"][0]),
        "w_in1": np.ascontiguousarray(w_in1p), "w_out1": np.ascontiguousarray(inp["cd_w_out"][0]),
        "invf": invf128, "sgn": sgn, "maskg": maskg, "mask0": mask_first,
        "sink_bc": np.ascontiguousarray(np.broadcast_to(sinks[None, :], (128, 16))).astype(np.float32),
        "ident": np.eye(128, dtype=np.float32),
        "cw": np.ascontiguousarray(conv_w.reshape(31, 8, 128).transpose(2, 1, 0)),
        "cb": pl(inp["ab_conv_b"][0]), "lg": pl(inp["ab_conv_ln_g"][0]), "lb": pl(inp["ab_conv_ln_b"][0]),
        "zflag": np.zeros((128, 1), np.float32),
        "rw": np.ascontiguousarray(np.asarray(inp["router_w"], np.float32).reshape(16, 128, 16).transpose(1, 0, 2)),
        "rb_bc": np.ascontiguousarray(np.broadcast_to(np.asarray(inp["router_bias"], np.float32)[None], (128, 16))),
    }
    for l in range(2):
        common["ada_w%d" % l] = np.ascontiguousarray(inp["ada_w"][l])
        common["ada_b%d" % l] = pl(inp["ada_b"][l])
        common["g_mix%d" % l] = pl(inp["norm_mix"][l])
        common["g_ffn%d" % l] = pl(inp["norm_ffn"][l])
        a, b_, c_ = moe_weights_layout(np.asarray(inp["moe_w1"][l]), np.asarray(inp["moe_w3"][l]), np.asarray(inp["moe_w2"][l]))
        common["w1r%d" % l], common["w3r%d" % l], common["w2r%d" % l] = a, b_, c_
    for k, v in cd_params(inp).items():
        common["p_" + k] = np.ascontiguousarray(v, dtype=np.float32)
    maps = []
    for b in range(B):
        xh = np.concatenate([np.zeros((128, D), np.float32), x[b]], 0)
        ph = np.concatenate([np.zeros((128,), np.int32), pos[b]])
        xhT = np.ascontiguousarray(xh.T)
        cT = np.ascontiguousarray(np.asarray(inp["c"], np.float32)[b].reshape(D, 1))
        pbc = np.ascontiguousarray(np.broadcast_to(ph[None, :], (128, ph.shape[0]))).astype(np.int32)
        for f in range(2):
            m = dict(common)
            m["xhT"] = xhT
            m["cT"] = cT
            m["pos_bc"] = pbc
            m["hflag"] = np.full((128, 1), float(f), np.float32)
            maps.append(m)
    return maps


def kernel(**inputs):
    inp = {k: np.asarray(v) for k, v in inputs.items()}
    B, S, D = inp["x"].shape
    DE = inp["moe_w1"].shape[-1]
    P = build_full(S, D, DE)
    maps = full_inmaps(inp)
    res = run(P, maps)
    TH = S // 2
    out = np.empty((B, S, D), np.float32)
    for b in range(B):
        for f in range(2):
            out[b, f * TH:(f + 1) * TH, :] = res.results[2 * b + f]["outT"].T
    return out


PAIRS = [[0, 1], [2, 3], [4, 5], [6, 7]]


def build_full2(S, D=2048, DE=1024):
    P = Prog()
    TH = S // 2
    TK = TH + 128
    AB_IN = 3584
    JC = DE // 128
    di = P.dram_in
    xhT = di("xhT", [D, TK]); cT = di("cT", [D, 1])
    ada_w = [di("ada_w%d" % l, [D, 6 * D]) for l in range(2)]
    ada_b = [di("ada_b%d" % l, [128, 6 * D // 128]) for l in range(2)]
    g_mix = [di("g_mix%d" % l, [128, 16]) for l in range(2)]
    g_ffn = [di("g_ffn%d" % l, [128, 16]) for l in range(2)]
    g_fin = di("g_fin", [128, 16])
    w_in0 = di("w_in0", [D, AB_IN]); w_out0 = di("w_out0", [D, D])
    w_in1 = di("w_in1", [D, CD_INP]); w_out1 = di("w_out1", [D, D])
    pos_bc = di("pos_bc", [128, TK], I32)
    invf = di("invf", [128, 1]); sgn = di("sgn", [128, 1])
    maskg = di("maskg", [128, 256]); mask0 = di("mask0", [128, 256])
    sink_bc = di("sink_bc", [128, 16]); ident = di("ident", [128, 128])
    cw = di("cw", [128, 8, 31]); cb = di("cb", [128, 8]); lg = di("lg", [128, 8]); lb = di("lb", [128, 8])
    zflag = di("zflag", [128, 1]); hflag = di("hflag", [128, 1])
    rw = di("rw", [128, 16, 16]); rb = di("rb_bc", [128, 16])
    w1 = [di("w1r%d" % l, [16, JC, 128, 16, 128]) for l in range(2)]
    w3 = [di("w3r%d" % l, [16, JC, 128, 16, 128]) for l in range(2)]
    w2 = [di("w2r%d" % l, [16, 16, 128, JC, 128]) for l in range(2)]
    prm = {k: di("p_" + k, sh) for k, sh in CD_PRM_SHAPES.items()}
    outT = P.dram_out("outT", [D, TH])
    tmp = P.dram_tmp
    modT = [tmp("modT%d" % l, [6 * D, 1]) for l in range(2)]
    hT = tmp("hT", [D, TK]); zT = tmp("zT", [AB_IN, TK]); mixT = tmp("mixT", [D, TH])
    x1T = tmp("x1T", [D, TH]); hfT = tmp("hfT", [D, TH]); gatesT = tmp("gatesT", [16, TH]); x2hT = tmp("x2hT", [D, TH])
    x2g = tmp("x2g", [2 * D, TH])
    h1T = tmp("h1T", [D, S]); z1T = tmp("z1T", [CD_INP, S]); mix1T = tmp("mix1T", [D, S])
    mix1hT = tmp("mix1hT", [D, TH]); x3T = tmp("x3T", [D, TH]); x4T = tmp("x4T", [D, TH])
    mv = [m.rearrange("(c p) o -> p (c o)", p=128) for m in modT]
    for l in range(2):
        stage_lin(P, cT, ada_w[l], modT[l], K=D, M=6 * D, T=1, mode="bias", fp32=True, in_silu=True, bias_d=ada_b[l])
    stage_normfm(P, xhT, modT[0], g_mix[0], 0, 1, hT, TK)
    stage_lin(P, hT, w_in0, zT, K=D, M=AB_IN, T=TK)
    stage_att(P, zT, pos_bc, invf, sgn, maskg, mask0, sink_bc, ident, mixT[0:1024, :], TH)
    stage_conv(P, zT[1536:2560, 98:TK], zT[2560:3584, 98:TK], cw, cb, lg, lb, zflag, mixT[1024:2048, :], TH)
    stage_lin(P, mixT, w_out0, x1T, K=D, M=D, T=TH, mode="resid", gate_d=mv[0][:, 32:48], rT=xhT[:, 128:TK])
    stage_normfm(P, x1T, modT[0], g_ffn[0], 3, 4, hfT, TH)
    stage_route(P, hfT, rw, rb, ident, gatesT, TH)
    stage_moe(P, hfT, x1T, gatesT, w1[0], w3[0], w2[0], modT[0], 5, x2hT, TH, DE=DE)
    P.allgather(x2g[:, :], x2hT[:, :], PAIRS)
    P.end_stage()
    for r in range(2):
        stage_normfm(P, x2g[r * D:(r + 1) * D, :], modT[1], g_mix[1], 0, 1, h1T[:, r * TH:(r + 1) * TH], TH)
    stage_lin(P, h1T, w_in1, z1T, K=D, M=CD_INP, T=S)
    emit_mixer_cd(P, z1T, prm, mix1T, S)
    stage_select(P, mix1T, hflag, mix1hT, D, TH)
    stage_lin(P, mix1hT, w_out1, x3T, K=D, M=D, T=TH, mode="resid", gate_d=mv[1][:, 32:48], rT=x2hT)
    stage_normfm(P, x3T, modT[1], g_ffn[1], 3, 4, hfT, TH)
    stage_route(P, hfT, rw, rb, ident, gatesT, TH)
    stage_moe(P, hfT, x3T, gatesT, w1[1], w3[1], w2[1], modT[1], 5, x4T, TH, DE=DE)
    stage_normfm(P, x4T, None, g_fin, 0, 0, outT, TH)
    return P


def full2_inmaps(inp):
    maps = full_inmaps(inp)
    x = np.asarray(inp["x"], np.float32)
    B, S, D = x.shape
    TH = S // 2
    pos = np.asarray(inp["positions"], np.int32)
    maskg = maps[0]["maskg"]
    mask_first = maps[0]["mask0"]
    for b in range(B):
        for f in range(2):
            m = maps[2 * b + f]
            if f == 0:
                xh = np.concatenate([np.zeros((128, D), np.float32), x[b, 0:TH]], 0)
                ph = np.concatenate([np.zeros((128,), np.int32), pos[b, 0:TH]])
            else:
                xh = x[b, TH - 128:S]
                ph = pos[b, TH - 128:S]
            m["xhT"] = np.ascontiguousarray(xh.T)
            m["pos_bc"] = np.ascontiguousarray(np.broadcast_to(ph[None, :], (128, ph.shape[0]))).astype(np.int32)
            m["mask0"] = mask_first if f == 0 else maskg
            m["zflag"] = np.full((128, 1), float(f), np.float32)
    return maps
```

```python
import numpy as np
from contextlib import ExitStack
import concourse.bass as bass
import concourse.mybir as mybir
from concourse.bass_utils import run_bass_kernel_spmd

F32 = mybir.dt.float32
BF16 = mybir.dt.bfloat16
I32 = mybir.dt.int32
AF = mybir.ActivationFunctionType
ALU = mybir.AluOpType
AX = mybir.AxisListType
NCORES = 8


class V:
    def __init__(self, ap, key):
        self.ap = ap
        self.key = key

    def __getitem__(self, idx):
        return V(self.ap[idx], self.key)


def _u(x):
    return x.ap if isinstance(x, V) else x


def _key(x):
    if isinstance(x, str):
        return x
    if isinstance(x, V):
        return x.key
    t = getattr(x, "tensor", x)
    return t.name


class Prog:
    ENG = ("tensor", "vector", "scalar", "gpsimd", "sync")
    NLANES = 8

    def __init__(self, name="k"):
        self.nc = bass.Bass("TRN2", target_bir_lowering=False)
        self.es = ExitStack()
        self.ges = ExitStack()
        self.stage_no = 0
        self._reset()

    def _reset(self):
        self.streams = {e: [] for e in self.ENG}
        self.count = {e: 0 for e in self.ENG}
        self.known = {e: {} for e in self.ENG}
        self.last_w = {}
        self.readers = {}
        self.lane_cnt = [0] * self.NLANES
        self.lane_next = 0
        self.out_tokens = []
        self.ntiles = 0

    def dram_in(self, name, shape, dtype=F32):
        return self.nc.dram_tensor(name, list(shape), dtype, kind="ExternalInput").ap()

    def dram_out(self, name, shape, dtype=F32):
        return self.nc.dram_tensor(name, list(shape), dtype, kind="ExternalOutput").ap()

    def dram_tmp(self, name, shape, dtype=F32):
        return self.nc.dram_tensor(name, list(shape), dtype, kind="Internal").ap()

    def sb(self, name, shape, dtype=F32):
        return self.es.enter_context(self.nc.sbuf_tensor("g%d_%s" % (self.stage_no, name), list(shape), dtype))

    def ps(self, name, shape, dtype=F32):
        return self.es.enter_context(self.nc.psum_tensor("g%d_%s" % (self.stage_no, name), list(shape), dtype))

    def _deps(self, eng, r, w):
        deps = set()
        for k in r:
            k = _key(k)
            if k in self.last_w:
                deps.add(self.last_w[k])
        for k in w:
            k = _key(k)
            if k in self.last_w:
                deps.add(self.last_w[k])
            for t in self.readers.get(k, ()):
                deps.add(t)
        need = {}
        for (s, v) in deps:
            if s == "tensor" and eng == "tensor":
                continue
            if self.known[eng].get(s, 0) < v:
                need[s] = max(need.get(s, 0), v)
        for s, v in need.items():
            self.known[eng][s] = v
        return list(need.items())

    def _commit(self, tok, r, w):
        for k in w:
            k = _key(k)
            self.last_w[k] = tok
            self.readers[k] = []
        for k in r:
            k = _key(k)
            self.readers.setdefault(k, []).append(tok)

    def op(self, eng, fn, r=(), w=()):
        waits = self._deps(eng, r, w)
        self.count[eng] += 1
        tok = (eng, self.count[eng])
        self.known[eng][eng] = max(self.known[eng].get(eng, 0), 0)
        self.streams[eng].append((waits, fn, (eng, 1)))
        self._commit(tok, r, w)
        return tok

    def dma(self, out, in_, r=(), w=(), q="sync", is_out=False, **kw):
        lane = self.lane_next
        self.lane_next = (self.lane_next + 1) % self.NLANES
        s = "lane%d" % lane
        waits = self._deps(q, r, w)
        prev = self.lane_cnt[lane]
        if prev > 0 and self.known[q].get(s, 0) < prev:
            waits.append((s, prev))
            self.known[q][s] = prev
        self.lane_cnt[lane] += 16
        tok = (s, self.lane_cnt[lane])
        self.streams[q].append((waits, (lambda e, o=out, i=in_, kw=kw: e.dma_start(out=o, in_=i, **kw)), (s, 16)))
        self._commit(tok, r, w)
        if is_out:
            self.out_tokens.append(tok)
        return tok

    def allgather(self, out, in_, groups):
        lane = self.lane_next
        self.lane_next = (self.lane_next + 1) % self.NLANES
        s = "lane%d" % lane
        waits = []
        prev = self.lane_cnt[lane]
        if prev > 0 and self.known["gpsimd"].get(s, 0) < prev:
            waits.append((s, prev))
            self.known["gpsimd"][s] = prev
        self.lane_cnt[lane] += 16
        tok = (s, self.lane_cnt[lane])
        self.streams["gpsimd"].append((waits, (lambda e, o=out, i=in_: e.collective_compute(
            "AllGather", ALU.bypass, replica_groups=groups, ins=[i], outs=[o])), (s, 16)))
        self.out_tokens.append(tok)
        return tok

    def mm(self, out, lhsT, rhs, start=True, stop=True, r=None, w=None, **kw):
        r = [lhsT, rhs] if r is None else r
        w = [out] if w is None else w
        return self.op("tensor", lambda e: e.matmul(_u(out), _u(lhsT), _u(rhs), start=start, stop=stop, **kw), r=r, w=w)

    def tr(self, out, in_, ident):
        return self.op("tensor", lambda e: e.transpose(_u(out), _u(in_), _u(ident)), r=[in_, ident], w=[out])

    def act(self, out, in_, func, bias=None, scale=None, accum_out=None, eng="scalar", extra_r=()):
        kw = {}
        r = [in_] + list(extra_r)
        w = [out]
        if bias is not None:
            kw["bias"] = _u(bias)
            if not isinstance(bias, (int, float)):
                r.append(bias)
        if scale is not None:
            kw["scale"] = _u(scale)
            if not isinstance(scale, (int, float)):
                r.append(scale)
        if accum_out is not None:
            kw["accum_out"] = _u(accum_out)
            w.append(accum_out)
        return self.op("scalar", lambda e: e.activation(_u(out), _u(in_), func, **kw), r=r, w=w)

    def tt(self, out, in0, in1, op, eng="vector"):
        return self.op(eng, lambda e: e.tensor_tensor(_u(out), _u(in0), _u(in1), op), r=[in0, in1], w=[out])

    def ts(self, out, in0, s1, s2, op0, op1=None, eng="vector", accum_out=None):
        r = [in0] + [s for s in (s1, s2) if s is not None and not isinstance(s, (int, float))]
        w = [out] + ([accum_out] if accum_out is not None else [])
        kw = {}
        if op1 is not None:
            kw["op1"] = op1
        if accum_out is not None:
            kw["accum_out"] = _u(accum_out)
        return self.op(eng, lambda e: e.tensor_scalar(_u(out), _u(in0), _u(s1), _u(s2), op0, **kw), r=r, w=w)

    def stt(self, out, in0, scalar, in1, op0, op1, eng="vector"):
        r = [in0, in1] + ([] if isinstance(scalar, (int, float)) else [scalar])
        return self.op(eng, lambda e: e.scalar_tensor_tensor(_u(out), _u(in0), _u(scalar), _u(in1), op0, op1), r=r, w=[out])

    def copy(self, out, in_, eng="vector"):
        if eng == "scalar":
            return self.op("scalar", lambda e: e.copy(_u(out), _u(in_)), r=[in_], w=[out])
        return self.op(eng, lambda e: e.tensor_copy(_u(out), _u(in_)), r=[in_], w=[out])

    def memset(self, out, val, eng="vector"):
        return self.op(eng, lambda e: e.memset(out, val), r=[], w=[out])

    def recip(self, out, in_):
        return self.op("vector", lambda e: e.reciprocal(out, in_), r=[in_], w=[out])

    def reduce(self, out, in_, op, axis=AX.X, eng="vector"):
        return self.op(eng, lambda e: e.tensor_reduce(out, in_, axis, op), r=[in_], w=[out])

    def end_stage(self):
        self.build(final=False)
        self.stage_no += 1
        self.es = ExitStack()
        self._reset()

    def build(self, final=True):
        nc = self.nc
        fin = {}
        for (s, v) in self.out_tokens:
            fin[s] = max(fin.get(s, 0), v)
        sem_names = set(self.ENG)
        for i in range(self.NLANES):
            sem_names.add("lane%d" % i)
        sems = {}
        for s in sorted(sem_names):
            sems[s] = nc.alloc_semaphore(name="s%d_%s" % (self.stage_no, s))
        streams = self.streams
        block = self.es.enter_context(nc.Block())

        def emit(eng_name):
            def body(e):
                for waits, fn, (s, n) in streams[eng_name]:
                    for (ws, wv) in waits:
                        e.wait_ge(sems[ws], wv)
                    ins = fn(e)
                    ins.then_inc(sems[s], n)
                if eng_name == "sync":
                    for s, v in fin.items():
                        e.wait_ge(sems[s], v)
            return body

        block.tensor(emit("tensor"))
        block.vector(emit("vector"))
        block.scalar(emit("scalar"))
        block.gpsimd(emit("gpsimd"))
        block.sync(emit("sync"))
        self.es.close()
        nc.clear_and_free_semaphores(list(sems.values()))
        nc.all_engine_barrier()
        return nc


def run(P, in_maps, trace=False):
    P.ges.close()
    nc = P.nc
    res = run_bass_kernel_spmd(nc, in_maps, core_ids=list(range(len(in_maps))), trace=trace)
    return res


def stage_lin(P, xT, w, yT, K, M, T, mode="plain", fp32=False, in_silu=False, TS=2048,
              bias_d=None, gate_d=None, rT=None):
    KC, MC = K // 128, M // 128
    TS = min(TS, T)
    xv = xT.rearrange("(kc p) t -> p kc t", p=128)
    wv = w.rearrange("(kc p) m -> p kc m", p=128)
    DT = F32 if fp32 else BF16
    if mode == "bias":
        bias = P.sb("bias_s", [128, MC])
        P.dma(bias[:], bias_d, w=[bias])
    if mode == "resid":
        gate = P.sb("gate_s", [128, MC])
        P.dma(gate[:], gate_d, w=[gate], allow_slow_non_contiguous=True)
    TT = min(512, TS)
    xb = P.sb("xb", [128, KC, TS], DT)
    xst = [P.sb("xst%d" % i, [128, TS]) for i in range(2)]
    wst = [P.sb("wst%d" % i, [128, KC, 128]) for i in range(2)]
    wb = [P.sb("wb%d" % i, [128, KC, 128], DT) for i in range(2)] if not fp32 else wst
    acc = [P.ps("acc%d" % i, [128, TT]) for i in range(4)]
    ot = [P.sb("ot%d" % i, [128, TT]) for i in range(3)]
    rt = [P.sb("rt%d" % i, [128, TT]) for i in range(2)]
    cnt = 0
    wcnt = 0
    for t0 in range(0, T, TS):
        tl = min(TS, T - t0)
        tt_ = min(TT, tl)
        for kc in range(KC):
            s = xst[kc % 2]
            P.dma(s[:, :tl], xv[:, kc, t0:t0 + tl], w=[s])
            if in_silu:
                P.act(xb[:, kc, :tl], s[:, :tl], AF.Silu)
            else:
                P.copy(xb[:, kc, :tl], s[:, :tl], eng="vector")
        for mc in range(MC):
            wi = wcnt % 2
            wcnt += 1
            P.dma(wst[wi][:], wv[:, :, mc * 128:(mc + 1) * 128], w=[wst[wi]])
            if not fp32:
                P.copy(wb[wi][:], wst[wi][:], eng="gpsimd")
            for tt in range(0, tl, TT):
                tt_ = min(TT, tl - tt)
                a = acc[cnt % 4]
                o = ot[cnt % 3]
                for kc in range(KC):
                    P.mm(a[:, :tt_], wb[wi][:, kc, :], xb[:, kc, tt:tt + tt_], start=(kc == 0), stop=(kc == KC - 1))
                if mode == "plain":
                    P.act(o[:, :tt_], a[:, :tt_], AF.Copy)
                elif mode == "bias":
                    P.act(o[:, :tt_], a[:, :tt_], AF.Identity, bias=bias[:, mc:mc + 1])
                else:
                    rr = rt[cnt % 2]
                    P.dma(rr[:, :tt_], rT[mc * 128:(mc + 1) * 128, t0 + tt:t0 + tt + tt_], w=[rr])
                    P.stt(o[:, :tt_], a[:, :tt_], gate[:, mc:mc + 1], rr[:, :tt_], ALU.mult, ALU.add)
                P.dma(yT[mc * 128:(mc + 1) * 128, t0 + tt:t0 + tt + tt_], o[:, :tt_], r=[o], q="gpsimd", is_out=True)
                cnt += 1
    P.end_stage()


def stage_normfm(P, xT, modT, g_d, shift_row, scale_row, hT, T, D=2048, eps=1e-6, TT=512):
    KC = D // 128
    g = P.sb("g_s", [128, KC]); P.dma(g[:], g_d, w=[g])
    A = P.sb("A_s", [128, KC]); B = P.sb("B_s", [128, KC])
    if modT is None:
        P.copy(A[:], g[:])
        P.memset(B[:], 0.0)
    else:
        mv = modT.rearrange("(c p) o -> p (c o)", p=128)
        P.dma(A[:], mv[:, scale_row * KC:(scale_row + 1) * KC], w=[A], allow_slow_non_contiguous=True)
        P.dma(B[:], mv[:, shift_row * KC:(shift_row + 1) * KC], w=[B], allow_slow_non_contiguous=True)
        P.stt(A[:], A[:], 1.0, g[:], ALU.add, ALU.mult)
    ones = P.sb("ones_s", [128, 128]); P.memset(ones[:], 1.0 / D)
    xs = [P.sb("xs%d" % i, [128, KC, TT]) for i in range(2)]
    sq = [P.sb("sq%d" % i, [128, TT]) for i in range(2)]
    ps = [P.ps("ps%d" % i, [128, TT]) for i in range(2)]
    rstd = [P.sb("rstd%d" % i, [128, TT]) for i in range(2)]
    tm = [P.sb("tm%d" % i, [128, TT]) for i in range(2)]
    ho = [P.sb("ho%d" % i, [128, TT]) for i in range(3)]
    xv = xT.rearrange("(kc p) t -> p kc t", p=128)
    n = 0
    k = 0
    for t0 in range(0, T, TT):
        tl = min(TT, T - t0)
        i = n % 2; n += 1
        x = xs[i]
        for kc in range(KC):
            P.dma(x[:, kc, :tl], xv[:, kc, t0:t0 + tl], w=[x])
        for kc in range(KC):
            q = sq[kc % 2]
            P.act(q[:, :tl], x[:, kc, :tl], AF.Square)
            P.mm(ps[i][:, :tl], ones[:], q[:, :tl], start=(kc == 0), stop=(kc == KC - 1))
        P.ts(rstd[i][:, :tl], ps[i][:, :tl], eps, None, ALU.add)
        P.act(rstd[i][:, :tl], rstd[i][:, :tl], AF.Sqrt)
        P.recip(rstd[i][:, :tl], rstd[i][:, :tl])
        for kc in range(KC):
            t_ = tm[kc % 2]
            o = ho[k % 3]; k += 1
            P.tt(t_[:, :tl], x[:, kc, :tl], rstd[i][:, :tl], ALU.mult)
            P.act(o[:, :tl], t_[:, :tl], AF.Identity, scale=A[:, kc:kc + 1], bias=B[:, kc:kc + 1])
            P.dma(hT[kc * 128:(kc + 1) * 128, t0:t0 + tl], o[:, :tl], r=[o], q="gpsimd", is_out=True)
    P.end_stage()


def stage_att(P, zT, pos_bc, invf_d, sgn_d, maskg_d, mask0_d, sink_d, ident_d, attT, TQ, NH=16, NKV=4):
    import math
    TK = TQ + 128
    NB = TQ // 128
    QC = NH // 2
    QOFF, KOFF, VOFF = 0, NH * 64, NH * 64 + NKV * 64
    invf = P.sb("invf_s", [128, 1]); P.dma(invf[:], invf_d, w=[invf])
    sgn = P.sb("sgn_s", [128, 1]); P.dma(sgn[:], sgn_d, w=[sgn])
    maskg = P.sb("maskg_s", [128, 256]); P.dma(maskg[:], maskg_d, w=[maskg])
    mask0 = P.sb("mask0_s", [128, 256]); P.dma(mask0[:], mask0_d, w=[mask0])
    sink = P.sb("sink_s", [128, NH]); P.dma(sink[:], sink_d, w=[sink])
    identf = P.sb("identf", [128, 128]); P.dma(identf[:], ident_d, w=[identf])
    ident = P.sb("identb", [128, 128], BF16); P.copy(ident[:], identf[:])

    bufA = P.sb("bufA", [128, TK])
    bufB = P.sb("bufB", [128, TK])
    S = P.sb("S", [128, TK])
    C = P.sb("C", [128, TK])
    posi = bufB[:].bitcast(I32)
    P.dma(posi, pos_bc, w=[bufB])
    ang = bufA
    P.copy(ang[:], posi)
    P.ts(ang[:], ang[:], invf[:, 0:1], None, ALU.mult)
    TWO_PI = 2.0 * math.pi
    C1 = 6.28125
    C2 = TWO_PI - C1
    Wt = TK // 4
    for q4 in range(0, TK, Wt):
        wl = min(Wt, TK - q4)
        kf = bufB[:, 0:wl]
        ki = bufB[:, Wt:Wt + wl].bitcast(I32)
        yy = bufB[:, 2 * Wt:2 * Wt + wl]
        mw = bufB[:, 3 * Wt:3 * Wt + wl]
        for which, dst in ((0, S), (1, C)):
            src = ang[:, q4:q4 + wl]
            if which == 1:
                P.ts(yy, src, math.pi / 2.0, None, ALU.add)
                src = yy
            P.ts(kf, src, 1.0 / TWO_PI, None, ALU.mult)
            P.copy(ki, kf)
            P.copy(kf, ki)
            P.stt(yy, kf, -C1, src, ALU.mult, ALU.add)
            P.stt(yy, kf, -C2, yy, ALU.mult, ALU.add)
            P.ts(mw, yy, math.pi, -TWO_PI, ALU.is_gt, ALU.mult)
            P.tt(yy, yy, mw, ALU.add)
            P.ts(mw, yy, -math.pi, TWO_PI, ALU.is_lt, ALU.mult)
            P.tt(yy, yy, mw, ALU.add)
            P.ts(yy, yy, math.pi, -math.pi, ALU.min, ALU.max)
            if which == 0:
                P.act(dst[:, q4:q4 + wl], yy, AF.Sin, scale=sgn[:, 0:1])
            else:
                P.act(dst[:, q4:q4 + wl], yy, AF.Sin)

    kr = P.sb("kr", [128, NKV, TK], BF16)
    vb = P.sb("vb", [128, NB + 1, NKV * 64], BF16)
    for g in range(NKV):
        r0 = KOFF + g * 64
        for dup in range(2):
            P.dma(bufA[dup * 64:(dup + 1) * 64, :], zT[r0:r0 + 64, :], w=[bufA])
            for half in range(2):
                P.dma(bufB[dup * 64 + half * 32:dup * 64 + half * 32 + 32, :],
                      zT[r0 + (1 - half) * 32:r0 + (1 - half) * 32 + 32, :], w=[bufB])
        P.tt(bufA[:], bufA[:], C[:], ALU.mult)
        P.tt(bufB[:], bufB[:], S[:], ALU.mult, eng="gpsimd")
        P.tt(kr[:, g, :], bufA[:], bufB[:], ALU.add)
    ps_s = [P.ps("ps_s%d" % i, [128, 256]) for i in range(2)]
    VC = NKV * 64 // 128
    vt = [P.sb("vt%d" % i, [128, VC, 128]) for i in range(2)]
    vv = zT[VOFF:VOFF + NKV * 64, :].rearrange("(c p) t -> p c t", p=128)
    for blk in range(NB + 1):
        v_ = vt[blk % 2]
        P.dma(v_[:], vv[:, :, blk * 128:(blk + 1) * 128], w=[v_])
        for c in range(VC):
            P.tr(ps_s[blk % 2][:, c * 128:(c + 1) * 128], v_[:, c, :], identf[:])
        P.copy(vb[:, blk, :], ps_s[blk % 2][:, 0:VC * 128], eng="scalar")

    GS = min(512, TQ)
    qa = [P.sb("qa%d" % i, [128, GS]) for i in range(2)]
    qb = [P.sb("qb%d" % i, [128, GS]) for i in range(2)]
    qr = [P.sb("qr%d" % i, [128, QC, GS], BF16) for i in range(2)]
    ps_t = [P.ps("ps_t%d" % i, [128, 2, 128], BF16) for i in range(2)]
    ps_o = [P.ps("ps_o%d" % i, [128, 128]) for i in range(2)]
    sm = [P.sb("sm%d" % i, [128, 256]) for i in range(2)]
    pe_ = [P.sb("pe%d" % i, [128, 256]) for i in range(2)]
    pb = [P.sb("pb%d" % i, [128, 256], BF16) for i in range(2)]
    pT = [P.sb("pT%d" % i, [128, 2, 128], BF16) for i in range(2)]
    st = [[P.sb("st%s%d" % (nm, i), [128, 1]) for i in range(2)] for nm in ("mx", "ng", "rs", "es")]
    ao = [P.sb("ao%d" % i, [128, QC, 128]) for i in range(2)]
    attv = attT.rearrange("(c p) t -> p c t", p=128)
    u = 0
    n = 0
    for g0 in range(0, TQ, GS):
        qrg = qr[(g0 // GS) % 2]
        for c in range(QC):
            a, b = qa[n % 2], qb[n % 2]; n += 1
            r0 = QOFF + c * 128
            P.dma(a[:], zT[r0:r0 + 128, 128 + g0:128 + g0 + GS], w=[a])
            for hh in range(2):
                for half in range(2):
                    P.dma(b[hh * 64 + half * 32:hh * 64 + half * 32 + 32, :],
                          zT[r0 + hh * 64 + (1 - half) * 32:r0 + hh * 64 + (1 - half) * 32 + 32, 128 + g0:128 + g0 + GS], w=[b])
            P.tt(a[:], a[:], C[:, 128 + g0:128 + g0 + GS], ALU.mult)
            P.tt(b[:], b[:], S[:, 128 + g0:128 + g0 + GS], ALU.mult, eng="gpsimd")
            P.tt(qrg[:, c, :], a[:], b[:], ALU.add)
        for jl in range(GS // 128):
            j = g0 // 128 + jl
            msk = mask0 if j == 0 else maskg
            aoj = ao[j % 2]
            for c in range(QC):
                po = ps_o[c % 2]
                for hh in range(2):
                    h = 2 * c + hh
                    g = h // (NH // NKV)
                    i2 = u % 2; u += 1
                    pb0 = hh * 64
                    mx, ng, rs, es = st[0][i2], st[1][i2], st[2][i2], st[3][i2]
                    P.mm(ps_s[i2][:], qrg[pb0:pb0 + 64, c, jl * 128:(jl + 1) * 128], kr[pb0:pb0 + 64, g, j * 128:j * 128 + 256])
                    P.stt(sm[i2][:], ps_s[i2][:], 0.125, msk[:], ALU.mult, ALU.add)
                    P.reduce(mx[:], sm[i2][:], ALU.max)
                    P.ts(ng[:], mx[:], sink[:, h:h + 1], -1.0, ALU.max, ALU.mult)
                    P.act(pe_[i2][:], sm[i2][:], AF.Exp, bias=ng[:, 0:1], accum_out=rs[:])
                    P.act(es[:], sink[:, h:h + 1], AF.Exp, bias=ng[:, 0:1])
                    P.tt(rs[:], rs[:], es[:], ALU.add)
                    P.recip(rs[:], rs[:])
                    P.ts(pb[i2][:], pe_[i2][:], rs[:, 0:1], None, ALU.mult)
                    for kc in range(2):
                        P.tr(ps_t[i2][:, kc, :], pb[i2][:, kc * 128:(kc + 1) * 128], ident[:])
                    P.copy(pT[i2][:], ps_t[i2][:], eng="scalar")
                    for kc in range(2):
                        P.mm(po[pb0:pb0 + 64, :], vb[:, j + kc, g * 64:(g + 1) * 64], pT[i2][:, kc, :],
                             start=(kc == 0), stop=(kc == 1))
                P.copy(aoj[:, c, :], po[:], eng="vector")
            P.dma(attv[:, :, j * 128:(j + 1) * 128], aoj[:], r=[aoj], q="gpsimd", is_out=True)
    P.end_stage()


def stage_conv(P, valT, gateT, cw_d, cb_d, lg_d, lb_d, flag_d, outT, TQ, CH=1024, W=31, ln_eps=1e-5, TT=512, ident_d=None):
    CC = CH // 128
    HL = W - 1
    TT = min(TT, TQ)
    cw = P.sb("cw_s", [128, CC, W]); P.dma(cw[:], cw_d, w=[cw])
    cb = P.sb("cb_s", [128, CC]); P.dma(cb[:], cb_d, w=[cb])
    lg = P.sb("lg_s", [128, CC]); P.dma(lg[:], lg_d, w=[lg])
    lb = P.sb("lb_s", [128, CC]); P.dma(lb[:], lb_d, w=[lb])
    flag = P.sb("flag_s", [128, 1]); P.dma(flag[:], flag_d, w=[flag])
    ones = P.sb("ones_s", [128, 128]); P.memset(ones[:], 1.0 / CH)
    eye = P.sb("eye_s", [128, 128]); P.dma(eye[:], ident_d, w=[eye])
    dg = [P.sb("dg%d" % c, [128, W, 128], BF16) for c in range(CC)]
    for c in range(CC):
        for j in range(W):
            P.ts(dg[c][:, j, :], eye[:], cw[:, c, j:j + 1], None, ALU.mult, eng=("vector" if (c * W + j) % 2 else "gpsimd"))
    va = [P.sb("va%d" % i, [128, TT + HL]) for i in range(2)]
    ga = [P.sb("ga%d" % i, [128, TT + HL]) for i in range(2)]
    vb = [P.sb("vb%d" % i, [128, TT + HL], BF16) for i in range(2)]
    yc = [P.sb("yc%d" % c, [128, TT]) for c in range(CC)]
    sq = [P.sb("sq%d" % i, [128, TT]) for i in range(2)]
    ps_c = [P.ps("ps_c%d" % i, [128, TT]) for i in range(2)]
    ps_m = P.ps("ps_m", [128, TT])
    ps_q = P.ps("ps_q", [128, TT])
    mean = P.sb("mean", [128, TT])
    rstd = P.sb("rstd", [128, TT])
    ot = [P.sb("cot%d" % i, [128, TT]) for i in range(2)]
    n = 0
    for t0 in range(0, TQ, TT):
        for c in range(CC):
            i2 = n % 2; n += 1
            v, g = va[i2], ga[i2]
            P.dma(v[:], valT[c * 128:(c + 1) * 128, t0:t0 + TT + HL], w=[v])
            P.dma(g[:], gateT[c * 128:(c + 1) * 128, t0:t0 + TT + HL], w=[g])
            P.act(g[:], g[:], AF.Sigmoid)
            if t0 == 0:
                P.tt(v[:], v[:], g[:], ALU.mult)
                P.ts(v[:, 0:HL], v[:, 0:HL], flag[:, 0:1], None, ALU.mult)
                P.copy(vb[i2][:], v[:], eng="gpsimd")
            else:
                P.tt(vb[i2][:], v[:], g[:], ALU.mult)
            for j in range(W):
                P.mm(ps_c[i2][:], dg[c][:, j, :], vb[i2][:, j:j + TT], start=(j == 0), stop=(j == W - 1))
            P.act(yc[c][:], ps_c[i2][:], AF.Identity, bias=cb[:, c:c + 1])
            s_ = sq[c % 2]
            P.act(s_[:], yc[c][:], AF.Square)
            P.mm(ps_m[:], ones[:], yc[c][:], start=(c == 0), stop=(c == CC - 1))
            P.mm(ps_q[:], ones[:], s_[:], start=(c == 0), stop=(c == CC - 1))
        P.copy(mean[:], ps_m[:])
        P.tt(rstd[:], mean[:], mean[:], ALU.mult)
        P.tt(rstd[:], ps_q[:], rstd[:], ALU.subtract)
        P.ts(rstd[:], rstd[:], ln_eps, None, ALU.add)
        P.act(rstd[:], rstd[:], AF.Sqrt)
        P.recip(rstd[:], rstd[:])
        for c in range(CC):
            o = ot[c % 2]
            P.tt(yc[c][:], yc[c][:], mean[:], ALU.subtract)
            P.tt(yc[c][:], yc[c][:], rstd[:], ALU.mult, eng="gpsimd")
            P.act(o[:], yc[c][:], AF.Silu, scale=lg[:, c:c + 1], bias=lb[:, c:c + 1])
            P.dma(outT[c * 128:(c + 1) * 128, t0:t0 + TT], o[:], r=[o], q="gpsimd", is_out=True)
    P.end_stage()


def stage_route(P, hT, rw_d, rb_d, ident_d, gatesT, T, D=2048, E=16, G=4, TS=1024):
    KC = D // 128
    EG = E // G
    TS = min(TS, T)
    hv = hT.rearrange("(kc p) t -> p kc t", p=128)
    rw = P.sb("rw_s", [128, KC, E]); P.dma(rw[:], rw_d, w=[rw])
    rb = P.sb("rb_s", [128, E]); P.dma(rb[:], rb_d, w=[rb])
    identf = P.sb("identf", [128, 128]); P.dma(identf[:], ident_d, w=[identf])
    hs = [P.sb("hs%d" % i, [128, KC, TS]) for i in range(1)]
    ps = [P.ps("psl%d" % i, [128, E]) for i in range(2)]
    psT = [P.ps("psT%d" % i, [E, 128]) for i in range(2)]
    def t(name, shape):
        return [P.sb("%s%d" % (name, i), shape) for i in range(2)]
    lg, pr, sel, sel2, msk = t("lg", [128, E]), t("pr", [128, E]), t("sel", [128, E]), t("sel2", [128, E]), t("msk", [128, E])
    mx, sm, m1, m2, gs, gm, gsel = t("mx", [128, 1]), t("sm", [128, 1]), t("m1", [128, G]), t("m2", [128, G]), t("gs", [128, G]), t("gm", [128, 1]), t("gsel", [128, G])
    og = t("og", [128, E])
    ogT = t("ogT", [E, 128])
    n = 0
    for t0 in range(0, T, TS):
        h = hs[0]
        for kc in range(KC):
            P.dma(h[:, kc, :], hv[:, kc, t0:t0 + TS], w=[h])
        for tt in range(0, TS, 128):
            i = n % 2; n += 1
            for kc in range(KC):
                P.mm(ps[i][:], h[:, kc, tt:tt + 128], rw[:, kc, :], start=(kc == 0), stop=(kc == KC - 1))
            P.copy(lg[i][:], ps[i][:])
            P.reduce(mx[i][:], lg[i][:], ALU.max)
            P.ts(mx[i][:], mx[i][:], -1.0, None, ALU.mult)
            P.act(pr[i][:], lg[i][:], AF.Exp, bias=mx[i][:, 0:1], accum_out=sm[i][:])
            P.recip(sm[i][:], sm[i][:])
            P.ts(pr[i][:], pr[i][:], sm[i][:, 0:1], None, ALU.mult)
            P.tt(sel[i][:], pr[i][:], rb[:], ALU.add)
            s3 = sel[i][:].rearrange("p (g e) -> p g e", g=G)
            s23 = sel2[i][:].rearrange("p (g e) -> p g e", g=G)
            k3 = msk[i][:].rearrange("p (g e) -> p g e", g=G)
            P.reduce(m1[i][:], s3, ALU.max)
            m1b = m1[i][:].unsqueeze(2).broadcast_to([128, G, EG])
            P.tt(s23, s3, m1b, ALU.is_equal)
            P.stt(sel2[i][:], sel2[i][:], -1e9, sel[i][:], ALU.mult, ALU.add)
            P.reduce(m2[i][:], s23, ALU.max)
            P.tt(gs[i][:], m1[i][:], m2[i][:], ALU.add)
            P.reduce(gm[i][:], gs[i][:], ALU.max)
            P.ts(gsel[i][:], gs[i][:], gm[i][:, 0:1], None, ALU.is_equal)
            m2b = m2[i][:].unsqueeze(2).broadcast_to([128, G, EG])
            P.tt(k3, s3, m2b, ALU.is_ge)
            gselb = gsel[i][:].unsqueeze(2).broadcast_to([128, G, EG])
            P.tt(k3, k3, gselb, ALU.mult)
            P.tt(og[i][:], pr[i][:], msk[i][:], ALU.mult)
            P.reduce(sm[i][:], og[i][:], ALU.add)
            P.recip(sm[i][:], sm[i][:])
            P.ts(og[i][:], og[i][:], sm[i][:, 0:1], None, ALU.mult)
            P.tr(psT[i][:], og[i][:], identf[:])
            P.copy(ogT[i][:], psT[i][:], eng="scalar")
            P.dma(gatesT[:, t0 + tt:t0 + tt + 128], ogT[i][:], r=[ogT[i]], q="gpsimd", is_out=True)
    P.end_stage()


def _swap_half(a):
    sh = a.shape
    a4 = a.reshape(sh[:-1] + (sh[-1] // 64, 2, 32))
    return np.ascontiguousarray(a4[..., ::-1, :]).reshape(sh)


def att_inmaps(q, k, v, pos, sinks, TQ):
    import math
    B, S, _ = q.shape
    per = S // TQ
    half = 32
    invf = (10000.0 ** (-np.arange(half, dtype=np.float32) / half)).astype(np.float32)
    invf128 = np.tile(invf, 4).reshape(128, 1).astype(np.float32)
    sgn = np.tile(np.concatenate([-np.ones(32), np.ones(32)]), 2).reshape(128, 1).astype(np.float32)
    qi = np.arange(128)[:, None]
    kj = np.arange(256)[None, :]
    dist = 128 + qi - kj
    valid = (dist >= 0) & (dist < 128)
    maskg = np.where(valid, 0.0, -1e30).astype(np.float32)
    mask_first = np.where(valid & (kj >= 128), 0.0, -1e30).astype(np.float32)
    ident = np.eye(128, dtype=np.float32)
    sink_bc = np.ascontiguousarray(np.broadcast_to(sinks[None, :], (128, sinks.shape[0]))).astype(np.float32)
    qs = _swap_half(q)
    ks = _swap_half(k)
    maps = []
    for b in range(B):
        for c in range(per):
            t0 = c * TQ
            def halo(a):
                if c == 0:
                    return np.concatenate([np.zeros((128,) + a.shape[2:], a.dtype), a[b, 0:TQ]], 0)
                return a[b, t0 - 128:t0 + TQ]
            kh, ksh, vh = halo(k), halo(ks), halo(v)
            if c == 0:
                ph = np.concatenate([np.zeros((128,), np.int32), pos[b, 0:TQ]])
            else:
                ph = pos[b, t0 - 128:t0 + TQ]
            def dup(a):
                aT = a.T.reshape(4, 64, -1)
                return np.ascontiguousarray(np.concatenate([aT, aT], 1).reshape(512, -1))
            maps.append({
                "qT": np.ascontiguousarray(q[b, t0:t0 + TQ].T), "qsT": np.ascontiguousarray(qs[b, t0:t0 + TQ].T),
                "kdT": dup(kh), "ksdT": dup(ksh), "vtm": np.ascontiguousarray(vh),
                "pos_bc": np.ascontiguousarray(np.broadcast_to(ph[None, :], (128, ph.shape[0]))).astype(np.int32),
                "invf": invf128, "sgn": sgn, "maskg": maskg, "mask0": mask_first if c == 0 else maskg,
                "sink_bc": sink_bc, "ident": ident})
    return maps


def pl(v):
    return np.ascontiguousarray(np.asarray(v, np.float32).reshape(-1, 128).T)


def conv_inmaps(u, conv_w, conv_b, ln_g, ln_b, TQ):
    B, S, _ = u.shape
    per = S // TQ
    CH = 1024
    W = conv_w.shape[0]
    cw = np.ascontiguousarray(conv_w.reshape(W, CH // 128, 128).transpose(2, 1, 0)).astype(np.float32)
    maps = []
    for b in range(B):
        for c in range(per):
            t0 = c * TQ
            if c == 0:
                seg = np.concatenate([np.zeros((W - 1, 2 * CH), np.float32), u[b, 0:TQ]], 0)
            else:
                seg = u[b, t0 - (W - 1):t0 + TQ]
            maps.append({"valT": np.ascontiguousarray(seg[:, :CH].T), "gateT": np.ascontiguousarray(seg[:, CH:].T),
                         "cw": cw, "cb": pl(conv_b), "lg": pl(ln_g), "lb": pl(ln_b)})
    return maps


def stage_moe(P, hT, xT, gatesT, w1, w3, w2, modT, gate_row, oT, T, D=2048, DE=1024, E=16, TS=512):
    KC, JC = D // 128, DE // 128
    TS = min(TS, T)
    hv = hT.rearrange("(kc p) t -> p kc t", p=128)
    mv = modT.rearrange("(c p) o -> p (c o)", p=128)
    gf = P.sb("gf_s", [128, KC]); P.dma(gf[:], mv[:, gate_row * KC:(gate_row + 1) * KC], w=[gf], allow_slow_non_contiguous=True)
    hb = P.sb("hb", [128, KC, TS], BF16)
    hst = [P.sb("hst%d" % i, [128, TS]) for i in range(2)]
    yacc = [P.sb("yacc%d" % f, [128, TS]) for f in range(KC)]
    hid = [P.sb("hid%d" % j, [128, TS], BF16) for j in range(JC)]
    ge = [P.sb("ge%d" % i, [128, TS]) for i in range(2)]
    wst = [P.sb("wst%d" % i, [128, KC, 128]) for i in range(4)]
    wbf = [P.sb("wbf%d" % i, [128, KC, 128], BF16) for i in range(4)]
    w2st = [P.sb("w2st%d" % i, [128, JC, 128]) for i in range(2)]
    w2bf = [P.sb("w2bf%d" % i, [128, JC, 128], BF16) for i in range(2)]
    sa = [P.sb("sa%d" % i, [128, TS]) for i in range(2)]
    ps_a = [P.ps("ps_a%d" % i, [128, TS]) for i in range(2)]
    ps_b = [P.ps("ps_b%d" % i, [128, TS]) for i in range(2)]
    ps_y = [P.ps("ps_y%d" % i, [128, TS]) for i in range(2)]
    xt = [P.sb("xt%d" % i, [128, TS]) for i in range(2)]
    n1 = n2 = 0
    for t0 in range(0, T, TS):
        for kc in range(KC):
            s = hst[kc % 2]
            P.dma(s[:], hv[:, kc, t0:t0 + TS], w=[s])
            P.copy(hb[:, kc, :], s[:], eng="vector")
        for e in range(E):
            g = ge[e % 2]
            P.dma(g[:], gatesT[e:e + 1, t0:t0 + TS].partition_broadcast(128), w=[g])
            for jc in range(JC):
                i = n1 % 2; n1 += 1
                P.dma(wst[2 * i][:], w1[e, jc], w=[wst[2 * i]])
                P.dma(wst[2 * i + 1][:], w3[e, jc], w=[wst[2 * i + 1]])
                P.copy(wbf[2 * i][:], wst[2 * i][:], eng="gpsimd")
                P.copy(wbf[2 * i + 1][:], wst[2 * i + 1][:], eng="scalar")
                for kc in range(KC):
                    P.mm(ps_a[i][:], wbf[2 * i][:, kc, :], hb[:, kc, :], start=(kc == 0), stop=(kc == KC - 1))
                for kc in range(KC):
                    P.mm(ps_b[i][:], wbf[2 * i + 1][:, kc, :], hb[:, kc, :], start=(kc == 0), stop=(kc == KC - 1))
                P.act(sa[i][:], ps_a[i][:], AF.Silu)
                P.tt(sa[i][:], sa[i][:], ps_b[i][:], ALU.mult)
                P.tt(hid[jc][:], sa[i][:], g[:], ALU.mult)
            for fc in range(KC):
                i = n2 % 2; n2 += 1
                P.dma(w2st[i][:], w2[e, fc], w=[w2st[i]])
                P.copy(w2bf[i][:], w2st[i][:], eng="gpsimd")
                for jc in range(JC):
                    P.mm(ps_y[i][:], w2bf[i][:, jc, :], hid[jc][:], start=(jc == 0), stop=(jc == JC - 1))
                if e == 0:
                    P.copy(yacc[fc][:], ps_y[i][:])
                else:
                    P.tt(yacc[fc][:], yacc[fc][:], ps_y[i][:], ALU.add)
        for fc in range(KC):
            x_ = xt[fc % 2]
            P.dma(x_[:], xT[fc * 128:(fc + 1) * 128, t0:t0 + TS], w=[x_])
            P.stt(x_[:], yacc[fc][:], gf[:, fc:fc + 1], x_[:], ALU.mult, ALU.add)
            P.dma(oT[fc * 128:(fc + 1) * 128, t0:t0 + TS], x_[:], r=[x_], q="gpsimd", is_out=True)
    P.end_stage()


def moe_weights_layout(w1, w3, w2):
    E, D, DE = w1.shape
    KC, JC = D // 128, DE // 128
    f = lambda w: np.ascontiguousarray(w.reshape(E, KC, 128, JC, 128).transpose(0, 3, 2, 1, 4))
    w2r = np.ascontiguousarray(w2.reshape(E, JC, 128, KC, 128).transpose(0, 3, 2, 1, 4))
    return f(w1), f(w3), w2r


def build_block0(TQ, D=2048):
    P = Prog()
    TK = TQ + 128
    AB_IN = 3584
    xhT = P.dram_in("xhT", [D, TK])
    cT = P.dram_in("cT", [D, 1])
    ada_w = P.dram_in("ada_w", [D, 6 * D])
    ada_b = P.dram_in("ada_b", [128, 6 * D // 128])
    g_mix = P.dram_in("g_mix", [128, D // 128])
    g_ffn = P.dram_in("g_ffn", [128, D // 128])
    w_in = P.dram_in("w_in", [D, AB_IN])
    w_out = P.dram_in("w_out", [D, D])
    pos_bc = P.dram_in("pos_bc", [128, TK], I32)
    invf = P.dram_in("invf", [128, 1])
    sgn = P.dram_in("sgn", [128, 1])
    maskg = P.dram_in("maskg", [128, 256])
    mask0 = P.dram_in("mask0", [128, 256])
    sink_bc = P.dram_in("sink_bc", [128, 16])
    ident = P.dram_in("ident", [128, 128])
    cw = P.dram_in("cw", [128, 8, 31])
    cb = P.dram_in("cb", [128, 8])
    lg = P.dram_in("lg", [128, 8])
    lb = P.dram_in("lb", [128, 8])
    flag = P.dram_in("flag", [128, 1])
    rw = P.dram_in("rw", [128, 16, 16])
    rb = P.dram_in("rb_bc", [128, 16])
    w1 = P.dram_in("w1r", [16, 8, 128, 16, 128])
    w3 = P.dram_in("w3r", [16, 8, 128, 16, 128])
    w2 = P.dram_in("w2r", [16, 16, 128, 8, 128])
    x2T = P.dram_out("x2T", [D, TQ])
    modT = P.dram_tmp("modT", [6 * D, 1])
    hT = P.dram_tmp("hT", [D, TK])
    zT = P.dram_tmp("zT", [AB_IN, TK])
    mixT = P.dram_tmp("mixT", [D, TQ])
    x1T = P.dram_tmp("x1T", [D, TQ])
    hfT = P.dram_tmp("hfT", [D, TQ])
    gatesT = P.dram_tmp("gatesT", [16, TQ])
    mv = modT.rearrange("(c p) o -> p (c o)", p=128)
    stage_lin(P, cT, ada_w, modT, K=D, M=6 * D, T=1, mode="bias", fp32=True, in_silu=True, bias_d=ada_b)
    stage_normfm(P, xhT, modT, g_mix, 0, 1, hT, TK)
    stage_lin(P, hT, w_in, zT, K=D, M=AB_IN, T=TK)
    stage_att(P, zT, pos_bc, invf, sgn, maskg, mask0, sink_bc, ident, mixT[0:1024, :], TQ)
    stage_conv(P, zT[1536:2560, 98:TK], zT[2560:3584, 98:TK], cw, cb, lg, lb, flag, mixT[1024:2048, :], TQ, ident_d=ident)
    stage_lin(P, mixT, w_out, x1T, K=D, M=D, T=TQ, mode="resid", gate_d=mv[:, 32:48], rT=xhT[:, 128:TK])
    stage_normfm(P, x1T, modT, g_ffn, 3, 4, hfT, TQ)
    stage_route(P, hfT, rw, rb, ident, gatesT, TQ)
    stage_moe(P, hfT, x1T, gatesT, w1, w3, w2, modT, 5, x2T, TQ)
    return P


def block0_inmaps(inp, TQ, layer=0):
    x = np.asarray(inp["x"], np.float32)
    B, S, D = x.shape
    per = S // TQ
    pos = np.asarray(inp["positions"], np.int32)
    j = layer // 2
    invf = (10000.0 ** (-np.arange(32, dtype=np.float32) / 32)).astype(np.float32)
    invf128 = np.tile(invf, 4).reshape(128, 1).astype(np.float32)
    sgn = np.tile(np.concatenate([-np.ones(32), np.ones(32)]), 2).reshape(128, 1).astype(np.float32)
    qi = np.arange(128)[:, None]
    kj = np.arange(256)[None, :]
    dist = 128 + qi - kj
    valid = (dist >= 0) & (dist < 128)
    maskg = np.where(valid, 0.0, -1e30).astype(np.float32)
    mask_first = np.where(valid & (kj >= 128), 0.0, -1e30).astype(np.float32)
    ident = np.eye(128, dtype=np.float32)
    sinks = np.asarray(inp["ab_sinks"][j], np.float32)
    sink_bc = np.ascontiguousarray(np.broadcast_to(sinks[None, :], (128, 16))).astype(np.float32)
    conv_w = np.asarray(inp["ab_conv_w"][j], np.float32).reshape(31, 1024)
    cw = np.ascontiguousarray(conv_w.reshape(31, 8, 128).transpose(2, 1, 0))
    w1r, w3r, w2r = moe_weights_layout(np.asarray(inp["moe_w1"][layer]), np.asarray(inp["moe_w3"][layer]), np.asarray(inp["moe_w2"][layer]))
    rwl = np.ascontiguousarray(np.asarray(inp["router_w"], np.float32).reshape(16, 128, 16).transpose(1, 0, 2))
    rbb = np.ascontiguousarray(np.broadcast_to(np.asarray(inp["router_bias"], np.float32)[None], (128, 16)))
    common = {
        "ada_w": np.ascontiguousarray(inp["ada_w"][layer]), "ada_b": pl(inp["ada_b"][layer]),
        "g_mix": pl(inp["norm_mix"][layer]), "g_ffn": pl(inp["norm_ffn"][layer]),
        "w_in": np.ascontiguousarray(inp["ab_w_in"][j]), "w_out": np.ascontiguousarray(inp["ab_w_out"][j]),
        "invf": invf128, "sgn": sgn, "maskg": maskg, "sink_bc": sink_bc, "ident": ident,
        "cw": cw, "cb": pl(inp["ab_conv_b"][j]), "lg": pl(inp["ab_conv_ln_g"][j]), "lb": pl(inp["ab_conv_ln_b"][j]),
        "rw": rwl, "rb_bc": rbb, "w1r": w1r, "w3r": w3r, "w2r": w2r,
    }
    maps = []
    for b in range(B):
        for c in range(per):
            t0 = c * TQ
            if c == 0:
                xh = np.concatenate([np.zeros((128, D), np.float32), x[b, 0:TQ]], 0)
                ph = np.concatenate([np.zeros((128,), np.int32), pos[b, 0:TQ]])
            else:
                xh = x[b, t0 - 128:t0 + TQ]
                ph = pos[b, t0 - 128:t0 + TQ]
            m = dict(common)
            m["xhT"] = np.ascontiguousarray(xh.T)
            m["cT"] = np.ascontiguousarray(np.asarray(inp["c"], np.float32)[b].reshape(D, 1))
            m["pos_bc"] = np.ascontiguousarray(np.broadcast_to(ph[None, :], (128, ph.shape[0]))).astype(np.int32)
            m["mask0"] = mask_first if c == 0 else maskg
            m["flag"] = np.full((128, 1), 0.0 if c == 0 else 1.0, np.float32)
            maps.append(m)
    return maps


CH = 64


def _lerp_load(P, dst, tmp, src_rows, t0, tl, mu_col, np_):
    if t0 == 0:
        P.memset(tmp[:np_, 0:1], 0.0)
        P.dma(tmp[:np_, 1:tl + 1], src_rows[:, 0:tl], w=[tmp])
    else:
        P.dma(tmp[:np_, 0:tl + 1], src_rows[:, t0 - 1:t0 + tl], w=[tmp])
    P.tt(dst[:np_, :tl], tmp[:np_, 0:tl], tmp[:np_, 1:tl + 1], ALU.subtract)
    P.stt(dst[:np_, :tl], dst[:np_, :tl], mu_col, tmp[:np_, 1:tl + 1], ALU.mult, ALU.add)


def stage_rwkv_prep(P, z1T, prm, scr, T, TT=512):
    import math
    NC = 8
    mu_rkv = P.sb("mu_rkv", [128, 24]); P.dma(mu_rkv[:], prm["mu_rkv"], w=[mu_rkv])
    mu_w = P.sb("mu_w", [96, 1]); P.dma(mu_w[:], prm["mu_w"], w=[mu_w])
    mu_a = P.sb("mu_a", [96, 1]); P.dma(mu_a[:], prm["mu_a"], w=[mu_a])
    mu_g = P.sb("mu_g", [128, 2]); P.dma(mu_g[:], prm["mu_g"], w=[mu_g])
    w0 = P.sb("w0", [128, NC]); P.dma(w0[:], prm["w0"], w=[w0])
    a0 = P.sb("a0", [128, NC]); P.dma(a0[:], prm["a0"], w=[a0])
    k_k = P.sb("k_k", [128, NC]); P.dma(k_k[:], prm["k_k"], w=[k_k])
    k_a = P.sb("k_a", [128, NC]); P.dma(k_a[:], prm["k_a"], w=[k_a])
    r_k = P.sb("r_k", [128, NC]); P.dma(r_k[:], prm["r_k"], w=[r_k])
    w2 = P.sb("w2", [96, 1024]); P.dma(w2[:], prm["w2"], w=[w2])
    a2 = P.sb("a2", [96, 1024]); P.dma(a2[:], prm["a2"], w=[a2])
    g2 = P.sb("g2", [128, 2, 1024]); P.dma(g2[:], prm["g2"].rearrange("(c p) m -> p c m", p=128), w=[g2])
    bones = P.sb("bones", [128, 128]); P.dma(bones[:], prm["bones"], w=[bones])
    cmask = P.sb("cmask", [128, TT]); P.dma(cmask[:], prm["cmask"], w=[cmask])
    tmp = [P.sb("tmp%d" % i, [128, TT + 1]) for i in range(2)]
    twT = P.sb("twT", [96, TT]); zaT = P.sb("zaT", [96, TT]); sgT = P.sb("sgT", [128, 2, TT])
    rl = P.sb("rl", [128, TT]); kl = P.sb("kl", [128, TT]); vl = P.sb("vl", [128, TT])
    ps_w = P.ps("ps_w", [128, TT]); ps_a = P.ps("ps_a", [128, TT]); ps_g = P.ps("ps_g", [128, TT])
    ps_s = P.ps("ps_s", [128, TT]); ps_r = P.ps("ps_r", [128, TT])
    def t(n):
        return P.sb(n, [128, TT])
    lw, av, gv, kk, kp, bb, L, e1, e2, t1, t2, o3f = [t(n) for n in
        ("lw", "av", "gv", "kk", "kp", "bb", "L", "e1", "e2", "t1", "t2", "o3f")]
    o1, o2, o3, o4, o5, o6, vb16 = [P.sb(n, [128, TT], BF16) for n in ("o1", "o2", "o3", "o4", "o5", "o6", "vb16")]
    elc = P.sb("elc", [128, TT // CH])
    CW = -math.exp(-0.5)
    nch = TT // CH
    for t0 in range(0, T, TT):
        _lerp_load(P, twT, tmp[0], z1T[3072:3168, :], t0, TT, mu_w[:, 0:1], 96)
        P.act(twT[:], twT[:], AF.Tanh)
        _lerp_load(P, zaT, tmp[1], z1T[3168:3264, :], t0, TT, mu_a[:, 0:1], 96)
        for c2 in range(2):
            _lerp_load(P, sgT[:, c2, :], tmp[c2], z1T[3264 + c2 * 128:3264 + (c2 + 1) * 128, :], t0, TT, mu_g[:, c2:c2 + 1], 128)
        P.act(sgT[:], sgT[:], AF.Sigmoid)
        for c in range(NC):
            rows = slice(c * 128, (c + 1) * 128)
            _lerp_load(P, rl, tmp[0], z1T[0 + c * 128:0 + (c + 1) * 128, :], t0, TT, mu_rkv[:, c:c + 1], 128)
            _lerp_load(P, kl, tmp[1], z1T[1024 + c * 128:1024 + (c + 1) * 128, :], t0, TT, mu_rkv[:, 8 + c:9 + c], 128)
            _lerp_load(P, vl, tmp[0], z1T[2048 + c * 128:2048 + (c + 1) * 128, :], t0, TT, mu_rkv[:, 16 + c:17 + c], 128)
            P.copy(vb16[:], vl[:], eng="gpsimd")
            P.dma(scr["vT"][rows, t0:t0 + TT], vb16[:], r=[vb16], q="gpsimd", is_out=True)
            P.mm(ps_w[:], w2[:, rows], twT[:])
            P.act(lw[:], ps_w[:], AF.Sigmoid, bias=w0[:, c:c + 1])
            P.ts(lw[:], lw[:], CW, None, ALU.mult)
            P.mm(ps_a[:], a2[:, rows], zaT[:])
            P.act(av[:], ps_a[:], AF.Sigmoid, bias=a0[:, c:c + 1])
            for c2 in range(2):
                P.mm(ps_g[:], g2[:, c2, rows], sgT[:, c2, :], start=(c2 == 0), stop=(c2 == 1))
            P.copy(gv[:], ps_g[:], eng="scalar")
            P.dma(scr["gT"][rows, t0:t0 + TT], gv[:], r=[gv], q="gpsimd", is_out=True)
            P.ts(kk[:], kl[:], k_k[:, c:c + 1], None, ALU.mult)
            P.act(t1[:], kk[:], AF.Square)
            P.mm(ps_s[:], bones[:], t1[:])
            P.ts(t1[:], ps_s[:], 1e-24, None, ALU.max)
            P.act(t1[:], t1[:], AF.Sqrt)
            P.recip(t1[:], t1[:])
            P.tt(kk[:], kk[:], t1[:], ALU.mult)
            P.ts(t2[:], av[:], -1.0, k_a[:, c:c + 1], ALU.add, ALU.mult)
            P.stt(kp[:], t2[:], 1.0, kl[:], ALU.add, ALU.mult)
            P.tt(bb[:], kk[:], av[:], ALU.mult)
            P.op("vector", lambda e, L=L, lw=lw: e.tensor_tensor_scan(L[:], cmask[:], lw[:], 0.0, ALU.mult, ALU.add),
                 r=[cmask, lw], w=[L])
            P.act(e1[:], L[:], AF.Exp)
            P.tt(o1[:], rl[:], e1[:], ALU.mult)
            P.dma(scr["rtT"][rows, t0:t0 + TT], o1[:], r=[o1], q="gpsimd", is_out=True)
            P.copy(elc[:], e1[:].rearrange("p (c s) -> p c s", s=CH)[:, :, CH - 1])
            P.dma(scr["eLC"][rows, t0 // CH:t0 // CH + nch], elc[:], r=[elc], q="gpsimd", is_out=True)
            P.tt(t1[:], L[:], lw[:], ALU.subtract)
            P.act(t1[:], t1[:], AF.Exp)
            P.stt(o2[:], kk[:], -1.0, t1[:], ALU.mult, ALU.mult)
            P.dma(scr["atT"][rows, t0:t0 + TT], o2[:], r=[o2], q="gpsimd", is_out=True)
            P.act(e2[:], L[:], AF.Exp, scale=-1.0)
            P.tt(o3[:], bb[:], e2[:], ALU.mult)
            P.dma(scr["btT"][rows, t0:t0 + TT], o3[:], r=[o3], q="gpsimd", is_out=True)
            P.tt(o4[:], kp[:], e2[:], ALU.mult)
            P.dma(scr["ktT"][rows, t0:t0 + TT], o4[:], r=[o4], q="gpsimd", is_out=True)
            L3 = L[:].rearrange("p (c s) -> p c s", s=CH)
            P.tt(t2[:].rearrange("p (c s) -> p c s", s=CH), L3[:, :, CH - 1:CH].broadcast_to([128, nch, CH]), L3, ALU.subtract)
            P.act(t2[:], t2[:], AF.Exp)
            P.tt(o5[:], bb[:], t2[:], ALU.mult)
            P.dma(scr["BhT"][rows, t0:t0 + TT], o5[:], r=[o5], q="gpsimd", is_out=True)
            P.tt(o6[:], kp[:], t2[:], ALU.mult)
            P.dma(scr["KhT"][rows, t0:t0 + TT], o6[:], r=[o6], q="gpsimd", is_out=True)
            P.stt(t1[:], rl[:], r_k[:, c:c + 1], kp[:], ALU.mult, ALU.mult)
            P.mm(ps_r[:], bones[:], t1[:])
            P.tt(o3f[:], ps_r[:], vl[:], ALU.mult)
            P.dma(scr["bonT"][rows, t0:t0 + TT], o3f[:], r=[o3f], q="gpsimd", is_out=True)
    P.end_stage()


def stage_rwkv_chunk(P, scr, masks_d, ident_d, yT, T, NH=16, SUP=512):
    U = 2
    identf = P.sb("identf", [128, 128]); P.dma(identf[:], ident_d, w=[identf])
    ident = P.sb("identb", [128, 128], BF16); P.copy(ident[:], identf[:])
    msk = P.sb("msk", [64, 320]); P.dma(msk[:], masks_d, w=[msk])
    ST = [P.sb("ST%d" % h, [64, 64]) for h in range(NH)]
    STb = [P.sb("STb%d" % h, [64, 64], BF16) for h in range(NH)]
    for h in range(NH):
        P.memset(ST[h][:], 0.0, eng="gpsimd")
        P.memset(STb[h][:], 0.0, eng="gpsimd")
    names = ("atT", "rtT", "btT", "ktT", "BhT", "KhT", "vT")
    nsc = SUP // CH
    bufs = {}
    for par in range(2):
        for j in range(U):
            for nm in names:
                bufs[(nm, par, j)] = P.sb("in_%s%d_%d" % (nm, par, j), [64, SUP], BF16)
            bufs[("elc", par, j)] = P.sb("in_elc%d_%d" % (par, j), [64, nsc])
            bufs[("y", par, j)] = P.sb("out_y%d_%d" % (par, j), [64, SUP])
    def dbl(name, shape, dt=BF16):
        return [[P.sb("%s%d_%d" % (name, i, j), shape, dt) for j in range(U)] for i in range(2)]
    bG = [P.ps("bG%d" % j, [64, 320]) for j in range(U)]
    bTM = P.ps("bTM", [64, U * 192], BF16)
    bP = P.ps("bP", [64, U * 64]); bQ = P.ps("bQ", [64, U * 64]); bX = P.ps("bX", [64, U * 64])
    bRU = P.ps("bRU", [64, U * 128]); bYS = P.ps("bYS", [64, U * 128])
    tm = dbl("tm", [64, 192]); gm = dbl("gm", [64, 320])
    Pm = [dbl("Pm%d_" % i, [64, 64]) for i in range(2)]
    Qm = [dbl("Qm%d_" % i, [64, 64]) for i in range(2)]
    Xm = [dbl("Xm%d_" % i, [64, 64]) for i in range(2)]
    r0s = dbl("r0s", [64, 64]); us = dbl("us", [64, 64])
    I64 = ident[0:64, 0:64]
    u = 0
    R = range(U)
    for s0 in range(0, T, SUP):
        for g in range(NH // U):
            par = g % 2
            hs = [g * U + j for j in R]
            for j in R:
                rows = slice(hs[j] * 64, (hs[j] + 1) * 64)
                for nm in names:
                    P.dma(bufs[(nm, par, j)][:], scr[nm][rows, s0:s0 + SUP], w=[bufs[(nm, par, j)]])
                P.dma(bufs[("elc", par, j)][:], scr["eLC"][rows, s0 // CH:s0 // CH + nsc], w=[bufs[("elc", par, j)]])
            B = [[bufs[(nm, par, j)] for nm in names] for j in R]
            for ci in range(nsc):
                i2 = u % 2; u += 1
                cs = slice(ci * CH, (ci + 1) * CH)
                for j in R:
                    at, rt, bt, kt, Bh, Kh, vT_ = B[j]
                    for k3, src in enumerate((vT_, Bh, Kh)):
                        P.tr(bTM[:, j * 192 + k3 * 64:j * 192 + (k3 + 1) * 64], src[:, cs], I64)
                for j in R:
                    at, rt, bt, kt, Bh, Kh, vT_ = B[j]
                    P.mm(bG[j][:, 0:64], bt[:, cs], at[:, cs])
                    P.mm(bG[j][:, 64:128], bt[:, cs], rt[:, cs])
                    P.mm(bG[j][:, 128:192], kt[:, cs], at[:, cs])
                    P.mm(bG[j][:, 192:256], kt[:, cs], rt[:, cs])
                    P.mm(bG[j][:, 256:320], at[:, cs], bt[:, cs])
                for j in R:
                    P.copy(tm[i2][j][:], bTM[:, j * 192:(j + 1) * 192], eng="scalar")
                    P.tt(gm[i2][j][:], bG[j][:], msk[:], ALU.mult)
                Vt = [tm[i2][j][:, 0:64] for j in R]; Bt = [tm[i2][j][:, 64:128] for j in R]; Kt = [tm[i2][j][:, 128:192] for j in R]
                P0 = [gm[i2][j][:, 0:64] for j in R]; NrbT = [gm[i2][j][:, 64:128] for j in R]
                MakT = [gm[i2][j][:, 128:192] for j in R]; NrkT = [gm[i2][j][:, 192:256] for j in R]
                Q0 = [gm[i2][j][:, 256:320] for j in R]
                Pc, Qc = list(P0), list(Q0)
                X = [Xm[0][i2][j] for j in R]
                for j in R:
                    P.tt(X[j][:], P0[j], I64, ALU.add, eng="gpsimd")
                for it in range(5):
                    Qn = [Qm[it % 2][i2][j] for j in R]
                    for j in R:
                        P.mm(bQ[:, j * 64:(j + 1) * 64], Pc[j], Qc[j])
                    if it < 4:
                        Pn = [Pm[it % 2][i2][j] for j in R]
                        for j in R:
                            P.mm(bP[:, j * 64:(j + 1) * 64], Qc[j], Pc[j])
                    for j in R:
                        P.copy(Qn[j][:], bQ[:, j * 64:(j + 1) * 64], eng="scalar")
                    if it < 4:
                        for j in R:
                            P.copy(Pn[j][:], bP[:, j * 64:(j + 1) * 64], eng="vector")
                    Xn = [Xm[(it + 1) % 2][i2][j] for j in R]
                    for j in R:
                        P.mm(bX[:, j * 64:(j + 1) * 64], Qn[j][:], X[j][:], start=True, stop=False)
                        P.mm(bX[:, j * 64:(j + 1) * 64], I64, X[j][:], start=False, stop=True)
                    for j in R:
                        P.copy(Xn[j][:], bX[:, j * 64:(j + 1) * 64], eng=("scalar" if it % 2 else "vector"))
                    X = Xn
                    Qc = [Qn[j][:] for j in R]
                    if it < 4:
                        Pc = [Pn[j][:] for j in R]
                for j in R:
                    at = B[j][0]
                    P.mm(bRU[:, j * 128:j * 128 + 64], at[:, cs], STb[hs[j]][:], start=True, stop=False)
                    P.mm(bRU[:, j * 128:j * 128 + 64], MakT[j], Vt[j], start=False, stop=True)
                for j in R:
                    P.copy(r0s[i2][j][:], bRU[:, j * 128:j * 128 + 64], eng="scalar")
                for j in R:
                    P.mm(bRU[:, j * 128 + 64:j * 128 + 128], X[j][:], r0s[i2][j][:])
                for j in R:
                    P.copy(us[i2][j][:], bRU[:, j * 128 + 64:j * 128 + 128], eng="scalar")
                for j in R:
                    rt = B[j][1]
                    P.mm(bYS[:, j * 128:j * 128 + 64], STb[hs[j]][:], rt[:, cs], start=True, stop=False)
                    P.mm(bYS[:, j * 128:j * 128 + 64], us[i2][j][:], NrbT[j], start=False, stop=False)
                    P.mm(bYS[:, j * 128:j * 128 + 64], Vt[j], NrkT[j], start=False, stop=True)
                    P.mm(bYS[:, j * 128 + 64:j * 128 + 128], Bt[j], us[i2][j][:], start=True, stop=False)
                    P.mm(bYS[:, j * 128 + 64:j * 128 + 128], Kt[j], Vt[j], start=False, stop=True)
                for j in R:
                    elc = bufs[("elc", par, j)]
                    P.copy(bufs[("y", par, j)][:, cs], bYS[:, j * 128:j * 128 + 64], eng="vector")
                    P.stt(ST[hs[j]][:], ST[hs[j]][:], elc[:, ci:ci + 1], bYS[:, j * 128 + 64:j * 128 + 128], ALU.mult, ALU.add)
                for j in R:
                    P.copy(STb[hs[j]][:], ST[hs[j]][:], eng="gpsimd")
            for j in R:
                rows = slice(hs[j] * 64, (hs[j] + 1) * 64)
                P.dma(yT[rows, s0:s0 + SUP], bufs[("y", par, j)][:], r=[bufs[("y", par, j)]], q="gpsimd", is_out=True)
    P.end_stage()


def stage_rwkv_post(P, yT, scr, prm, outT, T, TT=512, gn_eps=64e-5):
    NC = 8
    lg = P.sb("lg", [128, NC]); P.dma(lg[:], prm["ln_g"], w=[lg])
    lb = P.sb("lb", [128, NC]); P.dma(lb[:], prm["ln_b"], w=[lb])
    bo = P.sb("bo64", [128, 128]); P.dma(bo[:], prm["bones64"], w=[bo])
    def d(n):
        return [P.sb("%s%d" % (n, i), [128, TT]) for i in range(2)]
    y, sq, mu, rs, gv, bn, o = d("y"), d("sq"), d("mu"), d("rs"), d("gv"), d("bn"), d("o")
    ps_m = [P.ps("ps_m%d" % i, [128, TT]) for i in range(2)]
    ps_q = [P.ps("ps_q%d" % i, [128, TT]) for i in range(2)]
    n = 0
    for t0 in range(0, T, TT):
        for c in range(NC):
            i = n % 2; n += 1
            rows = slice(c * 128, (c + 1) * 128)
            P.dma(y[i][:], yT[rows, t0:t0 + TT], w=[y[i]])
            P.dma(gv[i][:], scr["gT"][rows, t0:t0 + TT], w=[gv[i]])
            P.dma(bn[i][:], scr["bonT"][rows, t0:t0 + TT], w=[bn[i]])
            P.act(sq[i][:], y[i][:], AF.Square)
            P.mm(ps_m[i][:], bo[:], y[i][:])
            P.mm(ps_q[i][:], bo[:], sq[i][:])
            P.copy(mu[i][:], ps_m[i][:])
            P.tt(rs[i][:], mu[i][:], mu[i][:], ALU.mult)
            P.tt(rs[i][:], ps_q[i][:], rs[i][:], ALU.subtract)
            P.ts(rs[i][:], rs[i][:], gn_eps, None, ALU.add)
            P.act(rs[i][:], rs[i][:], AF.Sqrt)
            P.recip(rs[i][:], rs[i][:])
            P.tt(y[i][:], y[i][:], mu[i][:], ALU.subtract)
            P.tt(y[i][:], y[i][:], rs[i][:], ALU.mult, eng="gpsimd")
            P.act(o[i][:], y[i][:], AF.Identity, scale=lg[:, c:c + 1], bias=lb[:, c:c + 1])
            P.tt(o[i][:], o[i][:], bn[i][:], ALU.add)
            P.tt(o[i][:], o[i][:], gv[i][:], ALU.mult, eng="gpsimd")
            P.dma(outT[rows, t0:t0 + TT], o[i][:], r=[o[i]], q="gpsimd", is_out=True)
    P.end_stage()


def stage_lru(P, z1T, prm, outT, T, TT=512):
    NC = 8
    XO, GO = 3520, 4544
    cw = P.sb("cw", [128, NC, 4]); P.dma(cw[:], prm["lru_cw"], w=[cw])
    cb = P.sb("cb", [128, NC]); P.dma(cb[:], prm["lru_cb"], w=[cb])
    ba = P.sb("ba", [128, NC]); P.dma(ba[:], prm["lru_ba"], w=[ba])
    bx = P.sb("bx", [128, NC]); P.dma(bx[:], prm["lru_bx"], w=[bx])
    lam = P.sb("lam", [128, NC]); P.dma(lam[:], prm["lru_lam"], w=[lam])
    wa = P.sb("wa", [128, NC, 128]); P.dma(wa[:], prm["lru_wa_bd"], w=[wa])
    wx = P.sb("wx", [128, NC, 128]); P.dma(wx[:], prm["lru_wx_bd"], w=[wx])
    cl = P.sb("cl", [128, NC])
    P.act(cl[:], lam[:], AF.Exp, scale=-1.0)
    P.act(cl[:], cl[:], AF.Ln, bias=1.0)
    P.ts(cl[:], cl[:], -8.0, None, ALU.mult)
    hst = [P.sb("hst%d" % c, [128, 1]) for c in range(NC)]
    for c in range(NC):
        P.memset(hst[c][:], 0.0)
    def d(n, w=TT):
        return [P.sb("%s%d" % (n, i), [128, w]) for i in range(2)]
    xin, xc, gb, r_, i_, a_, u_, h_, o_ = d("xin", TT + 3), d("xc"), d("gb"), d("r_"), d("i_"), d("a_"), d("u_"), d("h_"), d("o_")
    ps_a = [P.ps("ps_a%d" % i, [128, TT]) for i in range(2)]
    ps_x = [P.ps("ps_x%d" % i, [128, TT]) for i in range(2)]
    n = 0
    for t0 in range(0, T, TT):
        for c in range(NC):
            i = n % 2; n += 1
            rows = slice(c * 128, (c + 1) * 128)
            if t0 == 0:
                P.memset(xin[i][:, 0:3], 0.0)
                P.dma(xin[i][:, 3:TT + 3], z1T[XO + c * 128:XO + (c + 1) * 128, 0:TT], w=[xin[i]])
            else:
                P.dma(xin[i][:], z1T[XO + c * 128:XO + (c + 1) * 128, t0 - 3:t0 + TT], w=[xin[i]])
            P.dma(gb[i][:], z1T[GO + c * 128:GO + (c + 1) * 128, t0:t0 + TT], w=[gb[i]])
            P.ts(xc[i][:], xin[i][:, 0:TT], cw[:, c, 0:1], cb[:, c:c + 1], ALU.mult, ALU.add)
            for j in range(1, 4):
                P.stt(xc[i][:], xin[i][:, j:j + TT], cw[:, c, j:j + 1], xc[i][:], ALU.mult, ALU.add)
            P.mm(ps_a[i][:], wa[:, c, :], xc[i][:])
            P.mm(ps_x[i][:], wx[:, c, :], xc[i][:])
            P.act(r_[i][:], ps_a[i][:], AF.Sigmoid, bias=ba[:, c:c + 1])
            P.act(i_[i][:], ps_x[i][:], AF.Sigmoid, bias=bx[:, c:c + 1])
            P.act(a_[i][:], r_[i][:], AF.Exp, scale=cl[:, c:c + 1])
            P.tt(u_[i][:], a_[i][:], a_[i][:], ALU.mult)
            P.ts(u_[i][:], u_[i][:], -1.0, 1.0, ALU.mult, ALU.add)
            P.act(u_[i][:], u_[i][:], AF.Sqrt)
            P.tt(i_[i][:], i_[i][:], xc[i][:], ALU.mult, eng="gpsimd")
            P.tt(u_[i][:], u_[i][:], i_[i][:], ALU.mult)
            P.op("vector", lambda e, h=h_[i], a=a_[i], uu=u_[i], st=hst[c]: e.tensor_tensor_scan(h[:], a[:], uu[:], st[:, 0:1], ALU.mult, ALU.add),
                 r=[a_[i], u_[i], hst[c]], w=[h_[i]])
            P.copy(hst[c][:], h_[i][:, TT - 1:TT])
            P.act(r_[i][:], gb[i][:], AF.Square)
            P.ts(r_[i][:], r_[i][:], 0.044715, 1.0, ALU.mult, ALU.add)
            P.tt(r_[i][:], r_[i][:], gb[i][:], ALU.mult, eng="gpsimd")
            P.act(r_[i][:], r_[i][:], AF.Tanh, scale=0.7978845608028654)
            P.ts(r_[i][:], r_[i][:], 1.0, 0.5, ALU.add, ALU.mult)
            P.tt(gb[i][:], gb[i][:], r_[i][:], ALU.mult, eng="gpsimd")
            P.tt(o_[i][:], h_[i][:], gb[i][:], ALU.mult, eng="gpsimd")
            P.dma(outT[rows, t0:t0 + TT], o_[i][:], r=[o_[i]], q="gpsimd", is_out=True)
    P.end_stage()


def cd_params(inp, j=0):
    f = lambda k: np.asarray(inp[k][j], np.float32)
    mu = f("cd_shift_mu")
    mu_rkv = np.ascontiguousarray(mu[:3072].reshape(24, 128).T)
    bones = np.kron(np.eye(2, dtype=np.float32), np.ones((64, 64), np.float32))
    su = np.triu(np.ones((64, 64), np.float32), 1)
    iu = np.triu(np.ones((64, 64), np.float32), 0)
    sl = np.tril(np.ones((64, 64), np.float32), -1)
    masks = np.ascontiguousarray(np.concatenate([su, iu, su, iu, sl], 1))
    cm = np.ones((128, 512), np.float32); cm[:, ::CH] = 0.0
    def bd(w):
        out = np.zeros((8, 128, 128), np.float32)
        for c in range(8):
            out[c, :64, :64] = w[2 * c]
            out[c, 64:, 64:] = w[2 * c + 1]
        return np.ascontiguousarray(out.transpose(1, 0, 2))
    return {
        "mu_rkv": mu_rkv, "mu_w": mu[3072:3168].reshape(96, 1).copy(), "mu_a": mu[3168:3264].reshape(96, 1).copy(),
        "mu_g": np.ascontiguousarray(mu[3264:3520].reshape(2, 128).T),
        "w0": pl(f("cd_w0")), "a0": pl(f("cd_a0")), "k_k": pl(f("cd_k_k")), "k_a": pl(f("cd_k_a")),
        "r_k": pl(f("cd_r_k").reshape(-1)), "w2": f("cd_w2"), "a2": f("cd_a2"), "g2": f("cd_g2"),
        "bones": bones, "bones64": bones / 64.0 * 1.0, "cmask": cm, "masks": masks,
        "ln_g": pl(f("cd_ln_x_g")), "ln_b": pl(f("cd_ln_x_b")),
        "lru_cw": np.ascontiguousarray(f("cd_lru_conv_w").reshape(4, 8, 128).transpose(2, 1, 0)),
        "lru_cb": pl(f("cd_lru_conv_b")), "lru_ba": pl(f("cd_lru_ba")), "lru_bx": pl(f("cd_lru_bx")),
        "lru_lam": pl(f("cd_lru_lambda")), "lru_wa_bd": bd(f("cd_lru_wa")), "lru_wx_bd": bd(f("cd_lru_wx")),
        "ident": np.eye(128, dtype=np.float32),
    }


CD_PRM_SHAPES = {
    "mu_rkv": [128, 24], "mu_w": [96, 1], "mu_a": [96, 1], "mu_g": [128, 2], "w0": [128, 8], "a0": [128, 8],
    "k_k": [128, 8], "k_a": [128, 8], "r_k": [128, 8], "w2": [96, 1024], "a2": [96, 1024], "g2": [256, 1024],
    "bones": [128, 128], "bones64": [128, 128], "cmask": [128, 512], "masks": [64, 320],
    "ln_g": [128, 8], "ln_b": [128, 8], "lru_cw": [128, 8, 4], "lru_cb": [128, 8], "lru_ba": [128, 8],
    "lru_bx": [128, 8], "lru_lam": [128, 8], "lru_wa_bd": [128, 8, 128], "lru_wx_bd": [128, 8, 128],
    "ident": [128, 128],
}


def emit_mixer_cd(P, z1T, prm, mixT, T):
    scr = {nm: P.dram_tmp("cd_" + nm, [1024, T], BF16) for nm in ("atT", "rtT", "btT", "ktT", "BhT", "KhT", "vT")}
    for nm in ("gT", "bonT"):
        scr[nm] = P.dram_tmp("cd_" + nm, [1024, T])
    scr["eLC"] = P.dram_tmp("cd_eLC", [1024, T // CH])
    yT = P.dram_tmp("cd_yT", [1024, T])
    stage_rwkv_prep(P, z1T, prm, scr, T)
    stage_rwkv_chunk(P, scr, prm["masks"], prm["ident"], yT, T)
    stage_rwkv_post(P, yT, scr, prm, mixT[0:1024, :], T)
    stage_lru(P, z1T, prm, mixT[1024:2048, :], T)


def stage_select(P, srcT, flag_d, dstT, R, TH, TT=2048):
    flag = P.sb("flag_s", [128, 1]); P.dma(flag[:], flag_d, w=[flag])
    a = [P.sb("sa%d" % i, [128, TT]) for i in range(2)]
    b = [P.sb("sb%d" % i, [128, TT]) for i in range(2)]
    TT = min(TT, TH)
    n = 0
    for r0 in range(0, R, 128):
        for t0 in range(0, TH, TT):
            i = n % 2; n += 1
            P.dma(a[i][:, :TT], srcT[r0:r0 + 128, t0:t0 + TT], w=[a[i]])
            P.dma(b[i][:, :TT], srcT[r0:r0 + 128, TH + t0:TH + t0 + TT], w=[b[i]])
            P.tt(b[i][:, :TT], b[i][:, :TT], a[i][:, :TT], ALU.subtract, eng="gpsimd")
            P.stt(a[i][:, :TT], b[i][:, :TT], flag[:, 0:1], a[i][:, :TT], ALU.mult, ALU.add)
            P.dma(dstT[r0:r0 + 128, t0:t0 + TT], a[i][:, :TT], r=[a[i]], q="gpsimd", is_out=True)
    P.end_stage()


CD_INP = 5632


def build_full(S, D=2048, DE=1024):
    P = Prog()
    TH = S // 2
    SK = S + 128
    AB_IN = 3584
    JC = DE // 128
    di = P.dram_in
    xhT = di("xhT", [D, SK]); cT = di("cT", [D, 1])
    ada_w = [di("ada_w%d" % l, [D, 6 * D]) for l in range(2)]
    ada_b = [di("ada_b%d" % l, [128, 6 * D // 128]) for l in range(2)]
    g_mix = [di("g_mix%d" % l, [128, 16]) for l in range(2)]
    g_ffn = [di("g_ffn%d" % l, [128, 16]) for l in range(2)]
    g_fin = di("g_fin", [128, 16])
    w_in0 = di("w_in0", [D, AB_IN]); w_out0 = di("w_out0", [D, D])
    w_in1 = di("w_in1", [D, CD_INP]); w_out1 = di("w_out1", [D, D])
    pos_bc = di("pos_bc", [128, SK], I32)
    invf = di("invf", [128, 1]); sgn = di("sgn", [128, 1])
    maskg = di("maskg", [128, 256]); mask0 = di("mask0", [128, 256])
    sink_bc = di("sink_bc", [128, 16]); ident = di("ident", [128, 128])
    cw = di("cw", [128, 8, 31]); cb = di("cb", [128, 8]); lg = di("lg", [128, 8]); lb = di("lb", [128, 8])
    zflag = di("zflag", [128, 1]); hflag = di("hflag", [128, 1])
    rw = di("rw", [128, 16, 16]); rb = di("rb_bc", [128, 16])
    w1 = [di("w1r%d" % l, [16, JC, 128, 16, 128]) for l in range(2)]
    w3 = [di("w3r%d" % l, [16, JC, 128, 16, 128]) for l in range(2)]
    w2 = [di("w2r%d" % l, [16, 16, 128, JC, 128]) for l in range(2)]
    prm = {k: di("p_" + k, sh) for k, sh in CD_PRM_SHAPES.items()}
    outT = P.dram_out("outT", [D, TH])
    tmp = P.dram_tmp
    modT = [tmp("modT%d" % l, [6 * D, 1]) for l in range(2)]
    hT = tmp("hT", [D, SK]); zT = tmp("zT", [AB_IN, SK]); mixT = tmp("mixT", [D, S])
    x1T = tmp("x1T", [D, S]); hfT = tmp("hfT", [D, S]); gatesT = tmp("gatesT", [16, S]); x2T = tmp("x2T", [D, S])
    z1T = tmp("z1T", [CD_INP, S]); mix1T = tmp("mix1T", [D, S])
    x2hT = tmp("x2hT", [D, TH]); mix1hT = tmp("mix1hT", [D, TH]); x3T = tmp("x3T", [D, TH]); x4T = tmp("x4T", [D, TH])
    mv = [m.rearrange("(c p) o -> p (c o)", p=128) for m in modT]
    for l in range(2):
        stage_lin(P, cT, ada_w[l], modT[l], K=D, M=6 * D, T=1, mode="bias", fp32=True, in_silu=True, bias_d=ada_b[l])
    stage_normfm(P, xhT, modT[0], g_mix[0], 0, 1, hT, SK)
    stage_lin(P, hT, w_in0, zT, K=D, M=AB_IN, T=SK)
    TQ = min(4096, S)
    for sg in range(S // TQ):
        c0 = sg * TQ
        stage_att(P, zT[:, c0:c0 + TQ + 128], pos_bc[:, c0:c0 + TQ + 128], invf, sgn, maskg,
                  mask0 if sg == 0 else maskg, sink_bc, ident, mixT[0:1024, c0:c0 + TQ], TQ)
    stage_conv(P, zT[1536:2560, 98:SK], zT[2560:3584, 98:SK], cw, cb, lg, lb, zflag, mixT[1024:2048, :], S, ident_d=ident)
    stage_lin(P, mixT, w_out0, x1T, K=D, M=D, T=S, mode="resid", gate_d=mv[0][:, 32:48], rT=xhT[:, 128:SK])
    stage_normfm(P, x1T, modT[0], g_ffn[0], 3, 4, hfT, S)
    stage_route(P, hfT, rw, rb, ident, gatesT, S)
    stage_moe(P, hfT, x1T, gatesT, w1[0], w3[0], w2[0], modT[0], 5, x2T, S, DE=DE)
    stage_normfm(P, x2T, modT[1], g_mix[1], 0, 1, hT[:, 0:S], S)
    stage_lin(P, hT[:, 0:S], w_in1, z1T, K=D, M=CD_INP, T=S)
    emit_mixer_cd(P, z1T, prm, mix1T, S)
    stage_select(P, x2T, hflag, x2hT, D, TH)
    stage_select(P, mix1T, hflag, mix1hT, D, TH)
    stage_lin(P, mix1hT, w_out1, x3T, K=D, M=D, T=TH, mode="resid", gate_d=mv[1][:, 32:48], rT=x2hT)
    stage_normfm(P, x3T, modT[1], g_ffn[1], 3, 4, hfT[:, 0:TH], TH)
    stage_route(P, hfT[:, 0:TH], rw, rb, ident, gatesT[:, 0:TH], TH)
    stage_moe(P, hfT[:, 0:TH], x3T, gatesT[:, 0:TH], w1[1], w3[1], w2[1], modT[1], 5, x4T, TH, DE=DE)
    stage_normfm(P, x4T, None, g_fin, 0, 0, outT, TH)
    return P


def full_inmaps(inp):
    x = np.asarray(inp["x"], np.float32)
    B, S, D = x.shape
    pos = np.asarray(inp["positions"], np.int32)
    invf = (10000.0 ** (-np.arange(32, dtype=np.float32) / 32)).astype(np.float32)
    invf128 = np.tile(invf, 4).reshape(128, 1).astype(np.float32)
    sgn = np.tile(np.concatenate([-np.ones(32), np.ones(32)]), 2).reshape(128, 1).astype(np.float32)
    qi = np.arange(128)[:, None]
    kj = np.arange(256)[None, :]
    dist = 128 + qi - kj
    valid = (dist >= 0) & (dist < 128)
    maskg = np.where(valid, 0.0, -1e30).astype(np.float32)
    mask_first = np.where(valid & (kj >= 128), 0.0, -1e30).astype(np.float32)
    sinks = np.asarray(inp["ab_sinks"][0], np.float32)
    conv_w = np.asarray(inp["ab_conv_w"][0], np.float32).reshape(31, 1024)
    w_in1 = np.asarray(inp["cd_w_in"][0], np.float32)
    w_in1p = np.concatenate([w_in1, np.zeros((D, CD_INP - w_in1.shape[1]), np.float32)], 1)
    common = {
        "g_fin": pl(inp["final_norm"]),
        "w_in0": np.ascontiguousarray(inp["ab_w_in"][0]), "w_out0": np.ascontiguousarray(inp["ab_w_out"][0]),
        "w_in1": np.ascontiguousarray(w_in1p), "w_out1": np.ascontiguousarray(inp["cd_w_out"][0]),
        "invf": invf128, "sgn": sgn, "maskg": maskg, "mask0": mask_first,
        "sink_bc": np.ascontiguousarray(np.broadcast_to(sinks[None, :], (128, 16))).astype(np.float32),
        "ident": np.eye(128, dtype=np.float32),
        "cw": np.ascontiguousarray(conv_w.reshape(31, 8, 128).transpose(2, 1, 0)),
        "cb": pl(inp["ab_conv_b"][0]), "lg": pl(inp["ab_conv_ln_g"][0]), "lb": pl(inp["ab_conv_ln_b"][0]),
        "zflag": np.zeros((128, 1), np.float32),
        "rw": np.ascontiguousarray(np.asarray(inp["router_w"], np.float32).reshape(16, 128, 16).transpose(1, 0, 2)),
        "rb_bc": np.ascontiguousarray(np.broadcast_to(np.asarray(inp["router_bias"], np.float32)[None], (128, 16))),
    }
    for l in range(2):
        common["ada_w%d" % l] = np.ascontiguousarray(inp["ada_w"][l])
        common["ada_b%d" % l] = pl(inp["ada_b"][l])
        common["g_mix%d" % l] = pl(inp["norm_mix"][l])
        common["g_ffn%d" % l] = pl(inp["norm_ffn"][l])
        a, b_, c_ = moe_weights_layout(np.asarray(inp["moe_w1"][l]), np.asarray(inp["moe_w3"][l]), np.asarray(inp["moe_w2"][l]))
        common["w1r%d" % l], common["w3r%d" % l], common["w2r%d" % l] = a, b_, c_
    for k, v in cd_params(inp).items():
        common["p_" + k] = np.ascontiguousarray(v, dtype=np.float32)
    maps = []
    for b in range(B):
        xh = np.concatenate([np.zeros((128, D), np.float32), x[b]], 0)
        ph = np.concatenate([np.zeros((128,), np.int32), pos[b]])
        xhT = np.ascontiguousarray(xh.T)
        cT = np.ascontiguousarray(np.asarray(inp["c"], np.float32)[b].reshape(D, 1))
        pbc = np.ascontiguousarray(np.broadcast_to(ph[None, :], (128, ph.shape[0]))).astype(np.int32)
        for f in range(2):
            m = dict(common)
            m["xhT"] = xhT
            m["cT"] = cT
            m["pos_bc"] = pbc
            m["hflag"] = np.full((128, 1), float(f), np.float32)
            maps.append(m)
    return maps


def kernel(**inputs):
    inp = {k: np.asarray(v) for k, v in inputs.items()}
    B, S, D = inp["x"].shape
    DE = inp["moe_w1"].shape[-1]
    P = build_full(S, D, DE)
    maps = full_inmaps(inp)
    res = run(P, maps)
    TH = S // 2
    out = np.empty((B, S, D), np.float32)
    for b in range(B):
        for f in range(2):
            out[b, f * TH:(f + 1) * TH, :] = res.results[2 * b + f]["outT"].T
    return out


PAIRS = [[0, 1], [2, 3], [4, 5], [6, 7]]


def build_full2(S, D=2048, DE=1024):
    P = Prog()
    TH = S // 2
    TK = TH + 128
    AB_IN = 3584
    JC = DE // 128
    di = P.dram_in
    xhT = di("xhT", [D, TK]); cT = di("cT", [D, 1])
    ada_w = [di("ada_w%d" % l, [D, 6 * D]) for l in range(2)]
    ada_b = [di("ada_b%d" % l, [128, 6 * D // 128]) for l in range(2)]
    g_mix = [di("g_mix%d" % l, [128, 16]) for l in range(2)]
    g_ffn = [di("g_ffn%d" % l, [128, 16]) for l in range(2)]
    g_fin = di("g_fin", [128, 16])
    w_in0 = di("w_in0", [D, AB_IN]); w_out0 = di("w_out0", [D, D])
    w_in1 = di("w_in1", [D, CD_INP]); w_out1 = di("w_out1", [D, D])
    pos_bc = di("pos_bc", [128, TK], I32)
    invf = di("invf", [128, 1]); sgn = di("sgn", [128, 1])
    maskg = di("maskg", [128, 256]); mask0 = di("mask0", [128, 256])
    sink_bc = di("sink_bc", [128, 16]); ident = di("ident", [128, 128])
    cw = di("cw", [128, 8, 31]); cb = di("cb", [128, 8]); lg = di("lg", [128, 8]); lb = di("lb", [128, 8])
    zflag = di("zflag", [128, 1]); hflag = di("hflag", [128, 1])
    rw = di("rw", [128, 16, 16]); rb = di("rb_bc", [128, 16])
    w1 = [di("w1r%d" % l, [16, JC, 128, 16, 128]) for l in range(2)]
    w3 = [di("w3r%d" % l, [16, JC, 128, 16, 128]) for l in range(2)]
    w2 = [di("w2r%d" % l, [16, 16, 128, JC, 128]) for l in range(2)]
    prm = {k: di("p_" + k, sh) for k, sh in CD_PRM_SHAPES.items()}
    outT = P.dram_out("outT", [D, TH])
    tmp = P.dram_tmp
    modT = [tmp("modT%d" % l, [6 * D, 1]) for l in range(2)]
    hT = tmp("hT", [D, TK]); zT = tmp("zT", [AB_IN, TK]); mixT = tmp("mixT", [D, TH])
    x1T = tmp("x1T", [D, TH]); hfT = tmp("hfT", [D, TH]); gatesT = tmp("gatesT", [16, TH]); x2hT = tmp("x2hT", [D, TH])
    x2g = tmp("x2g", [2 * D, TH])
    h1T = tmp("h1T", [D, S]); z1T = tmp("z1T", [CD_INP, S]); mix1T = tmp("mix1T", [D, S])
    mix1hT = tmp("mix1hT", [D, TH]); x3T = tmp("x3T", [D, TH]); x4T = tmp("x4T", [D, TH])
    mv = [m.rearrange("(c p) o -> p (c o)", p=128) for m in modT]
    for l in range(2):
        stage_lin(P, cT, ada_w[l], modT[l], K=D, M=6 * D, T=1, mode="bias", fp32=True, in_silu=True, bias_d=ada_b[l])
    stage_normfm(P, xhT, modT[0], g_mix[0], 0, 1, hT, TK)
    stage_lin(P, hT, w_in0, zT, K=D, M=AB_IN, T=TK)
    stage_att(P, zT, pos_bc, invf, sgn, maskg, mask0, sink_bc, ident, mixT[0:1024, :], TH)
    stage_conv(P, zT[1536:2560, 98:TK], zT[2560:3584, 98:TK], cw, cb, lg, lb, zflag, mixT[1024:2048, :], TH, ident_d=ident)
    stage_lin(P, mixT, w_out0, x1T, K=D, M=D, T=TH, mode="resid", gate_d=mv[0][:, 32:48], rT=xhT[:, 128:TK])
    stage_normfm(P, x1T, modT[0], g_ffn[0], 3, 4, hfT, TH)
    stage_route(P, hfT, rw, rb, ident, gatesT, TH)
    stage_moe(P, hfT, x1T, gatesT, w1[0], w3[0], w2[0], modT[0], 5, x2hT, TH, DE=DE)
    P.allgather(x2g[:, :], x2hT[:, :], PAIRS)
    P.end_stage()
    for r in range(2):
        stage_normfm(P, x2g[r * D:(r + 1) * D, :], modT[1], g_mix[1], 0, 1, h1T[:, r * TH:(r + 1) * TH], TH)
    stage_lin(P, h1T, w_in1, z1T, K=D, M=CD_INP, T=S)
    emit_mixer_cd(P, z1T, prm, mix1T, S)
    stage_select(P, mix1T, hflag, mix1hT, D, TH)
    stage_lin(P, mix1hT, w_out1, x3T, K=D, M=D, T=TH, mode="resid", gate_d=mv[1][:, 32:48], rT=x2hT)
    stage_normfm(P, x3T, modT[1], g_ffn[1], 3, 4, hfT, TH)
    stage_route(P, hfT, rw, rb, ident, gatesT, TH)
    stage_moe(P, hfT, x3T, gatesT, w1[1], w3[1], w2[1], modT[1], 5, x4T, TH, DE=DE)
    stage_normfm(P, x4T, None, g_fin, 0, 0, outT, TH)
    return P


def full2_inmaps(inp):
    maps = full_inmaps(inp)
    x = np.asarray(inp["x"], np.float32)
    B, S, D = x.shape
    TH = S // 2
    pos = np.asarray(inp["positions"], np.int32)
    maskg = maps[0]["maskg"]
    mask_first = maps[0]["mask0"]
    for b in range(B):
        for f in range(2):
            m = maps[2 * b + f]
            if f == 0:
                xh = np.concatenate([np.zeros((128, D), np.float32), x[b, 0:TH]], 0)
                ph = np.concatenate([np.zeros((128,), np.int32), pos[b, 0:TH]])
            else:
                xh = x[b, TH - 128:S]
                ph = pos[b, TH - 128:S]
            m["xhT"] = np.ascontiguousarray(xh.T)
            m["pos_bc"] = np.ascontiguousarray(np.broadcast_to(ph[None, :], (128, ph.shape[0]))).astype(np.int32)
            m["mask0"] = mask_first if f == 0 else maskg
            m["zflag"] = np.full((128, 1), float(f), np.float32)
    return maps
```
